# Optimizing a Trainium2 kernel written in Bass

```python
import math
import jax, jax.numpy as jnp
from jax import lax
import numpy as np

D_MODEL = 4096
BATCH = 2
SEQ = 8192
DEPTH = 2

GRID_W = 64
CTX_LEN = 256
HEAD_DIM = 128
N_Q_HEADS = 24
N_KV_HEADS = 8
Q_PER_KV = N_Q_HEADS // N_KV_HEADS
ATTN_WIDTH = N_Q_HEADS * HEAD_DIM
KV_WIDTH = N_KV_HEADS * HEAD_DIM
WINDOW = 128
BLOCK = 128
ROPE_BASE = 10000.0
AXIS_ROPE_DIM = HEAD_DIM // 2
SCALE = HEAD_DIM ** -0.5
SSM_WIDTH = D_MODEL - ATTN_WIDTH
SSM_GROUP = 16
SSM_GROUPS = SSM_WIDTH // SSM_GROUP
SSM_STATE = 64
MIX_WIDTH = ATTN_WIDTH + SSM_WIDTH
IN_COLS = ATTN_WIDTH + 2 * KV_WIDTH + SSM_WIDTH
D_FF = 11008
N_EXPERTS = 8
TOP_K = 2
D_EXPERT = 4096
N_DENSE = (DEPTH + 1) // 2
N_MOE = DEPTH // 2
N_MOD = 6
EPS = 1e-6
NEG_INF = -1e30
DT_MIN = 1e-3
DT_MAX = 1e-1

kernel_name = "hybrid_swa_s5_moe_diffusion_block"


def rms_norm(x, g):
    xf = x.astype(jnp.float32)
    y = xf * lax.rsqrt(jnp.mean(xf * xf, axis=-1, keepdims=True) + EPS)
    return (y * g.astype(jnp.float32)).astype(x.dtype)


def modulate(x, g, shift, scale):
    return rms_norm(x, g) * (1.0 + scale) + shift


def ada_modulation(cond, w_mod, b_mod):
    m = jax.nn.silu(cond) @ w_mod + b_mod
    return jnp.split(m, N_MOD, axis=-1)


def axial_rope_tables(n_tok):
    rows = n_tok // GRID_W
    row = jnp.broadcast_to(jnp.arange(rows)[:, None], (rows, GRID_W)).reshape(-1).astype(jnp.float32)
    col = jnp.broadcast_to(jnp.arange(GRID_W)[None, :], (rows, GRID_W)).reshape(-1).astype(jnp.float32)
    inv_freq = ROPE_BASE ** (-jnp.arange(0, AXIS_ROPE_DIM, 2, dtype=jnp.float32) / AXIS_ROPE_DIM)
    ang_r = row[:, None] * inv_freq
    ang_c = col[:, None] * inv_freq
    ang = jnp.concatenate([ang_r, ang_r, ang_c, ang_c], axis=-1)
    return jnp.cos(ang), jnp.sin(ang)


def _rotate_half(x):
    x1, x2 = jnp.split(x, 2, axis=-1)
    return jnp.concatenate([-x2, x1], axis=-1)


def apply_axial_rope(x, cos, sin):
    xr, xc = jnp.split(x, 2, axis=-1)
    rot = jnp.concatenate([_rotate_half(xr), _rotate_half(xc)], axis=-1)
    return x * cos[:, None, :].astype(x.dtype) + rot * sin[:, None, :].astype(x.dtype)


def latent_attention(q, k, v, k_ctx, v_ctx, sink, cos, sin):
    bsz, n_tok = q.shape[:2]
    nb = n_tok // BLOCK
    n_ctx = k_ctx.shape[1]
    qr = apply_axial_rope(q, cos, sin).reshape(bsz, nb, BLOCK, N_KV_HEADS, Q_PER_KV, HEAD_DIM)
    qp = q.reshape(bsz, nb, BLOCK, N_KV_HEADS, Q_PER_KV, HEAD_DIM)
    kr = apply_axial_rope(k, cos, sin)

    def band(t):
        tb = t.reshape(bsz, nb, BLOCK, N_KV_HEADS, HEAD_DIM)
        tp = jnp.pad(tb, ((0, 0), (1, 1), (0, 0), (0, 0), (0, 0)))
        return jnp.concatenate([tp[:, :-2], tp[:, 1:-1], tp[:, 2:]], axis=2)

    kw, vw = band(kr), band(v)
    qpos = jnp.arange(nb)[:, None] * BLOCK + jnp.arange(BLOCK)[None, :]
    kpos = (jnp.arange(nb)[:, None] - 1) * BLOCK + jnp.arange(3 * BLOCK)[None, :]
    valid = ((kpos[:, None, :] >= 0) & (kpos[:, None, :] < n_tok)
             & (jnp.abs(qpos[:, :, None] - kpos[:, None, :]) <= WINDOW))
    s_w = jnp.einsum('bnqkgd,bnskd->bnkgqs', qr, kw).astype(jnp.float32) * SCALE
    s_w = jnp.where(valid[None, :, None, None], s_w, NEG_INF)
    s_c = jnp.einsum('bnqkgd,bskd->bnkgqs', qp, k_ctx).astype(jnp.float32) * SCALE
    s_sink = jnp.broadcast_to(sink.astype(jnp.float32).reshape(N_KV_HEADS, Q_PER_KV, 1, 1),
                              s_w.shape[:-1] + (1,))
    p = jax.nn.softmax(jnp.concatenate([s_w, s_c, s_sink], axis=-1), axis=-1)
    p_w = p[..., :3 * BLOCK].astype(v.dtype)
    p_c = p[..., 3 * BLOCK:3 * BLOCK + n_ctx].astype(v.dtype)
    o = (jnp.einsum('bnkgqs,bnskd->bnqkgd', p_w, vw)
         + jnp.einsum('bnkgqs,bskd->bnqkgd', p_c, v_ctx))
    return o.reshape(bsz, n_tok, ATTN_WIDTH)


def context_attention(q, k, v, sink):
    bsz, n_ctx = q.shape[:2]
    qg = q.reshape(bsz, n_ctx, N_KV_HEADS, Q_PER_KV, HEAD_DIM)
    s = jnp.einsum('bqkgd,bskd->bkgqs', qg, k).astype(jnp.float32) * SCALE
    s_sink = jnp.broadcast_to(sink.astype(jnp.float32).reshape(N_KV_HEADS, Q_PER_KV, 1, 1),
                              s.shape[:-1] + (1,))
    p = jax.nn.softmax(jnp.concatenate([s, s_sink], axis=-1), axis=-1)[..., :n_ctx]
    o = jnp.einsum('bkgqs,bskd->bqkgd', p.astype(v.dtype), v)
    return o.reshape(bsz, n_ctx, ATTN_WIDTH)


def s5_discretize(a_re, a_im, log_dt, b_re, b_im):
    lam = lax.complex(a_re.astype(jnp.float32), a_im.astype(jnp.float32))
    dt = jnp.exp(log_dt.astype(jnp.float32))[..., None]
    a_bar = jnp.exp(lam * dt)
    b_bar = ((a_bar - 1.0) / lam)[..., None] * lax.complex(b_re.astype(jnp.float32), b_im.astype(jnp.float32))
    return a_bar, b_bar


def _linear_recurrence_op(e1, e2):
    a1, b1 = e1
    a2, b2 = e2
    return a1 * a2, a2 * b1 + b2


def s5_states(u, a_bar, b_bar, h0_fwd, h0_bwd):
    bsz, n_tok, _ = u.shape
    ug = u.astype(jnp.float32).reshape(bsz, n_tok, SSM_GROUPS, SSM_GROUP)
    states = []
    for direction, (reverse, h0) in enumerate(((False, h0_fwd), (True, h0_bwd))):
        bu = jnp.einsum('blgc,gpc->blgp', ug, b_bar[direction])
        if h0 is not None:
            bu = bu.at[:, -1 if reverse else 0].add(a_bar[direction] * h0)
        a = jnp.broadcast_to(a_bar[direction], bu.shape)
        _, h = lax.associative_scan(_linear_recurrence_op, (a, bu), reverse=reverse, axis=1)
        states.append(h)
    return states[0], states[1]


def s5_readout(u, h_fwd, h_bwd, c_re, c_im, d_skip, w_glu):
    bsz, n_tok, _ = u.shape
    c_mat = lax.complex(c_re.astype(jnp.float32), c_im.astype(jnp.float32))
    y = (jnp.real(jnp.einsum('blgp,gcp->blgc', h_fwd, c_mat[0]))
         + jnp.real(jnp.einsum('blgp,gcp->blgc', h_bwd, c_mat[1]))).reshape(bsz, n_tok, SSM_WIDTH)
    y = y + d_skip.astype(jnp.float32) * u.astype(jnp.float32)
    y = jax.nn.gelu(y).astype(u.dtype)
    return y * jax.nn.sigmoid(y @ w_glu)


def merge_groups(o_attn, o_ssm, g_oa, g_os, w_out):
    return jnp.concatenate([rms_norm(o_attn, g_oa), rms_norm(o_ssm, g_os)], axis=-1) @ w_out


def hybrid_mixer(hx, hc, cos, sin, w_in, sink, a_re, a_im, log_dt, b_re, b_im, c_re, c_im,
                 d_skip, w_glu, g_oa, g_os, w_out, last):
    bsz, n_tok, _ = hx.shape
    n_ctx = hc.shape[1]
    px = hx @ w_in
    qx = px[..., :ATTN_WIDTH].reshape(bsz, n_tok, N_Q_HEADS, HEAD_DIM)
    kx = px[..., ATTN_WIDTH:ATTN_WIDTH + KV_WIDTH].reshape(bsz, n_tok, N_KV_HEADS, HEAD_DIM)
    vx = px[..., ATTN_WIDTH + KV_WIDTH:ATTN_WIDTH + 2 * KV_WIDTH].reshape(bsz, n_tok, N_KV_HEADS, HEAD_DIM)
    ux = px[..., ATTN_WIDTH + 2 * KV_WIDTH:]
    pc = hc @ w_in[:, ATTN_WIDTH:]
    kc = pc[..., :KV_WIDTH].reshape(bsz, n_ctx, N_KV_HEADS, HEAD_DIM)
    vc = pc[..., KV_WIDTH:2 * KV_WIDTH].reshape(bsz, n_ctx, N_KV_HEADS, HEAD_DIM)
    uc = pc[..., 2 * KV_WIDTH:]
    a_bar, b_bar = s5_discretize(a_re, a_im, log_dt, b_re, b_im)
    hcf, hcb = s5_states(uc, a_bar, b_bar, None, None)
    hxf, hxb = s5_states(ux, a_bar, b_bar, hcf[:, -1], hcb[:, 0])
    o_x = merge_groups(latent_attention(qx, kx, vx, kc, vc, sink, cos, sin),
                       s5_readout(ux, hxf, hxb, c_re, c_im, d_skip, w_glu), g_oa, g_os, w_out)
    if last:
        return o_x, None
    qc = (hc @ w_in[:, :ATTN_WIDTH]).reshape(bsz, n_ctx, N_Q_HEADS, HEAD_DIM)
    o_c = merge_groups(context_attention(qc, kc, vc, sink),
                       s5_readout(uc, hcf, hcb, c_re, c_im, d_skip, w_glu), g_oa, g_os, w_out)
    return o_x, o_c


def swiglu(h, w_g, w_u, w_d):
    return (jax.nn.silu(h @ w_g) * (h @ w_u)) @ w_d


def moe_ffn(h, w_router, w_g, w_u, w_d):
    logits = (h @ w_router).astype(jnp.float32)
    top_v, top_i = lax.top_k(logits, TOP_K)
    top_w = jax.nn.softmax(top_v, axis=-1)
    gates = jnp.sum(jax.nn.one_hot(top_i, N_EXPERTS, dtype=jnp.float32) * top_w[..., None], axis=-2)
    gates = gates.astype(h.dtype)
    out = jnp.zeros_like(h)
    for e in range(N_EXPERTS):
        out = out + gates[..., e:e + 1] * swiglu(h, w_g[e], w_u[e], w_d[e])
    return out


def channel_mixer(h, layer, w_ff_gate, w_ff_up, w_ff_down, w_router, w_exp_gate, w_exp_up, w_exp_down):
    j = layer // 2
    if layer % 2 == 0:
        return swiglu(h, w_ff_gate[j], w_ff_up[j], w_ff_down[j])
    return moe_ffn(h, w_router[j], w_exp_gate[j], w_exp_up[j], w_exp_down[j])


def setup_inputs(seed: int = 0) -> dict:
    key = jax.random.key(seed)
    ks = jax.random.split(key, 32)
    f32 = jnp.float32

    def nrm(i, shape, scale):
        return jax.random.normal(ks[i], shape, f32) * scale

    n_idx = jnp.arange(SSM_STATE, dtype=f32)
    ssm_shape = (DEPTH, 2, SSM_GROUPS, SSM_STATE)
    return {
        "x": nrm(0, (BATCH, SEQ, D_MODEL), 1.0),
        "c": nrm(1, (BATCH, D_MODEL), 1.0),
        "ctx": nrm(2, (BATCH, CTX_LEN, D_MODEL), 1.0),
        "c_ctx": nrm(3, (D_MODEL,), 1.0),
        "w_mod": nrm(4, (DEPTH, D_MODEL, N_MOD * D_MODEL), 0.5 * D_MODEL ** -0.5),
        "b_mod": nrm(5, (DEPTH, N_MOD * D_MODEL), 0.01),
        "g_attn_norm": 1.0 + nrm(6, (DEPTH, D_MODEL), 0.02),
        "w_in": nrm(7, (DEPTH, D_MODEL, IN_COLS), D_MODEL ** -0.5),
        "attn_sink": nrm(8, (DEPTH, N_Q_HEADS), 0.5),
        "ssm_a_re": -0.5 * jnp.exp(nrm(9, ssm_shape, 0.01)),
        "ssm_a_im": math.pi * n_idx + nrm(10, ssm_shape, 0.01),
        "ssm_log_dt": jax.random.uniform(ks[11], (DEPTH, 2, SSM_GROUPS), f32,
                                         math.log(DT_MIN), math.log(DT_MAX)),
        "ssm_b_re": nrm(12, (DEPTH, 2, SSM_GROUPS, SSM_STATE, SSM_GROUP), (2 * SSM_GROUP) ** -0.5),
        "ssm_b_im": nrm(13, (DEPTH, 2, SSM_GROUPS, SSM_STATE, SSM_GROUP), (2 * SSM_GROUP) ** -0.5),
        "ssm_c_re": nrm(14, (DEPTH, 2, SSM_GROUPS, SSM_GROUP, SSM_STATE), (2 * SSM_STATE) ** -0.5),
        "ssm_c_im": nrm(15, (DEPTH, 2, SSM_GROUPS, SSM_GROUP, SSM_STATE), (2 * SSM_STATE) ** -0.5),
        "ssm_d": nrm(16, (DEPTH, SSM_WIDTH), 1.0),
        "w_glu": nrm(17, (DEPTH, SSM_WIDTH, SSM_WIDTH), SSM_WIDTH ** -0.5),
        "g_out_attn": 1.0 + nrm(18, (DEPTH, ATTN_WIDTH), 0.02),
        "g_out_ssm": 1.0 + nrm(19, (DEPTH, SSM_WIDTH), 0.02),
        "w_out": nrm(20, (DEPTH, MIX_WIDTH, D_MODEL), MIX_WIDTH ** -0.5),
        "g_ffn_norm": 1.0 + nrm(21, (DEPTH, D_MODEL), 0.02),
        "w_ff_gate": nrm(22, (N_DENSE, D_MODEL, D_FF), D_MODEL ** -0.5),
        "w_ff_up": nrm(23, (N_DENSE, D_MODEL, D_FF), D_MODEL ** -0.5),
        "w_ff_down": nrm(24, (N_DENSE, D_FF, D_MODEL), D_FF ** -0.5),
        "w_router": nrm(25, (N_MOE, D_MODEL, N_EXPERTS), D_MODEL ** -0.5),
        "w_exp_gate": nrm(26, (N_MOE, N_EXPERTS, D_MODEL, D_EXPERT), D_MODEL ** -0.5),
        "w_exp_up": nrm(27, (N_MOE, N_EXPERTS, D_MODEL, D_EXPERT), D_MODEL ** -0.5),
        "w_exp_down": nrm(28, (N_MOE, N_EXPERTS, D_EXPERT, D_MODEL), D_EXPERT ** -0.5),
        "g_final": 1.0 + nrm(29, (D_MODEL,), 0.02),
    }


def reference(x, c, ctx, c_ctx, w_mod, b_mod, g_attn_norm, w_in, attn_sink, ssm_a_re, ssm_a_im,
              ssm_log_dt, ssm_b_re, ssm_b_im, ssm_c_re, ssm_c_im, ssm_d, w_glu, g_out_attn,
              g_out_ssm, w_out, g_ffn_norm, w_ff_gate, w_ff_up, w_ff_down, w_router, w_exp_gate,
              w_exp_up, w_exp_down, g_final):
    n_tok = x.shape[1]
    cos, sin = axial_rope_tables(n_tok)
    xc = ctx
    for i in range(DEPTH):
        last = i == DEPTH - 1
        mx = [t[:, None, :] for t in ada_modulation(c, w_mod[i], b_mod[i])]
        mc = ada_modulation(c_ctx, w_mod[i], b_mod[i])
        hx = modulate(x, g_attn_norm[i], mx[0], mx[1])
        hc = modulate(xc, g_attn_norm[i], mc[0], mc[1])
        o_x, o_c = hybrid_mixer(hx, hc, cos, sin, w_in[i], attn_sink[i], ssm_a_re[i], ssm_a_im[i],
                                ssm_log_dt[i], ssm_b_re[i], ssm_b_im[i], ssm_c_re[i], ssm_c_im[i],
                                ssm_d[i], w_glu[i], g_out_attn[i], g_out_ssm[i], w_out[i], last)
        x = x + mx[2] * o_x
        x = x + mx[5] * channel_mixer(modulate(x, g_ffn_norm[i], mx[3], mx[4]), i, w_ff_gate, w_ff_up,
                                      w_ff_down, w_router, w_exp_gate, w_exp_up, w_exp_down)
        if not last:
            xc = xc + mc[2] * o_c
            xc = xc + mc[5] * channel_mixer(modulate(xc, g_ffn_norm[i], mc[3], mc[4]), i, w_ff_gate,
                                            w_ff_up, w_ff_down, w_router, w_exp_gate, w_exp_up,
                                            w_exp_down)
    return rms_norm(x, g_final)
```

```python
import math
import numpy as np
import ml_dtypes
import concourse.bass as bass
import concourse.mybir as mybir
from concourse.bass_utils import run_bass_kernel_spmd

F32 = mybir.dt.float32
BF16 = mybir.dt.bfloat16
I32 = mybir.dt.int32
AF = mybir.ActivationFunctionType
ALU = mybir.AluOpType
NPBF = ml_dtypes.bfloat16
NCORES = 8

D = 4096; NQH = 24; NKV = 8; HD = 128; AW = 3072; KVW = 1024; SW = 1024
INC = 6144; DFF = 11008; NE = 8; DE = 4096; CTX = 256; SEQ = 8192; B = 2
EPS = 1e-6; SCALE = HD ** -0.5
TCH = 32
NCH = (SEQ + CTX) // TCH


class Buf:
    __slots__ = ("name", "w", "r", "dsem", "dcnt")

    def __init__(self, name):
        self.name = name; self.w = None; self.r = {}; self.dsem = None; self.dcnt = 0


class T:
    def __init__(self, t, b):
        self.t = t; self.b = b

    def __getitem__(self, k):
        return self.t[k]


class Prog:
    def __init__(self):
        self.nc = bass.Bass("TRN2", target_bir_lowering=False)
        nc = self.nc
        self.engs = {"pe": nc.tensor, "dve": nc.vector, "act": nc.scalar, "pool": nc.gpsimd, "sp": nc.sync}
        self.esem = {k: nc.semaphore("es_" + k).__enter__() for k in self.engs}
        self.ecnt = {k: 0 for k in self.engs}
        self.seen = {k: {} for k in self.engs}
        self.pend = {k: ([], []) for k in self.engs}
        self.nsem = len(self.engs)
        self.out_toks = []
        self.uid = 0

    def sb(self, name, shape, dt):
        self.uid += 1
        return T(self.nc.sbuf_tensor(f"{name}_{self.uid}", list(shape), dt).__enter__(), Buf(name))

    def ps(self, name, shape, dt=F32):
        self.uid += 1
        return T(self.nc.psum_tensor(f"{name}_{self.uid}", list(shape), dt).__enter__(), Buf(name))

    def dram(self, name, shape, dt, kind="Internal"):
        return T(self.nc.dram_tensor(name, list(shape), dt, kind=kind).ap(), Buf(name))

    def _wait(self, e, tok):
        sem, val = tok
        if self.seen[e].get(id(sem), 0) >= val:
            return
        self.engs[e].wait_ge(sem, val)
        self.seen[e][id(sem)] = val

    def _deps(self, e, reads, writes):
        toks = []
        for b in reads:
            if b.w is not None:
                toks.append(b.w)
        for b in writes:
            if b.w is not None:
                toks.append(b.w)
            toks.extend(b.r.values())
        for t in toks:
            if t[0] is self.esem[e] and e == "pe":
                continue
            self._wait(e, t)

    def _commit(self, tok, reads, writes):
        for b in writes:
            b.w = tok; b.r = {}
        for b in reads:
            k = id(tok[0])
            if k not in b.r or b.r[k][1] < tok[1]:
                b.r[k] = tok

    def op(self, e, fn, reads=(), writes=(), track=True):
        reads = [x.b if isinstance(x, T) else x for x in reads]
        writes = [x.b if isinstance(x, T) else x for x in writes]
        self._deps(e, reads, writes)
        ins = fn(self.engs[e])
        pr, pw = self.pend[e]
        if not track:
            pr.extend(reads); pw.extend(writes)
            return ins
        self.ecnt[e] += 1
        ins.then_inc(self.esem[e], 1)
        tok = (self.esem[e], self.ecnt[e])
        self._commit(tok, reads + pr, writes + pw)
        self.pend[e] = ([], [])
        return ins

    def dma(self, q, out, in_, reads=(), writes=(), sem_buf=None, is_output=False, **kw):
        reads = [x.b if isinstance(x, T) else x for x in reads]
        writes = [x.b if isinstance(x, T) else x for x in writes]
        self._deps(q, reads, writes)
        sb = sem_buf.b if isinstance(sem_buf, T) else sem_buf
        if sb.dsem is None:
            sb.dsem = self.nc.semaphore("ds_%d" % self.nsem).__enter__(); self.nsem += 1
        sb.dcnt += 16
        ins = self.engs[q].dma_start(out=out, in_=in_, **kw)
        ins.then_inc(sb.dsem, 16)
        tok = (sb.dsem, sb.dcnt)
        self._commit(tok, reads, writes)
        if is_output:
            self.out_toks.append(tok)
        return ins

    def finish(self):
        if getattr(self, "_fin", False):
            return
        self._fin = True
        last = {}
        for sem, val in self.out_toks:
            if id(sem) not in last or last[id(sem)][1] < val:
                last[id(sem)] = (sem, val)
        for tok in last.values():
            self._wait("sp", tok)


def run(prog, in_maps):
    prog.finish()
    res = run_bass_kernel_spmd(prog.nc, in_maps, core_ids=list(range(NCORES)))
    return res.results


def build_mod(ncol):
    P = Prog()
    KT = D // 128; MT = ncol // 128
    cv = P.dram("cv", [3, D], F32, "ExternalInput")
    w = P.dram("w", [D, ncol], F32, "ExternalInput")
    bb = P.dram("b", [ncol], F32, "ExternalInput")
    out = P.dram("out", [128, MT, 3], F32, "ExternalOutput")
    cT = P.sb("cT", [128, KT, 3], F32); cS = P.sb("cS", [128, KT, 3], BF16)
    bt = P.sb("bt", [128, MT], F32); ot = P.sb("ot", [128, MT, 3], F32)
    wt = [P.sb("wt%d" % i, [128, KT, 128], BF16) for i in range(3)]
    pp = [P.ps("pp%d" % i, [128, 512]) for i in range(2)]
    for j in range(3):
        P.dma("sp", cT[:, :, j], cv[j].rearrange("(kt p) -> p kt", p=128), writes=[cT], sem_buf=cT,
              allow_slow_non_contiguous=True)
    P.dma("sp", bt[:], bb.t.rearrange("(m p) -> p m", p=128), writes=[bt], sem_buf=bt, allow_slow_non_contiguous=True)
    P.op("act", lambda e: e.activation(out=cS[:], in_=cT[:], func=AF.Silu), reads=[cT], writes=[cS])
    for m in range(MT):
        wb = wt[m % 3]; pt = pp[m % 2]
        P.dma("pool", wb[:], w[:, m * 128:(m + 1) * 128].rearrange("(kt p) m -> p kt m", p=128), writes=[wb], sem_buf=wb)
        for kt in range(KT):
            P.op("pe", lambda e: e.matmul(pt[:, 0:3], lhsT=wb[:, kt, :], rhs=cS[:, kt, :], start=(kt == 0), stop=(kt == KT - 1)),
                 reads=[wb, cS], writes=[pt], track=(kt == KT - 1))
        P.op("dve", lambda e: e.tensor_scalar(out=ot[:, m, :], in0=pt[:, 0:3], scalar1=bt[:, m:m + 1], scalar2=None, op0=ALU.add),
             reads=[pt, bt], writes=[ot])
    P.dma("sp", out[:], ot[:], reads=[ot], writes=[out], sem_buf=ot, is_output=True)
    return P


def run_mod(c, c_ctx, w_mod, b_mod):
    depth = w_mod.shape[0]
    ncol = depth * 6 * D // NCORES
    cv = np.ascontiguousarray(np.concatenate([c, c_ctx[None]], 0))
    wall = w_mod.transpose(1, 0, 2).reshape(D, depth * 6 * D)
    ball = b_mod.reshape(-1)
    P = build_mod(ncol)
    ims = [{"cv": cv, "w": np.ascontiguousarray(wall[:, i * ncol:(i + 1) * ncol]),
            "b": np.ascontiguousarray(ball[i * ncol:(i + 1) * ncol])} for i in range(NCORES)]
    res = run(P, ims)
    o = np.concatenate([r["out"].transpose(1, 0, 2).reshape(ncol, 3) for r in res], 0)
    return o.reshape(depth, 6, D, 3)


def load_cols(P, dst, src_vec, q="sp"):
    P.dma(q, dst[:], src_vec.rearrange("(kt p) -> p kt", p=128), writes=[dst], sem_buf=dst, allow_slow_non_contiguous=True)


def rstd_from_ssq(P, ssq_ps, rstd, tmp, n, fdim):
    P.op("act", lambda e: e.activation(out=tmp[:, :n], in_=ssq_ps[:, :n], func=AF.Sqrt, scale=1.0 / fdim, bias=EPS),
         reads=[ssq_ps], writes=[tmp])
    P.op("dve", lambda e: e.reciprocal(out=rstd[:, :n], in_=tmp[:, :n]), reads=[tmp], writes=[rstd])


def build_pre(nlat, nctx, TT=512):
    P = Prog()
    NT = nlat + nctx; KT = D // 128; MT = INC // 128
    xT = P.dram("xT", [D, NT], F32, "ExternalInput")
    w = P.dram("w", [D, INC], F32, "ExternalInput")
    modv = P.dram("modv", [2, 2, D], F32, "ExternalInput")
    gvec = P.dram("g", [D], F32, "ExternalInput")
    cosd = P.dram("cosT", [128, nlat], F32, "ExternalInput")
    sind = P.dram("sinT", [128, nlat], F32, "ExternalInput")
    rmat = P.dram("rmat", [128, 128], F32, "ExternalInput")
    qr = P.dram("qr", [128, NQH, nlat], BF16, "ExternalOutput")
    qp = P.dram("qp", [128, NQH, NT], BF16, "ExternalOutput")
    kk = P.dram("kk", [128, NKV, NT], BF16, "ExternalOutput")
    vv = P.dram("vv", [128, NKV, NT], BF16, "ExternalOutput")
    uu = P.dram("uu", [128, SW // 128, NT], F32, "ExternalOutput")

    ones = P.sb("ones", [128, 128], BF16)
    P.op("dve", lambda e: e.memset(ones[:], 1.0), writes=[ones])
    R = P.sb("R", [128, 128], BF16)
    P.dma("pool", R[:], rmat[:, :], writes=[R], sem_buf=R)
    cosT = P.sb("cos", [128, nlat], F32); sinT = P.sb("sin", [128, nlat], F32)
    P.dma("sp", cosT[:], cosd[:, :], writes=[cosT], sem_buf=cosT)
    P.dma("sp", sinT[:], sind[:, :], writes=[sinT], sem_buf=sinT)
    gt = P.sb("gt", [128, KT], F32); load_cols(P, gt, gvec.t)
    gs = []; sh = []
    for j in range(2):
        s_ = P.sb("sh%d" % j, [128, KT], F32); load_cols(P, s_, modv[j, 0])
        c_ = P.sb("sc%d" % j, [128, KT], F32); load_cols(P, c_, modv[j, 1])
        g_ = P.sb("gs%d" % j, [128, KT], F32)
        P.op("dve", lambda e: e.scalar_tensor_tensor(out=g_[:], in0=c_[:], scalar=1.0, in1=gt[:], op0=ALU.add, op1=ALU.mult),
             reads=[c_, gt], writes=[g_])
        gs.append(g_); sh.append(s_)

    xall = P.sb("xall", [128, KT, TT], F32)
    xk = [Buf("xk%d" % i) for i in range(KT)]
    sq = [P.sb("sq%d" % i, [128, TT], BF16) for i in range(2)]
    hT = P.sb("hT", [128, KT, TT], BF16)
    hk = [Buf("hk%d" % i) for i in range(KT)]
    tmpn = [P.sb("tmpn%d" % i, [128, TT], F32) for i in range(2)]
    rstd = P.sb("rstd", [128, TT], F32); rtmp = P.sb("rtmp", [128, TT], F32)
    wt = [P.sb("wt%d" % i, [128, KT, 128], BF16) for i in range(3)]
    ssq = P.ps("ssq", [128, 512])
    acc = [P.ps("acc%d" % i, [128, 512]) for i in range(2)]
    rot = [P.ps("rot%d" % i, [128, 512]) for i in range(2)]
    ob = [P.sb("ob%d" % i, [128, TT], BF16) for i in range(3)]
    of = [P.sb("of%d" % i, [128, TT], F32) for i in range(2)]
    t1 = [P.sb("t1_%d" % i, [128, TT], F32) for i in range(2)]
    t2 = [P.sb("t2_%d" % i, [128, TT], F32) for i in range(2)]
    orr = [P.sb("orr%d" % i, [128, TT], BF16) for i in range(2)]

    tiles = [(c0, min(TT, nlat - c0), 0) for c0 in range(0, nlat, TT)]
    if nctx:
        tiles += [(nlat + c0, min(TT, nctx - c0), 1) for c0 in range(0, nctx, TT)]
    it = 0
    for (c0, n, isctx) in tiles:
        for kt in range(KT):
            P.dma("sp", xall[:, kt, :n], xT[kt * 128:(kt + 1) * 128, c0:c0 + n], reads=[], writes=[xk[kt]], sem_buf=xk[kt])
            s = sq[kt % 2]
            P.op("act", lambda e: e.activation(out=s[:, :n], in_=xall[:, kt, :n], func=AF.Square), reads=[xk[kt]], writes=[s])
            P.op("pe", lambda e: e.matmul(ssq[:, :n], lhsT=ones[:], rhs=s[:, :n], start=(kt == 0), stop=(kt == KT - 1)),
                 reads=[s, ones], writes=[ssq], track=True)
        rstd_from_ssq(P, ssq, rstd, rtmp, n, D)
        for kt in range(KT):
            tn = tmpn[kt % 2]
            P.op("dve", lambda e: e.tensor_tensor(out=tn[:, :n], in0=xall[:, kt, :n], in1=rstd[:, :n], op=ALU.mult),
                 reads=[xk[kt], rstd], writes=[tn])
            P.op("act", lambda e: e.activation(out=hT[:, kt, :n], in_=tn[:, :n], func=AF.Identity,
                                               scale=gs[isctx][:, kt:kt + 1], bias=sh[isctx][:, kt:kt + 1]),
                 reads=[tn, gs[isctx], sh[isctx]], writes=[hk[kt]])
        for m in range(MT):
            wb = wt[it % 3]; a = acc[it % 2]; it += 1
            P.dma("pool", wb[:], w[:, m * 128:(m + 1) * 128].rearrange("(kt p) m -> p kt m", p=128), writes=[wb], sem_buf=wb)
            for kt in range(KT):
                P.op("pe", lambda e: e.matmul(a[:, :n], lhsT=wb[:, kt, :], rhs=hT[:, kt, :n], start=(kt == 0), stop=(kt == KT - 1)),
                     reads=[wb, hk[kt]], writes=[a], track=(kt == KT - 1))
            if m < 32:
                o = ob[m % 3]
                P.op("act", lambda e: e.copy(out=o[:, :n], in_=a[:, :n]), reads=[a], writes=[o])
                isq = m < NQH
                if isq:
                    P.dma("sp", qp[:, m, c0:c0 + n], o[:, :n], reads=[o], writes=[], sem_buf=o, is_output=True)
                if isctx:
                    if not isq:
                        P.dma("sp", kk[:, m - NQH, c0:c0 + n], o[:, :n], reads=[o], writes=[], sem_buf=o, is_output=True)
                else:
                    r = rot[m % 2]; a1 = t1[m % 2]; a2 = t2[m % 2]; oo = orr[m % 2]
                    P.op("pe", lambda e: e.matmul(r[:, :n], lhsT=R[:], rhs=o[:, :n], start=True, stop=True), reads=[R, o], writes=[r])
                    P.op("pool", lambda e: e.tensor_tensor(out=a1[:, :n], in0=o[:, :n], in1=cosT[:, c0:c0 + n], op=ALU.mult),
                         reads=[o, cosT], writes=[a1])
                    P.op("dve", lambda e: e.tensor_tensor(out=a2[:, :n], in0=r[:, :n], in1=sinT[:, c0:c0 + n], op=ALU.mult),
                         reads=[r, sinT], writes=[a2])
                    P.op("dve", lambda e: e.tensor_tensor(out=oo[:, :n], in0=a2[:, :n], in1=a1[:, :n], op=ALU.add),
                         reads=[a1, a2], writes=[oo])
                    dst = qr[:, m, c0:c0 + n] if isq else kk[:, m - NQH, c0:c0 + n]
                    P.dma("sp", dst, oo[:, :n], reads=[oo], writes=[], sem_buf=oo, is_output=True)
            elif m < 40:
                o = ob[m % 3]
                P.op("act", lambda e: e.copy(out=o[:, :n], in_=a[:, :n]), reads=[a], writes=[o])
                P.dma("sp", vv[:, m - 32, c0:c0 + n], o[:, :n], reads=[o], writes=[], sem_buf=o, is_output=True)
            else:
                o = of[m % 2]
                P.op("dve", lambda e: e.tensor_copy(out=o[:, :n], in_=a[:, :n]), reads=[a], writes=[o])
                P.dma("sp", uu[:, m - 40, c0:c0 + n], o[:, :n], reads=[o], writes=[], sem_buf=o, is_output=True)
    return P


def rope_consts(tok0, n):
    t = np.arange(tok0, tok0 + n)
    row = (t // 64).astype(np.float32); col = (t % 64).astype(np.float32)
    inv = (10000.0 ** (-np.arange(0, 64, 2, dtype=np.float32) / 64)).astype(np.float32)
    ar = row[:, None] * inv; ac = col[:, None] * inv
    ang = np.concatenate([ar, ar, ac, ac], -1)
    cosT = np.ascontiguousarray(np.cos(ang).T.astype(np.float32)); sinT = np.ascontiguousarray(np.sin(ang).T.astype(np.float32))
    Rm = np.zeros((128, 128), np.float32)
    for h0 in (0, 64):
        for d in range(32):
            Rm[h0 + d + 32, h0 + d] = -1.0
            Rm[h0 + d, h0 + d + 32] = 1.0
    return cosT, sinT, Rm


def build_attn(NB, NCQ):
    P = Prog()
    NL = NB * 128; NQ = NL + NCQ; NKB = NB + 2; NCB = CTX // 128
    qr = P.dram("qr", [128, NQH, NL], BF16, "ExternalInput")
    qp = P.dram("qp", [128, NQH, NQ], BF16, "ExternalInput")
    kr = P.dram("kr", [128, NKV, NKB * 128], BF16, "ExternalInput")
    vv = P.dram("v", [128, NKB, KVW], BF16, "ExternalInput")
    kc = P.dram("kc", [128, NKV, CTX], BF16, "ExternalInput")
    vc = P.dram("vc", [128, NCB, KVW], BF16, "ExternalInput")
    mk = P.dram("mask", [128, 4, 128], BF16, "ExternalInput")
    snk = P.dram("sink", [NQH], F32, "ExternalInput")
    oT = P.dram("oT", [128, NQH, NQ], F32, "ExternalOutput")

    ones = P.sb("ones", [128, 128], BF16)
    P.op("dve", lambda e: e.memset(ones[:], 1.0), writes=[ones])
    KR = P.sb("KR", [128, NKV, NKB * 128], BF16); P.dma("sp", KR[:], kr[:], writes=[KR], sem_buf=KR)
    VV = P.sb("VV", [128, NKB, KVW], BF16); P.dma("sp", VV[:], vv[:], writes=[VV], sem_buf=VV)
    KC = P.sb("KC", [128, NKV, CTX], BF16); P.dma("sp", KC[:], kc[:], writes=[KC], sem_buf=KC)
    VC = P.sb("VC", [128, NCB, KVW], BF16); P.dma("sp", VC[:], vc[:], writes=[VC], sem_buf=VC)
    MK = P.sb("MK", [128, 4, 128], BF16); P.dma("sp", MK[:], mk[:], writes=[MK], sem_buf=MK)
    es0 = P.sb("es0", [128, NQH], F32); es = P.sb("es", [128, NQH], F32)
    P.dma("sp", es0[:], snk.t.partition_broadcast(128), writes=[es0], sem_buf=es0, allow_slow_non_contiguous=True)
    P.op("act", lambda e: e.activation(out=es[:], in_=es0[:], func=AF.Exp), reads=[es0], writes=[es])

    QR = [P.sb("QR%d" % i, [128, 3, 128], BF16) for i in range(2)]
    QP = [P.sb("QP%d" % i, [128, 3, 128], BF16) for i in range(2)]
    sps = [P.ps("sps%d" % i, [128, 512]) for i in range(3)]
    ops_ = [P.ps("ops%d" % i, [128, 512]) for i in range(2)]
    dps = [P.ps("dps%d" % i, [128, 512]) for i in range(2)]
    pt = [P.sb("pt%d" % i, [128, 3, 128], BF16) for i in range(4)]
    dn = [P.sb("dn%d" % i, [128, 3, 128], F32) for i in range(2)]
    oo = [P.sb("oo%d" % i, [128, 3, 128], F32) for i in range(2)]
    it = 0; ip = 0
    blocks = [(n, 128, False) for n in range(NB)] + ([(NB, NCQ, True)] if NCQ else [])
    for (n, nq, isctx) in blocks:
        c0 = n * 128; N3 = 3 * nq
        for h in range(NKV):
            Qp_ = QP[it % 2]; Qr_ = QR[it % 2]
            P.dma("sp", Qp_[:, :, :nq], qp[:, 3 * h:3 * h + 3, c0:c0 + nq], writes=[Qp_], sem_buf=Qp_)
            if not isctx:
                P.dma("sp", Qr_[:, :, :nq], qr[:, 3 * h:3 * h + 3, c0:c0 + nq], writes=[Qr_], sem_buf=Qr_)
            kbs = [] if isctx else [("w", n + j, (None, (2 if n == 0 else 0), None, None)[1] if j == 0 else None) for j in range(3)]
            if not isctx:
                kbs = [("w", n, 2 if n == 0 else 0), ("w", n + 1, None), ("w", n + 2, 3 if n == NB - 1 else 1)]
            kbs += [("c", c, None) for c in range(NCB)]
            O = ops_[it % 2]; Dn = dps[it % 2]
            for bi, (kind, kb, mi) in enumerate(kbs):
                S = sps[ip % 3]; Pt = pt[ip % 4]; ip += 1
                if kind == "w":
                    P.op("pe", lambda e: e.matmul(S[:, :N3], lhsT=KR[:, h, kb * 128:(kb + 1) * 128], rhs=Qr_[:, :, :nq], start=True, stop=True),
                         reads=[KR, Qr_], writes=[S])
                else:
                    P.op("pe", lambda e: e.matmul(S[:, :N3], lhsT=KC[:, h, kb * 128:(kb + 1) * 128], rhs=Qp_[:, :, :nq], start=True, stop=True),
                         reads=[KC, Qp_], writes=[S])
                P.op("act", lambda e: e.activation(out=Pt[:, :, :nq], in_=S[:, :N3].rearrange("p (a b) -> p a b", a=3), func=AF.Exp, scale=SCALE),
                     reads=[S], writes=[Pt])
                if mi is not None:
                    P.op("pool", lambda e: e.tensor_tensor(out=Pt[:, :, :nq], in0=Pt[:, :, :nq],
                                                           in1=MK[:, mi:mi + 1, :nq].broadcast_to([128, 3, nq]), op=ALU.mult),
                         reads=[Pt, MK], writes=[Pt])
                vsrc = VV[:, kb, h * 128:(h + 1) * 128] if kind == "w" else VC[:, kb, h * 128:(h + 1) * 128]
                last = bi == len(kbs) - 1
                P.op("pe", lambda e: e.matmul(O[:, :N3], lhsT=vsrc, rhs=Pt[:, :, :nq], start=(bi == 0), stop=last),
                     reads=[VV, VC, Pt], writes=[O], track=last)
                P.op("pe", lambda e: e.matmul(Dn[:, :N3], lhsT=ones[:], rhs=Pt[:, :, :nq], start=(bi == 0), stop=last),
                     reads=[ones, Pt], writes=[Dn], track=True)
            d_ = dn[it % 2]; o_ = oo[it % 2]
            P.op("dve", lambda e: e.tensor_tensor(out=d_[:, :, :nq], in0=Dn[:, :N3].rearrange("p (a b) -> p a b", a=3),
                                                  in1=es[:, 3 * h:3 * h + 3].unsqueeze(2).broadcast_to([128, 3, nq]), op=ALU.add),
                 reads=[Dn, es], writes=[d_])
            P.op("dve", lambda e: e.reciprocal(out=d_[:, :, :nq], in_=d_[:, :, :nq]), reads=[d_], writes=[d_])
            P.op("dve", lambda e: e.tensor_tensor(out=o_[:, :, :nq], in0=O[:, :N3].rearrange("p (a b) -> p a b", a=3), in1=d_[:, :, :nq], op=ALU.mult),
                 reads=[O, d_], writes=[o_])
            P.dma("sp", oT[:, 3 * h:3 * h + 3, c0:c0 + nq], o_[:, :, :nq], reads=[o_], sem_buf=o_, is_output=True)
            it += 1
    return P


def attn_masks(first, last):
    m = np.arange(128)[:, None]; i = np.arange(128)[None, :]
    L = (i <= m).astype(np.float32); R = (m <= i).astype(np.float32)
    Z = np.zeros_like(L)
    return np.stack([L, R, Z if first else L, Z if last else R], 1).astype(NPBF)


def build_s5(NG=8, with_ctx=True):
    P = Prog()
    NGD = 2 * NG; NCC = CTX // TCH; NXC = SEQ // TCH; NJ = NCH; NU = NCH + NCC; NS = NG * B
    TWO_PI = 2.0 * math.pi
    U_d = P.dram("U", [128, 4, NG, B, NU], F32, "ExternalInput")
    are_d = P.dram("a_re", [2, NG, 64], F32, "ExternalInput"); aim_d = P.dram("a_im", [2, NG, 64], F32, "ExternalInput")
    ldt_d = P.dram("log_dt", [2 * NG], F32, "ExternalInput")
    bre_d = P.dram("b_re", [2, NG, 64, 16], F32, "ExternalInput"); bim_d = P.dram("b_im", [2, NG, 64, 16], F32, "ExternalInput")
    cre_d = P.dram("c_re", [2, NG, 16, 64], F32, "ExternalInput"); cim_d = P.dram("c_im", [2, NG, 16, 64], F32, "ExternalInput")
    msk_d = P.dram("msk", [128, 2, 4, 512], BF16, "ExternalInput")
    idn_d = P.dram("idn", [64, 64], F32, "ExternalInput")
    y_d = P.dram("y", [128, 4, NG, B, NXC + NCC], F32, "ExternalOutput")
    NP = 64
    cnt = [0]

    def tl(shape, dt=F32, name="t"):
        cnt[0] += 1
        return P.sb("%s%d" % (name, cnt[0]), [NP] + list(shape), dt)

    def tt(o, a, b, op, e="dve"):
        P.op(e, lambda en: en.tensor_tensor(out=o[:], in0=a[:], in1=b[:], op=op), reads=[a, b], writes=[o]); return o

    def ts(o, a, s1, op0, s2=None, op1=None, e="dve"):
        if op1 is None:
            P.op(e, lambda en: en.tensor_scalar(out=o[:], in0=a[:], scalar1=s1, scalar2=None, op0=op0), reads=[a], writes=[o])
        else:
            P.op(e, lambda en: en.tensor_scalar(out=o[:], in0=a[:], scalar1=s1, scalar2=s2, op0=op0, op1=op1), reads=[a], writes=[o])
        return o

    def act(o, a, func, **kw):
        P.op("act", lambda en: en.activation(out=o[:], in_=a[:], func=func, **kw), reads=[a], writes=[o]); return o

    are = tl([NGD]); aim = tl([NGD]); ldt = tl([NGD])
    P.dma("sp", are[:], are_d.t.rearrange("d g p -> p (d g)"), writes=[are], sem_buf=are, allow_slow_non_contiguous=True)
    P.dma("sp", aim[:], aim_d.t.rearrange("d g p -> p (d g)"), writes=[aim], sem_buf=aim, allow_slow_non_contiguous=True)
    P.dma("sp", ldt[:], ldt_d.t.partition_broadcast(NP), writes=[ldt], sem_buf=ldt, allow_slow_non_contiguous=True)
    bre = tl([NGD, 16]); bim = tl([NGD, 16]); cre = tl([NGD, 16]); cim = tl([NGD, 16])
    P.dma("sp", bre[:], bre_d.t.rearrange("d g p c -> p (d g) c"), writes=[bre], sem_buf=bre, allow_slow_non_contiguous=True)
    P.dma("sp", bim[:], bim_d.t.rearrange("d g p c -> p (d g) c"), writes=[bim], sem_buf=bim, allow_slow_non_contiguous=True)
    P.dma("sp", cre[:], cre_d.t.rearrange("d g c p -> p (d g) c"), writes=[cre], sem_buf=cre, allow_slow_non_contiguous=True)
    P.dma("sp", cim[:], cim_d.t.rearrange("d g c p -> p (d g) c"), writes=[cim], sem_buf=cim, allow_slow_non_contiguous=True)
    idn = P.sb("idn", [NP, 64], BF16); P.dma("pool", idn[:], idn_d[:, :], writes=[idn], sem_buf=idn)
    msk = P.sb("msk", [128, 2, 4, 512], BF16); P.dma("sp", msk[:], msk_d[:], writes=[msk], sem_buf=msk)

    dt_ = act(tl([NGD]), ldt, AF.Exp)
    xr = tt(tl([NGD]), are, dt_, ALU.mult); th = tt(tl([NGD]), aim, dt_, ALU.mult)
    mag = act(tl([NGD]), xr, AF.Exp)

    def sin_of(ang):
        q = ts(tl([NGD]), ang, 1.0 / TWO_PI, ALU.mult)
        qi = tl([NGD], I32)
        P.op("dve", lambda en: en.tensor_copy(out=qi[:], in_=q[:]), reads=[q], writes=[qi])
        qf = tl([NGD])
        P.op("dve", lambda en: en.tensor_copy(out=qf[:], in_=qi[:]), reads=[qi], writes=[qf])
        r = tl([NGD])
        P.op("dve", lambda en: en.scalar_tensor_tensor(out=r[:], in0=qf[:], scalar=-TWO_PI, in1=ang[:], op0=ALU.mult, op1=ALU.add),
             reads=[qf, ang], writes=[r])
        hi = ts(tl([NGD]), r, math.pi, ALU.is_gt, -TWO_PI, ALU.mult)
        lo = ts(tl([NGD]), r, -math.pi, ALU.is_lt, TWO_PI, ALU.mult)
        r2 = tt(tl([NGD]), r, hi, ALU.add); r3 = tt(tl([NGD]), r2, lo, ALU.add)
        r4 = ts(tl([NGD]), r3, math.pi, ALU.min, -math.pi, ALU.max)
        return act(tl([NGD]), r4, AF.Sin)

    sn = sin_of(th)
    thc = ts(tl([NGD]), th, math.pi / 2, ALU.add)
    cs = sin_of(thc)
    abr = tt(tl([NGD]), mag, cs, ALU.mult); abi = tt(tl([NGD]), mag, sn, ALU.mult)
    nr = ts(tl([NGD]), abr, -1.0, ALU.add)
    den = tt(tl([NGD]), tt(tl([NGD]), are, are, ALU.mult), tt(tl([NGD]), aim, aim, ALU.mult), ALU.add)
    rden = tl([NGD]); P.op("dve", lambda en: en.reciprocal(out=rden[:], in_=den[:]), reads=[den], writes=[rden])
    cfr = tt(tl([NGD]), tt(tl([NGD]), tt(tl([NGD]), nr, are, ALU.mult), tt(tl([NGD]), abi, aim, ALU.mult), ALU.add), rden, ALU.mult)
    cfi = tt(tl([NGD]), tt(tl([NGD]), tt(tl([NGD]), abi, are, ALU.mult), tt(tl([NGD]), nr, aim, ALU.mult), ALU.subtract), rden, ALU.mult)

    def cmul_b(va_r, va_i, vb_r, vb_i, rd, tmp, re, im, neg_im=False):
        m1, m2 = tmp
        P.op("dve", lambda en: en.tensor_tensor(out=m1[:], in0=va_r, in1=vb_r, op=ALU.mult), reads=rd, writes=[m1])
        P.op("dve", lambda en: en.tensor_tensor(out=m2[:], in0=va_i, in1=vb_i, op=ALU.mult), reads=rd, writes=[m2])
        P.op("dve", lambda en: en.tensor_tensor(out=re[:], in0=m1[:], in1=m2[:], op=ALU.subtract), reads=[m1, m2], writes=[re])
        P.op("dve", lambda en: en.tensor_tensor(out=m1[:], in0=va_r, in1=vb_i, op=ALU.mult), reads=rd, writes=[m1])
        P.op("dve", lambda en: en.tensor_tensor(out=m2[:], in0=va_i, in1=vb_r, op=ALU.mult), reads=rd, writes=[m2])
        P.op("dve", lambda en: en.tensor_tensor(out=im[:], in0=m1[:], in1=m2[:], op=ALU.add), reads=[m1, m2], writes=[im])
        if neg_im:
            P.op("dve", lambda en: en.tensor_scalar(out=im[:], in0=im[:], scalar1=-1.0, scalar2=None, op0=ALU.mult), reads=[im], writes=[im])

    Bbr = tl([NGD, 16]); Bbi = tl([NGD, 16]); tb1 = tl([NGD, 16]); tb2 = tl([NGD, 16])
    cmul_b(cfr[:].unsqueeze(2).broadcast_to([NP, NGD, 16]), cfi[:].unsqueeze(2).broadcast_to([NP, NGD, 16]), bre[:], bim[:],
           [cfr, cfi, bre, bim], (tb1, tb2), Bbr, Bbi)
    pwr = tl([NGD, TCH + 1]); pwi = tl([NGD, TCH + 1])
    P.op("dve", lambda en: en.memset(pwr[:], 1.0), writes=[pwr]); P.op("dve", lambda en: en.memset(pwi[:], 0.0), writes=[pwi])
    m1 = tl([NGD]); m2 = tl([NGD]); m3 = tl([NGD]); m4 = tl([NGD])
    for k in range(1, TCH + 1):
        P.op("dve", lambda en: en.tensor_tensor(out=m1[:], in0=pwr[:, :, k - 1], in1=abr[:], op=ALU.mult), reads=[pwr, abr], writes=[m1])
        P.op("dve", lambda en: en.tensor_tensor(out=m2[:], in0=pwi[:, :, k - 1], in1=abi[:], op=ALU.mult), reads=[pwi, abi], writes=[m2])
        P.op("dve", lambda en: en.tensor_tensor(out=m3[:], in0=pwr[:, :, k - 1], in1=abi[:], op=ALU.mult), reads=[pwr, abi], writes=[m3])
        P.op("dve", lambda en: en.tensor_tensor(out=m4[:], in0=pwi[:, :, k - 1], in1=abr[:], op=ALU.mult), reads=[pwi, abr], writes=[m4])
        P.op("dve", lambda en: en.tensor_tensor(out=pwr[:, :, k], in0=m1[:], in1=m2[:], op=ALU.subtract), reads=[m1, m2], writes=[pwr])
        P.op("dve", lambda en: en.tensor_tensor(out=pwi[:, :, k], in0=m3[:], in1=m4[:], op=ALU.add), reads=[m3, m4], writes=[pwi])
    mm = tt(tl([NGD, TCH + 1]), tt(tl([NGD, TCH + 1]), pwr, pwr, ALU.mult), tt(tl([NGD, TCH + 1]), pwi, pwi, ALU.mult), ALU.add)
    rm = tl([NGD, TCH + 1]); P.op("dve", lambda en: en.reciprocal(out=rm[:], in_=mm[:]), reads=[mm], writes=[rm])
    ipr = tt(tl([NGD, TCH + 1]), pwr, rm, ALU.mult)
    ipi = tt(tl([NGD, TCH + 1]), pwi, rm, ALU.mult); ts(ipi, ipi, -1.0, ALU.mult)
    E1r = tl([NGD, TCH]); E1i = tl([NGD, TCH]); E2r = tl([NGD, TCH]); E2i = tl([NGD, TCH])
    for (dst, f_src, b_src) in ((E1r, ipr, pwr), (E1i, ipi, pwi), (E2r, pwr, ipr), (E2i, pwi, ipi)):
        P.op("dve", lambda en: en.tensor_copy(out=dst[:, 0:NG, :], in_=f_src[:, 0:NG, 0:TCH]), reads=[f_src], writes=[dst])
        P.op("dve", lambda en: en.tensor_copy(out=dst[:, NG:NGD, :], in_=b_src[:, NG:NGD, 0:TCH]), reads=[b_src], writes=[dst])
    aTr = tl([NG, B]); aTi = tl([NG, B]); aTrb = tl([NG, B]); aTib = tl([NG, B])
    for (dst, src, lo_) in ((aTr, pwr, 0), (aTi, pwi, 0), (aTrb, pwr, NG), (aTib, pwi, NG)):
        P.op("dve", lambda en: en.tensor_copy(out=dst[:], in_=src[:, lo_:lo_ + NG, TCH:TCH + 1].broadcast_to([NP, NG, B])), reads=[src], writes=[dst])

    shp = [2, TCH, 16]
    gtmp = (tl(shp), tl(shp)); gre = tl(shp); gim = tl(shp)
    X1g = [P.sb("X1g%d" % i, [NP, 2, 2, TCH * 16], BF16) for i in range(2)]
    X2g = [P.sb("X2g%d" % i, [NP, 2, 2, TCH * 16], BF16) for i in range(2)]

    def dv(t_, g):
        return t_[:].rearrange("p (d g) n -> p d g n", d=2)[:, :, g, :]

    def gen_group(g, slot, need2):
        x1 = X1g[slot]; x2 = X2g[slot]
        e_b = lambda t_: dv(t_, g).unsqueeze(3).broadcast_to([NP] + shp)
        c_b = lambda t_: dv(t_, g).unsqueeze(2).broadcast_to([NP] + shp)
        cmul_b(e_b(E1r), e_b(E1i), c_b(Bbr), c_b(Bbi), [E1r, E1i, Bbr, Bbi], gtmp, gre, gim)
        P.op("act", lambda en: en.copy(out=x1[:, 0], in_=gre[:].rearrange("p d s c -> p d (s c)")), reads=[gre], writes=[x1])
        P.op("act", lambda en: en.copy(out=x1[:, 1], in_=gim[:].rearrange("p d s c -> p d (s c)")), reads=[gim], writes=[x1])
        if need2:
            cmul_b(e_b(E2r), e_b(E2i), c_b(cre), c_b(cim), [E2r, E2i, cre, cim], gtmp, gre, gim, neg_im=True)
            P.op("act", lambda en: en.copy(out=x2[:, 0], in_=gre[:].rearrange("p d s c -> p d (s c)")), reads=[gre], writes=[x2])
            P.op("act", lambda en: en.copy(out=x2[:, 1], in_=gim[:].rearrange("p d s c -> p d (s c)")), reads=[gim], writes=[x2])
        return x1, x2

    gps = [P.ps("gps%d" % i, [128, 512]) for i in range(3)]
    tps = [P.ps("tps%d" % i, [128, 512]) for i in range(2)]
    X1T = [P.sb("X1T%d" % i, [128, 2, 4, 2, 64], BF16) for i in range(2)]
    Ub = [P.sb("Ub%d" % i, [128, 4, NU], BF16) for i in range(4)]
    GH = {}
    for d in range(2):
        for c in range(2):
            GH[(d, c)] = P.sb("GH%d%d" % (d, c), [NP, NS, NJ + 1], F32)
            zc = 0 if d == 0 else NJ
            P.op("dve", lambda e: e.memset(GH[(d, c)][:, :, zc:zc + 1], 0.0), writes=[GH[(d, c)]])
    i_ = 0; iu = 0
    for g in range(NG):
        x1, _ = gen_group(g, g % 2, False)
        xt = X1T[g % 2]
        for d in range(2):
            for kt in range(4):
                t_ = tps[i_ % 2]; i_ += 1
                ks = slice(kt * 128, (kt + 1) * 128)
                for c in range(2):
                    P.op("pe", lambda e: e.matmul(t_[:, c * 64:(c + 1) * 64], lhsT=x1[:, c, d, ks], rhs=idn[:], start=True, stop=True), reads=[x1, idn], writes=[t_])
                P.op("act", lambda e: e.copy(out=xt[:, d, kt, :, :], in_=t_[:, 0:128].rearrange("p (a b) -> p a b", a=2)), reads=[t_], writes=[xt])
        for b in range(B):
            s = g * B + b
            ub = Ub[iu % 4]; iu += 1
            P.dma("pool", ub[:], U_d[:, :, g, b, :], writes=[ub], sem_buf=ub)
            for d in range(2):
                j0 = 0 if d == 0 else NCC; o0 = 1 if d == 0 else 0
                for c in range(2):
                    g_ = gps[i_ % 3]; i_ += 1
                    for kt in range(4):
                        P.op("pe", lambda e: e.matmul(g_[0:64, :NJ], lhsT=xt[:, d, kt, c, :], rhs=ub[:, kt, j0:j0 + NJ], start=(kt == 0), stop=(kt == 3)),
                             reads=[xt, ub], writes=[g_], track=(kt == 3))
                    P.op("act", lambda e: e.copy(out=GH[(d, c)][:, s, o0:o0 + NJ], in_=g_[0:64, :NJ]), reads=[g_], writes=[GH[(d, c)]])

    def rec(eng, Hr, Hi, ar_, ai_, j_in, j_io, tmps):
        tr, ti, q1, q2 = tmps
        av = ar_[:].rearrange("p g b -> p (g b)"); aiv = ai_[:].rearrange("p g b -> p (g b)")
        P.op(eng, lambda e: e.tensor_tensor(out=tr[:], in0=Hr[:, :, j_in], in1=Hr[:, :, j_io], op=ALU.add), reads=[Hr], writes=[tr])
        P.op(eng, lambda e: e.tensor_tensor(out=ti[:], in0=Hi[:, :, j_in], in1=Hi[:, :, j_io], op=ALU.add), reads=[Hi], writes=[ti])
        P.op(eng, lambda e: e.tensor_tensor(out=q1[:], in0=tr[:], in1=av, op=ALU.mult), reads=[tr, ar_], writes=[q1])
        P.op(eng, lambda e: e.tensor_tensor(out=q2[:], in0=ti[:], in1=aiv, op=ALU.mult), reads=[ti, ai_], writes=[q2])
        P.op(eng, lambda e: e.tensor_tensor(out=Hr[:, :, j_io], in0=q1[:], in1=q2[:], op=ALU.subtract), reads=[q1, q2], writes=[Hr])
        P.op(eng, lambda e: e.tensor_tensor(out=q1[:], in0=tr[:], in1=aiv, op=ALU.mult), reads=[tr, ai_], writes=[q1])
        P.op(eng, lambda e: e.tensor_tensor(out=q2[:], in0=ti[:], in1=av, op=ALU.mult), reads=[ti, ar_], writes=[q2])
        P.op(eng, lambda e: e.tensor_tensor(out=Hi[:, :, j_io], in0=q1[:], in1=q2[:], op=ALU.add), reads=[q1, q2], writes=[Hi])
    tf = [tl([NS]) for _ in range(4)]; tb = [tl([NS]) for _ in range(4)]
    for j in range(NJ):
        rec("dve", GH[(0, 0)], GH[(0, 1)], aTr, aTi, j, j + 1, tf)
        jb = NJ - 1 - j
        rec("pool", GH[(1, 0)], GH[(1, 1)], aTrb, aTib, jb + 1, jb, tb)

    A0 = [P.sb("A0_%d" % i, [128, 2, 4, 512], BF16) for i in range(2)]
    Hh = [P.sb("Hh%d" % i, [NP, 2, 2, NJ + 1], BF16) for i in range(2)]
    yo = [P.sb("yo%d" % i, [128, 4, NXC + NCC], F32) for i in range(2)]
    for g in range(NG):
        x1, x2 = gen_group(g, g % 2, True)
        a0 = A0[g % 2]
        for d in range(2):
            for kt in range(4):
                g_ = gps[i_ % 3]; i_ += 1
                ks = slice(kt * 128, (kt + 1) * 128)
                P.op("pe", lambda e: e.matmul(g_[:, :], lhsT=x1[:, 0, d, ks], rhs=x2[:, 0, d, :], start=True, stop=False), reads=[x1, x2], writes=[g_], track=False)
                P.op("pe", lambda e: e.matmul(g_[:, :], lhsT=x1[:, 1, d, ks], rhs=x2[:, 1, d, :], start=False, stop=True), reads=[x1, x2], writes=[g_])
                P.op("dve", lambda e: e.tensor_tensor(out=a0[:, d, kt, :], in0=g_[:, :], in1=msk[:, d, kt, :], op=ALU.mult), reads=[g_, msk], writes=[a0])
        for b in range(B):
            s = g * B + b; yt = yo[s % 2]; hh = Hh[s % 2]
            ub = Ub[iu % 4]; iu += 1
            P.dma("pool", ub[:], U_d[:, :, g, b, :], writes=[ub], sem_buf=ub)
            for d in range(2):
                for c in range(2):
                    P.op("act", lambda e: e.copy(out=hh[:, d, c, :], in_=GH[(d, c)][:, s, :]), reads=[GH[(d, c)]], writes=[hh])
            for mt in range(4):
                ms = slice(mt * 128, (mt + 1) * 128)
                segs = [(0, NXC, NCC, NCC, 1)]
                if with_ctx:
                    segs.append((NXC, NCC, 0, 0, NXC + 1))
                for (o0, n, u0f, hf0, hb0) in segs:
                    u0b = u0f if o0 == 0 else NCC + NXC
                    a_ = gps[i_ % 3]; i_ += 1
                    mms = []
                    for kt in range(4):
                        mms.append((a0[:, 0, kt, ms], ub[:, kt, u0f:u0f + n], [a0, ub]))
                        mms.append((a0[:, 1, kt, ms], ub[:, kt, u0b:u0b + n], [a0, ub]))
                    for c in range(2):
                        mms.append((x2[:, c, 0, ms], hh[:, 0, c, hf0:hf0 + n], [x2, hh]))
                        mms.append((x2[:, c, 1, ms], hh[:, 1, c, hb0:hb0 + n], [x2, hh]))
                    for mi, (l_, r_, rd) in enumerate(mms):
                        P.op("pe", lambda e: e.matmul(a_[:, :n], lhsT=l_, rhs=r_, start=(mi == 0), stop=(mi == len(mms) - 1)),
                             reads=rd, writes=[a_], track=(mi == len(mms) - 1))
                    P.op("act", lambda e: e.copy(out=yt[:, mt, o0:o0 + n], in_=a_[:, :n]), reads=[a_], writes=[yt])
            if not with_ctx:
                P.op("dve", lambda e: e.memset(yt[:, :, NXC:], 0.0), writes=[yt])
            P.dma("sp", y_d[:, :, g, b, :], yt[:], reads=[yt], sem_buf=yt, is_output=True)
    return P


def s5_consts():
    k = np.arange(512); n = np.arange(512)
    s = (k // 16)[:, None]; t = (n // 16)[None, :]
    mf = (t >= s).astype(np.float32); mb = (s >= t).astype(np.float32)
    msk = np.stack([mf.reshape(4, 128, 512).transpose(1, 0, 2), mb.reshape(4, 128, 512).transpose(1, 0, 2)], 1)
    return np.ascontiguousarray(msk.astype(NPBF)), np.eye(64, dtype=np.float32)


def s5_pack_u(u_x, u_c):
    st = np.concatenate([u_c, u_x, u_c], 1)
    NU = st.shape[1] // TCH
    a = st.reshape(B, NU, TCH, SW // 16, 16)
    a = a.transpose(2, 4, 3, 0, 1).reshape(TCH * 16, SW // 16, B, NU)
    a = a.reshape(4, 128, SW // 16, B, NU).transpose(1, 0, 2, 3, 4)
    return [np.ascontiguousarray(a[:, :, i * 8:(i + 1) * 8]) for i in range(NCORES)]


def s5_unpack_y(ys):
    a = np.concatenate(ys, 2)
    a = a.transpose(1, 0, 2, 3, 4).reshape(TCH, 16, SW // 16, B, -1)
    a = a.transpose(3, 4, 0, 2, 1).reshape(B, -1, SW)
    return a[:, :SEQ], a[:, SEQ:]


def token_tiles(nlat, nctx, TT):
    tiles = [(c0, min(TT, nlat - c0), 0) for c0 in range(0, nlat, TT)]
    if nctx:
        tiles += [(nlat + c0, min(TT, nctx - c0), 1) for c0 in range(0, nctx, TT)]
    return tiles


def build_post(nlat, nctx, TT=512):
    P = Prog()
    NT = nlat + nctx; KA_ = AW // 128; KS = SW // 128; KT = D // 128
    oT = P.dram("oT", [128, KA_, NT], F32, "ExternalInput")
    yT = P.dram("yT", [128, KS, NT], F32, "ExternalInput")
    uT = P.dram("uT", [128, KS, NT], F32, "ExternalInput")
    xT = P.dram("xT", [D, NT], F32, "ExternalInput")
    dsk = P.dram("dsk", [SW], F32, "ExternalInput")
    wglu = P.dram("wglu", [SW, SW], F32, "ExternalInput")
    goa = P.dram("goa", [AW], F32, "ExternalInput"); gos = P.dram("gos", [SW], F32, "ExternalInput")
    wout = P.dram("wout", [D, D], F32, "ExternalInput")
    gatev = P.dram("gatev", [2, D], F32, "ExternalInput")
    x1T = P.dram("x1T", [D, NT], F32, "ExternalOutput")

    ones = P.sb("ones", [128, 128], BF16); P.op("dve", lambda e: e.memset(ones[:], 1.0), writes=[ones])
    dskt = P.sb("dskt", [128, KS], F32); load_cols(P, dskt, dsk.t)
    goat = P.sb("goat", [128, KA_], F32); load_cols(P, goat, goa.t)
    gost = P.sb("gost", [128, KS], F32); load_cols(P, gost, gos.t)
    gat = [P.sb("gat%d" % j, [128, KT], F32) for j in range(2)]
    for j in range(2):
        load_cols(P, gat[j], gatev[j])
    OT = P.sb("OT", [128, KA_, TT], F32); ok = [Buf("ok%d" % i) for i in range(KA_)]
    YG = P.sb("YG", [128, KS, TT], F32); ygk = [Buf("ygk%d" % i) for i in range(KS)]
    YB = P.sb("YB", [128, KS, TT], BF16); ybk = [Buf("ybk%d" % i) for i in range(KS)]
    OS = P.sb("OS", [128, KS, TT], F32); osk = [Buf("osk%d" % i) for i in range(KS)]
    NTt = P.sb("NTt", [128, KT, TT], BF16); nk = [Buf("nk%d" % i) for i in range(KT)]
    yin = [P.sb("yin%d" % i, [128, TT], F32) for i in range(2)]
    uin = [P.sb("uin%d" % i, [128, TT], F32) for i in range(2)]
    e1 = [P.sb("e1_%d" % i, [128, TT], F32) for i in range(2)]
    e2 = [P.sb("e2_%d" % i, [128, TT], F32) for i in range(2)]
    e3 = [P.sb("e3_%d" % i, [128, TT], F32) for i in range(2)]
    sq = [P.sb("sq%d" % i, [128, TT], BF16) for i in range(2)]
    rs_a = P.sb("rs_a", [128, TT], F32); rs_s = P.sb("rs_s", [128, TT], F32); rtmp = P.sb("rtmp", [128, TT], F32)
    wg = [P.sb("wg%d" % i, [128, KS, 128], BF16) for i in range(2)]
    wt = [P.sb("wt%d" % i, [128, KT, 128], BF16) for i in range(3)]
    xin = [P.sb("xin%d" % i, [128, TT], F32) for i in range(3)]
    ssq = [P.ps("ssq%d" % i, [128, 512]) for i in range(2)]
    acc = [P.ps("acc%d" % i, [128, 512]) for i in range(3)]
    GC = 2.0 * math.sqrt(2.0 / math.pi)
    it = 0
    for (c0, n, isctx) in token_tiles(nlat, nctx, TT):
        for kt in range(KS):
            yi = yin[kt % 2]; ui = uin[kt % 2]; a1 = e1[kt % 2]; a2 = e2[kt % 2]; a3 = e3[kt % 2]
            P.dma("sp", yi[:, :n], yT[:, kt, c0:c0 + n], writes=[yi], sem_buf=yi)
            P.dma("sp", ui[:, :n], uT[:, kt, c0:c0 + n], writes=[ui], sem_buf=ui)
            P.op("dve", lambda e: e.scalar_tensor_tensor(out=a1[:, :n], in0=ui[:, :n], scalar=dskt[:, kt:kt + 1], in1=yi[:, :n], op0=ALU.mult, op1=ALU.add),
                 reads=[ui, yi, dskt], writes=[a1])
            P.op("act", lambda e: e.activation(out=a2[:, :n], in_=a1[:, :n], func=AF.Square), reads=[a1], writes=[a2])
            P.op("pool", lambda e: e.tensor_scalar(out=a2[:, :n], in0=a2[:, :n], scalar1=0.044715, scalar2=1.0, op0=ALU.mult, op1=ALU.add), reads=[a2], writes=[a2])
            P.op("pool", lambda e: e.tensor_tensor(out=a3[:, :n], in0=a2[:, :n], in1=a1[:, :n], op=ALU.mult), reads=[a1, a2], writes=[a3])
            P.op("act", lambda e: e.activation(out=a3[:, :n], in_=a3[:, :n], func=AF.Sigmoid, scale=GC), reads=[a3], writes=[a3])
            P.op("dve", lambda e: e.tensor_tensor(out=YG[:, kt, :n], in0=a1[:, :n], in1=a3[:, :n], op=ALU.mult), reads=[a1, a3], writes=[ygk[kt]])
            P.op("pool", lambda e: e.tensor_copy(out=YB[:, kt, :n], in_=YG[:, kt, :n]), reads=[ygk[kt]], writes=[ybk[kt]])
        for m in range(KS):
            wb = wg[m % 2]; a = acc[it % 3]; it += 1
            P.dma("pool", wb[:], wglu[:, m * 128:(m + 1) * 128].rearrange("(kt p) m -> p kt m", p=128), writes=[wb], sem_buf=wb)
            for kt in range(KS):
                P.op("pe", lambda e: e.matmul(a[:, :n], lhsT=wb[:, kt, :], rhs=YB[:, kt, :n], start=(kt == 0), stop=(kt == KS - 1)),
                     reads=[wb, ybk[kt]], writes=[a], track=(kt == KS - 1))
            s_ = e1[m % 2]
            P.op("act", lambda e: e.activation(out=s_[:, :n], in_=a[:, :n], func=AF.Sigmoid), reads=[a], writes=[s_])
            P.op("dve", lambda e: e.tensor_tensor(out=OS[:, m, :n], in0=YG[:, m, :n], in1=s_[:, :n], op=ALU.mult), reads=[ygk[m], s_], writes=[osk[m]])
        for kt in range(KS):
            s = sq[kt % 2]
            P.op("act", lambda e: e.activation(out=s[:, :n], in_=OS[:, kt, :n], func=AF.Square), reads=[osk[kt]], writes=[s])
            P.op("pe", lambda e: e.matmul(ssq[0][:, :n], lhsT=ones[:], rhs=s[:, :n], start=(kt == 0), stop=(kt == KS - 1)), reads=[s, ones], writes=[ssq[0]])
        rstd_from_ssq(P, ssq[0], rs_s, rtmp, n, SW)
        for kt in range(KA_):
            s = sq[kt % 2]
            P.dma("sp", OT[:, kt, :n], oT[:, kt, c0:c0 + n], writes=[ok[kt]], sem_buf=ok[kt])
            P.op("act", lambda e: e.activation(out=s[:, :n], in_=OT[:, kt, :n], func=AF.Square), reads=[ok[kt]], writes=[s])
            P.op("pe", lambda e: e.matmul(ssq[1][:, :n], lhsT=ones[:], rhs=s[:, :n], start=(kt == 0), stop=(kt == KA_ - 1)), reads=[s, ones], writes=[ssq[1]])
        rstd_from_ssq(P, ssq[1], rs_a, rtmp, n, AW)
        for kt in range(KT):
            a1 = e2[kt % 2]
            if kt < KA_:
                src = OT[:, kt, :n]; sb_ = ok[kt]; rs = rs_a; gsc = goat[:, kt:kt + 1]; gb_ = goat
            else:
                src = OS[:, kt - KA_, :n]; sb_ = osk[kt - KA_]; rs = rs_s; gsc = gost[:, kt - KA_:kt - KA_ + 1]; gb_ = gost
            P.op("dve", lambda e: e.tensor_tensor(out=a1[:, :n], in0=src, in1=rs[:, :n], op=ALU.mult), reads=[sb_, rs], writes=[a1])
            P.op("act", lambda e: e.activation(out=NTt[:, kt, :n], in_=a1[:, :n], func=AF.Copy, scale=gsc), reads=[a1, gb_], writes=[nk[kt]])
        for m in range(KT):
            wb = wt[m % 3]; a = acc[it % 3]; it += 1; xi = xin[m % 3]
            P.dma("pool", wb[:], wout[:, m * 128:(m + 1) * 128].rearrange("(kt p) m -> p kt m", p=128), writes=[wb], sem_buf=wb)
            P.dma("sp", xi[:, :n], xT[m * 128:(m + 1) * 128, c0:c0 + n], writes=[xi], sem_buf=xi)
            for kt in range(KT):
                P.op("pe", lambda e: e.matmul(a[:, :n], lhsT=wb[:, kt, :], rhs=NTt[:, kt, :n], start=(kt == 0), stop=(kt == KT - 1)),
                     reads=[wb, nk[kt]], writes=[a], track=(kt == KT - 1))
            P.op("dve", lambda e: e.scalar_tensor_tensor(out=xi[:, :n], in0=a[:, :n], scalar=gat[isctx][:, m:m + 1], in1=xi[:, :n], op0=ALU.mult, op1=ALU.add),
                 reads=[a, xi, gat[isctx]], writes=[xi])
            P.dma("sp", x1T[m * 128:(m + 1) * 128, c0:c0 + n], xi[:, :n], reads=[xi], sem_buf=xi, is_output=True)
    return P


def build_ffn(nlat, nctx, moe, final, TT=512):
    P = Prog()
    NT = nlat + nctx; KT = D // 128
    xT = P.dram("x1T", [D, NT], F32, "ExternalInput")
    modv = P.dram("modv", [2, 3, D], F32, "ExternalInput")
    gvec = P.dram("g", [D], F32, "ExternalInput")
    if moe:
        chunks = [(e, DE // 128) for e in range(NE)]
        wg = P.dram("wg", [NE, D, DE], F32, "ExternalInput"); wu = P.dram("wu", [NE, D, DE], F32, "ExternalInput")
        wd = P.dram("wd", [NE, DE, D], F32, "ExternalInput")
        wr = P.dram("wr", [D, NE], F32, "ExternalInput")
        idn_d = P.dram("idn", [128, 128], F32, "ExternalInput")
    else:
        HM = DFF // 128
        chunks = [(0, HM // 2), (HM // 2, HM - HM // 2)]
        wg = P.dram("wg", [D, DFF], F32, "ExternalInput"); wu = P.dram("wu", [D, DFF], F32, "ExternalInput")
        wd = P.dram("wd", [DFF, D], F32, "ExternalInput")
    HC = max(c[1] for c in chunks)
    if final:
        gfin = P.dram("gfin", [D], F32, "ExternalInput")
        x2T = P.dram("x2s", [D, NT], F32, "Internal")
        outT = P.dram("outT", [D, NT], F32, "ExternalOutput")
    else:
        x2T = P.dram("x2T", [D, NT], F32, "ExternalOutput")
    x2b = [Buf("x2b%d" % m) for m in range(KT)]

    ones = P.sb("ones", [128, 128], BF16); P.op("dve", lambda e: e.memset(ones[:], 1.0), writes=[ones])
    gt = P.sb("gt", [128, KT], F32); load_cols(P, gt, gvec.t)
    gs = []; sh = []; gf = []
    for j in range(2):
        s_ = P.sb("sh%d" % j, [128, KT], F32); load_cols(P, s_, modv[j, 0])
        c_ = P.sb("sc%d" % j, [128, KT], F32); load_cols(P, c_, modv[j, 1])
        f_ = P.sb("gf%d" % j, [128, KT], F32); load_cols(P, f_, modv[j, 2])
        g_ = P.sb("gs%d" % j, [128, KT], F32)
        P.op("dve", lambda e: e.scalar_tensor_tensor(out=g_[:], in0=c_[:], scalar=1.0, in1=gt[:], op0=ALU.add, op1=ALU.mult), reads=[c_, gt], writes=[g_])
        gs.append(g_); sh.append(s_); gf.append(f_)
    if final:
        gfn = P.sb("gfn", [128, KT], F32); load_cols(P, gfn, gfin.t)
    if moe:
        wrt = P.sb("wrt", [128, KT, NE], F32)
        P.dma("sp", wrt[:], wr.t.rearrange("(kt p) e -> p kt e", p=128), writes=[wrt], sem_buf=wrt, allow_slow_non_contiguous=True)
        idn = P.sb("idn", [128, 128], F32); P.dma("sp", idn[:], idn_d[:, :], writes=[idn], sem_buf=idn)
        GT = P.sb("GT", [128, NE, TT], F32)
        h32 = [P.sb("h32_%d" % i, [128, TT], F32) for i in range(2)]
        rps = [P.ps("rps%d" % i, [128, 512]) for i in range(4)]
        lg = P.sb("lg", [128, NE], F32); l2 = P.sb("l2", [128, NE], F32); eq1 = P.sb("eq1", [128, NE], F32); eq2 = P.sb("eq2", [128, NE], F32)
        mx1 = P.sb("mx1", [128, 1], F32); mx2 = P.sb("mx2", [128, 1], F32); dd = P.sb("dd", [128, 1], F32)
        w1 = P.sb("w1", [128, 1], F32); w2 = P.sb("w2", [128, 1], F32); gg = P.sb("gg", [128, NE], F32)
        gbb = [P.sb("gbb%d" % i, [128, 128], F32) for i in range(2)]
    hT = P.sb("hT", [128, KT, TT], BF16); hk = [Buf("hk%d" % i) for i in range(KT)]
    HID = P.sb("HID", [128, HC, TT], BF16); hidk = [Buf("hid%d" % i) for i in range(HC)]
    xs = [P.sb("xs%d" % i, [128, TT], F32) for i in range(3)]
    sq = [P.sb("sq%d" % i, [128, TT], BF16) for i in range(2)]
    tn = [P.sb("tn%d" % i, [128, TT], F32) for i in range(2)]
    rstd = P.sb("rstd", [128, TT], F32); rtmp = P.sb("rtmp", [128, TT], F32)
    wgt = [P.sb("wgt%d" % i, [128, KT, 128], BF16) for i in range(2)]
    wut = [P.sb("wut%d" % i, [128, KT, 128], BF16) for i in range(2)]
    wdt = [P.sb("wdt%d" % i, [128, HC, 128], BF16) for i in range(2)]
    sg = [P.sb("sg%d" % i, [128, TT], F32) for i in range(2)]
    ug = [P.sb("ug%d" % i, [128, TT], F32) for i in range(2)]
    ssq = P.ps("ssq", [128, 512])
    acc = [P.ps("acc%d" % i, [128, 512]) for i in range(3)]
    it = 0

    def stats(src_dram, src_bufs, c0, n, fdim):
        for kt in range(KT):
            xi = xs[kt % 3]; s = sq[kt % 2]
            P.dma("sp", xi[:, :n], src_dram[kt * 128:(kt + 1) * 128, c0:c0 + n], reads=[src_bufs[kt]] if src_bufs else [], writes=[xi], sem_buf=xi)
            P.op("act", lambda e: e.activation(out=s[:, :n], in_=xi[:, :n], func=AF.Square), reads=[xi], writes=[s])
            P.op("pe", lambda e: e.matmul(ssq[:, :n], lhsT=ones[:], rhs=s[:, :n], start=(kt == 0), stop=(kt == KT - 1)), reads=[s, ones], writes=[ssq])
        rstd_from_ssq(P, ssq, rstd, rtmp, n, fdim)

    for (c0, n, isctx) in token_tiles(nlat, nctx, TT):
        nblk = (n + 127) // 128
        stats(xT, None, c0, n, D)
        for kt in range(KT):
            xi = xs[kt % 3]; t_ = tn[kt % 2]
            P.dma("sp", xi[:, :n], xT[kt * 128:(kt + 1) * 128, c0:c0 + n], writes=[xi], sem_buf=xi)
            P.op("dve", lambda e: e.tensor_tensor(out=t_[:, :n], in0=xi[:, :n], in1=rstd[:, :n], op=ALU.mult), reads=[xi, rstd], writes=[t_])
            if not moe:
                P.op("act", lambda e: e.activation(out=hT[:, kt, :n], in_=t_[:, :n], func=AF.Identity, scale=gs[isctx][:, kt:kt + 1], bias=sh[isctx][:, kt:kt + 1]),
                     reads=[t_, gs[isctx], sh[isctx]], writes=[hk[kt]])
            else:
                h_ = h32[kt % 2]
                P.op("act", lambda e: e.activation(out=h_[:, :n], in_=t_[:, :n], func=AF.Identity, scale=gs[isctx][:, kt:kt + 1], bias=sh[isctx][:, kt:kt + 1]),
                     reads=[t_, gs[isctx], sh[isctx]], writes=[h_])
                P.op("pool", lambda e: e.tensor_copy(out=hT[:, kt, :n], in_=h_[:, :n]), reads=[h_], writes=[hk[kt]])
                for bk in range(nblk):
                    nb_ = min(128, n - bk * 128)
                    P.op("pe", lambda e: e.matmul(rps[bk][:nb_, 0:NE], lhsT=h_[:, bk * 128:bk * 128 + nb_], rhs=wrt[:, kt, :], start=(kt == 0), stop=(kt == KT - 1)),
                         reads=[h_, wrt], writes=[rps[bk]])
        if moe:
            for bk in range(nblk):
                nb_ = min(128, n - bk * 128)
                P.op("dve", lambda e: e.tensor_copy(out=lg[:nb_], in_=rps[bk][:nb_, 0:NE]), reads=[rps[bk]], writes=[lg])
                P.op("dve", lambda e: e.reduce_max(out=mx1[:nb_], in_=lg[:nb_], axis=mybir.AxisListType.X), reads=[lg], writes=[mx1])
                P.op("dve", lambda e: e.tensor_scalar(out=eq1[:nb_], in0=lg[:nb_], scalar1=mx1[:nb_, 0:1], scalar2=None, op0=ALU.is_equal), reads=[lg, mx1], writes=[eq1])
                P.op("dve", lambda e: e.scalar_tensor_tensor(out=l2[:nb_], in0=eq1[:nb_], scalar=-1e30, in1=lg[:nb_], op0=ALU.mult, op1=ALU.add), reads=[eq1, lg], writes=[l2])
                P.op("dve", lambda e: e.reduce_max(out=mx2[:nb_], in_=l2[:nb_], axis=mybir.AxisListType.X), reads=[l2], writes=[mx2])
                P.op("dve", lambda e: e.tensor_scalar(out=eq2[:nb_], in0=l2[:nb_], scalar1=mx2[:nb_, 0:1], scalar2=None, op0=ALU.is_equal), reads=[l2, mx2], writes=[eq2])
                P.op("dve", lambda e: e.tensor_tensor(out=dd[:nb_], in0=mx1[:nb_], in1=mx2[:nb_], op=ALU.subtract), reads=[mx1, mx2], writes=[dd])
                P.op("act", lambda e: e.activation(out=w1[:nb_], in_=dd[:nb_], func=AF.Sigmoid), reads=[dd], writes=[w1])
                P.op("act", lambda e: e.activation(out=w2[:nb_], in_=dd[:nb_], func=AF.Sigmoid, scale=-1.0), reads=[dd], writes=[w2])
                P.op("dve", lambda e: e.tensor_scalar(out=gg[:nb_], in0=eq1[:nb_], scalar1=w1[:nb_, 0:1], scalar2=None, op0=ALU.mult), reads=[eq1, w1], writes=[gg])
                P.op("dve", lambda e: e.scalar_tensor_tensor(out=gg[:nb_], in0=eq2[:nb_], scalar=w2[:nb_, 0:1], in1=gg[:nb_], op0=ALU.mult, op1=ALU.add), reads=[eq2, w2, gg], writes=[gg])
                for ex in range(NE):
                    gb_ = gbb[ex % 2]; a = acc[it % 3]; it += 1
                    P.op("dve", lambda e: e.tensor_copy(out=gb_[:nb_, :], in_=gg[:nb_, ex:ex + 1].broadcast_to([nb_, 128])), reads=[gg], writes=[gb_])
                    P.op("pe", lambda e: e.matmul(a[:, :nb_], lhsT=gb_[:nb_, :], rhs=idn[:nb_, :nb_], start=True, stop=True), reads=[gb_, idn], writes=[a])
                    P.op("act", lambda e: e.copy(out=GT[:, ex, bk * 128:bk * 128 + nb_], in_=a[:, :nb_]), reads=[a], writes=[GT])
        for ci, (cb, cn) in enumerate(chunks):
            for m in range(cn):
                wgb = wgt[m % 2]; wub = wut[m % 2]
                if moe:
                    gsrc = wg[cb, :, m * 128:(m + 1) * 128]; usrc = wu[cb, :, m * 128:(m + 1) * 128]
                else:
                    gsrc = wg[:, (cb + m) * 128:(cb + m + 1) * 128]; usrc = wu[:, (cb + m) * 128:(cb + m + 1) * 128]
                P.dma("pool", wgb[:], gsrc.rearrange("(kt p) m -> p kt m", p=128), writes=[wgb], sem_buf=wgb)
                P.dma("pool", wub[:], usrc.rearrange("(kt p) m -> p kt m", p=128), writes=[wub], sem_buf=wub)
                ag = acc[it % 3]; it += 1; au = acc[it % 3]; it += 1
                for kt in range(KT):
                    P.op("pe", lambda e: e.matmul(ag[:, :n], lhsT=wgb[:, kt, :], rhs=hT[:, kt, :n], start=(kt == 0), stop=(kt == KT - 1)),
                         reads=[wgb, hk[kt]], writes=[ag], track=(kt == KT - 1))
                for kt in range(KT):
                    P.op("pe", lambda e: e.matmul(au[:, :n], lhsT=wub[:, kt, :], rhs=hT[:, kt, :n], start=(kt == 0), stop=(kt == KT - 1)),
                         reads=[wub, hk[kt]], writes=[au], track=(kt == KT - 1))
                s_ = sg[m % 2]; u_ = ug[m % 2]
                P.op("act", lambda e: e.activation(out=s_[:, :n], in_=ag[:, :n], func=AF.Silu), reads=[ag], writes=[s_])
                if moe:
                    P.op("dve", lambda e: e.tensor_tensor(out=u_[:, :n], in0=au[:, :n], in1=GT[:, cb, :n], op=ALU.mult), reads=[au, GT], writes=[u_])
                    P.op("pool", lambda e: e.tensor_tensor(out=HID[:, m, :n], in0=s_[:, :n], in1=u_[:, :n], op=ALU.mult), reads=[s_, u_], writes=[hidk[m]])
                else:
                    P.op("dve", lambda e: e.tensor_tensor(out=HID[:, m, :n], in0=au[:, :n], in1=s_[:, :n], op=ALU.mult), reads=[au, s_], writes=[hidk[m]])
            for mo in range(KT):
                wdb = wdt[mo % 2]; a = acc[it % 3]; it += 1; xi = xs[mo % 3]
                if moe:
                    dsrc = wd[cb, :, mo * 128:(mo + 1) * 128]
                else:
                    dsrc = wd[cb * 128:(cb + cn) * 128, mo * 128:(mo + 1) * 128]
                P.dma("pool", wdb[:, :cn, :], dsrc.rearrange("(kt p) m -> p kt m", p=128), writes=[wdb], sem_buf=wdb)
                if ci == 0:
                    P.dma("sp", xi[:, :n], xT[mo * 128:(mo + 1) * 128, c0:c0 + n], writes=[xi], sem_buf=xi)
                else:
                    P.dma("sp", xi[:, :n], x2T[mo * 128:(mo + 1) * 128, c0:c0 + n], reads=[x2b[mo]], writes=[xi], sem_buf=xi)
                for kt in range(cn):
                    P.op("pe", lambda e: e.matmul(a[:, :n], lhsT=wdb[:, kt, :], rhs=HID[:, kt, :n], start=(kt == 0), stop=(kt == cn - 1)),
                         reads=[wdb, hidk[kt]], writes=[a], track=(kt == cn - 1))
                P.op("dve", lambda e: e.scalar_tensor_tensor(out=xi[:, :n], in0=a[:, :n], scalar=gf[isctx][:, mo:mo + 1], in1=xi[:, :n], op0=ALU.mult, op1=ALU.add),
                     reads=[a, xi, gf[isctx]], writes=[xi])
                P.dma("sp", x2T[mo * 128:(mo + 1) * 128, c0:c0 + n], xi[:, :n], reads=[xi], writes=[x2b[mo]], sem_buf=xi, is_output=not final)
        if final:
            stats(x2T, x2b, c0, n, D)
            for kt in range(KT):
                xi = xs[kt % 3]; t_ = tn[kt % 2]
                P.dma("sp", xi[:, :n], x2T[kt * 128:(kt + 1) * 128, c0:c0 + n], reads=[x2b[kt]], writes=[xi], sem_buf=xi)
                P.op("dve", lambda e: e.tensor_tensor(out=t_[:, :n], in0=xi[:, :n], in1=rstd[:, :n], op=ALU.mult), reads=[xi, rstd], writes=[t_])
                P.op("act", lambda e: e.activation(out=t_[:, :n], in_=t_[:, :n], func=AF.Copy, scale=gfn[:, kt:kt + 1]), reads=[t_, gfn], writes=[t_])
                P.dma("sp", outT[kt * 128:(kt + 1) * 128, c0:c0 + n], t_[:, :n], reads=[t_], sem_buf=t_, is_output=True)
    return P


NLAT = SEQ // 4
NCTX = CTX // 4
_PROGS = {}


def _prog(key, fn):
    if key not in _PROGS:
        _PROGS[key] = fn()
        _PROGS[key].finish()
    return _PROGS[key]


def _launch(P, ims):
    res = run_bass_kernel_spmd(P.nc, ims, core_ids=list(range(NCORES)))
    return res.results


def _ca(a):
    return np.ascontiguousarray(a)


def _mixer_layer(l, xTs, mods, W, nctx_post):
    f32 = np.float32
    P = _prog(("pre",), lambda: build_pre(NLAT, NCTX))
    ims = []
    for i in range(NCORES):
        b, r = divmod(i, 4)
        cosT, sinT, Rm = rope_consts(r * NLAT, NLAT)
        modv = np.stack([np.stack([mods[l, 0, :, b], mods[l, 1, :, b]]), np.stack([mods[l, 0, :, 2], mods[l, 1, :, 2]])])
        ims.append({"xT": xTs[i], "w": W["w_in"][l], "modv": _ca(modv), "g": W["g_attn_norm"][l], "cosT": cosT, "sinT": sinT, "rmat": Rm})
    ra = _launch(P, ims)
    NT = NLAT + NCTX
    ncq = nctx_post
    P = _prog(("attn", ncq), lambda: build_attn(NLAT // 128, ncq))
    ims = []
    zk = np.zeros((128, NKV, 128), NPBF)
    for i in range(NCORES):
        b, r = divmod(i, 4)
        kk = ra[i]["kk"]; vv = ra[i]["vv"]
        kl = ra[i - 1]["kk"][:, :, NLAT - 128:NLAT] if r > 0 else zk
        kr_ = ra[i + 1]["kk"][:, :, 0:128] if r < 3 else zk
        vl = ra[i - 1]["vv"][:, :, NLAT - 128:NLAT] if r > 0 else zk
        vr_ = ra[i + 1]["vv"][:, :, 0:128] if r < 3 else zk
        krh = np.concatenate([kl, kk[:, :, :NLAT], kr_], 2)
        vh = np.concatenate([vl, vv[:, :, :NLAT], vr_], 2)
        vt = vh.reshape(128, NKV, -1, 128).transpose(3, 2, 1, 0).reshape(128, -1, KVW)
        kc = np.concatenate([ra[4 * b + j]["kk"][:, :, NLAT:] for j in range(4)], 2)
        vcf = np.concatenate([ra[4 * b + j]["vv"][:, :, NLAT:] for j in range(4)], 2)
        vct = vcf.reshape(128, NKV, -1, 128).transpose(3, 2, 1, 0).reshape(128, -1, KVW)
        ims.append({"qr": ra[i]["qr"], "qp": _ca(ra[i]["qp"][:, :, :NLAT + ncq]), "kr": _ca(krh), "v": _ca(vt), "kc": _ca(kc), "vc": _ca(vct),
                    "mask": attn_masks(r == 0, r == 3), "sink": W["attn_sink"][l]})
    rb = _launch(P, ims)
    def tokmaj(a):
        return a.transpose(2, 1, 0).reshape(a.shape[2], -1)
    u_x = np.stack([np.concatenate([tokmaj(ra[4 * b + j]["uu"][:, :, :NLAT]) for j in range(4)], 0) for b in range(B)])
    u_c = np.stack([np.concatenate([tokmaj(ra[4 * b + j]["uu"][:, :, NLAT:]) for j in range(4)], 0) for b in range(B)])
    Us = s5_pack_u(u_x, u_c)
    msk, idn = s5_consts()
    with_ctx = nctx_post > 0
    P = _prog(("s5", with_ctx), lambda: build_s5(8, with_ctx))
    ims = []
    for i in range(NCORES):
        gsl = slice(i * 8, (i + 1) * 8)
        ims.append({"U": Us[i], "a_re": _ca(W["ssm_a_re"][l][:, gsl]), "a_im": _ca(W["ssm_a_im"][l][:, gsl]),
                    "log_dt": _ca(W["ssm_log_dt"][l][:, gsl].reshape(-1)), "b_re": _ca(W["ssm_b_re"][l][:, gsl]), "b_im": _ca(W["ssm_b_im"][l][:, gsl]),
                    "c_re": _ca(W["ssm_c_re"][l][:, gsl]), "c_im": _ca(W["ssm_c_im"][l][:, gsl]), "msk": msk, "idn": idn})
    rc = _launch(P, ims)
    y_x, y_c = s5_unpack_y([r_["y"] for r_ in rc])
    P = _prog(("post", nctx_post), lambda: build_post(NLAT, nctx_post))
    ims = []
    ntp = NLAT + nctx_post
    for i in range(NCORES):
        b, r = divmod(i, 4)
        yt = y_x[b, r * NLAT:(r + 1) * NLAT]
        if nctx_post:
            yt = np.concatenate([yt, y_c[b, r * NCTX:(r + 1) * NCTX]], 0)
        yT = yt.reshape(ntp, SW // 128, 128).transpose(2, 1, 0)
        gatev = np.stack([mods[l, 2, :, b], mods[l, 2, :, 2]])
        ims.append({"oT": _ca(rb[i]["oT"][:, :, :ntp]), "yT": _ca(yT), "uT": _ca(ra[i]["uu"][:, :, :ntp]), "xT": _ca(xTs[i][:, :ntp]),
                    "dsk": W["ssm_d"][l], "wglu": W["w_glu"][l], "goa": W["g_out_attn"][l], "gos": W["g_out_ssm"][l], "wout": W["w_out"][l],
                    "gatev": _ca(gatev)})
    rd = _launch(P, ims)
    return [r_["x1T"] for r_ in rd]


def kernel(x, c, ctx, c_ctx, w_mod, b_mod, g_attn_norm, w_in, attn_sink, ssm_a_re, ssm_a_im, ssm_log_dt, ssm_b_re, ssm_b_im,
           ssm_c_re, ssm_c_im, ssm_d, w_glu, g_out_attn, g_out_ssm, w_out, g_ffn_norm, w_ff_gate, w_ff_up, w_ff_down, w_router,
           w_exp_gate, w_exp_up, w_exp_down, g_final):
    W = {k: np.asarray(v) for k, v in dict(
        g_attn_norm=g_attn_norm, w_in=w_in, attn_sink=attn_sink, ssm_a_re=ssm_a_re, ssm_a_im=ssm_a_im, ssm_log_dt=ssm_log_dt,
        ssm_b_re=ssm_b_re, ssm_b_im=ssm_b_im, ssm_c_re=ssm_c_re, ssm_c_im=ssm_c_im, ssm_d=ssm_d, w_glu=w_glu, g_out_attn=g_out_attn,
        g_out_ssm=g_out_ssm, w_out=w_out, g_ffn_norm=g_ffn_norm).items()}
    x = np.asarray(x); ctx = np.asarray(ctx)
    mods = run_mod(np.asarray(c), np.asarray(c_ctx), np.asarray(w_mod), np.asarray(b_mod))
    xTs = []
    for i in range(NCORES):
        b, r = divmod(i, 4)
        xTs.append(_ca(np.concatenate([x[b, r * NLAT:(r + 1) * NLAT], ctx[b, r * NCTX:(r + 1) * NCTX]], 0).T))
    x1 = _mixer_layer(0, xTs, mods, W, NCTX)
    P = _prog(("ffn", 0), lambda: build_ffn(NLAT, NCTX, False, False))
    ims = []
    for i in range(NCORES):
        b, r = divmod(i, 4)
        modv = np.stack([np.stack([mods[0, 3 + j, :, b] for j in range(3)]), np.stack([mods[0, 3 + j, :, 2] for j in range(3)])])
        ims.append({"x1T": x1[i], "modv": _ca(modv), "g": W["g_ffn_norm"][0], "wg": np.asarray(w_ff_gate)[0], "wu": np.asarray(w_ff_up)[0],
                    "wd": np.asarray(w_ff_down)[0]})
    x2 = [r_["x2T"] for r_ in _launch(P, ims)]
    x1 = _mixer_layer(1, x2, mods, W, 0)
    P = _prog(("ffn", 1), lambda: build_ffn(NLAT, 0, True, True))
    ims = []
    for i in range(NCORES):
        b, r = divmod(i, 4)
        modv = np.stack([np.stack([mods[1, 3 + j, :, b] for j in range(3)]), np.stack([mods[1, 3 + j, :, 2] for j in range(3)])])
        ims.append({"x1T": x1[i], "modv": _ca(modv), "g": W["g_ffn_norm"][1], "wg": np.asarray(w_exp_gate)[0], "wu": np.asarray(w_exp_up)[0],
                    "wd": np.asarray(w_exp_down)[0], "wr": np.asarray(w_router)[0], "idn": np.eye(128, dtype=np.float32), "gfin": np.asarray(g_final)})
    ro = _launch(P, ims)
    out = np.empty((B, SEQ, D), np.float32)
    for i in range(NCORES):
        b, r = divmod(i, 4)
        out[b, r * NLAT:(r + 1) * NLAT] = ro[i]["outT"].T
    return out
```

```python
import math
import numpy as np
import ml_dtypes
import concourse.bass as bass
import concourse.mybir as mybir
from concourse.bass_utils import run_bass_kernel_spmd

F32 = mybir.dt.float32
BF16 = mybir.dt.bfloat16
I32 = mybir.dt.int32
AF = mybir.ActivationFunctionType
ALU = mybir.AluOpType
NPBF = ml_dtypes.bfloat16
NCORES = 8

D = 4096; NQH = 24; NKV = 8; HD = 128; AW = 3072; KVW = 1024; SW = 1024
INC = 6144; DFF = 11008; NE = 8; DE = 4096; CTX = 256; SEQ = 8192; B = 2
EPS = 1e-6; SCALE = HD ** -0.5
TCH = 32
NCH = (SEQ + CTX) // TCH


class Buf:
    __slots__ = ("name", "w", "r", "dsem", "dcnt")

    def __init__(self, name):
        self.name = name; self.w = None; self.r = {}; self.dsem = None; self.dcnt = 0


class T:
    def __init__(self, t, b):
        self.t = t; self.b = b

    def __getitem__(self, k):
        return self.t[k]


class Prog:
    def __init__(self):
        self.nc = bass.Bass("TRN2", target_bir_lowering=False)
        nc = self.nc
        self.engs = {"pe": nc.tensor, "dve": nc.vector, "act": nc.scalar, "pool": nc.gpsimd, "sp": nc.sync}
        self.esem = {k: nc.semaphore("es_" + k).__enter__() for k in self.engs}
        self.ecnt = {k: 0 for k in self.engs}
        self.seen = {k: {} for k in self.engs}
        self.pend = {k: ([], []) for k in self.engs}
        self.nsem = len(self.engs)
        self.out_toks = []
        self.uid = 0
        self.live = []
        self.dbufs = []
        self.free_sems = []
        self.prefix = ""
        self.over = {}
        self.kind_over = {}
        self.handles = {}

    def sb(self, name, shape, dt):
        self.uid += 1
        cm = self.nc.sbuf_tensor(f"{name}_{self.uid}", list(shape), dt)
        t = cm.__enter__(); self.live.append(("sb", cm))
        return T(t, Buf(name))

    def ps(self, name, shape, dt=F32):
        self.uid += 1
        cm = self.nc.psum_tensor(f"{name}_{self.uid}", list(shape), dt)
        t = cm.__enter__(); self.live.append(("ps", cm))
        return T(t, Buf(name))

    def dram(self, name, shape, dt, kind="Internal"):
        if name in self.over:
            return self.over[name]
        kind = self.kind_over.get(name, kind)
        t = T(self.nc.dram_tensor(self.prefix + name, list(shape), dt, kind=kind).ap(), Buf(name))
        self.handles[name] = t
        return t

    def barrier(self):
        toks = [(self.esem[k], self.ecnt[k]) for k in self.engs if self.ecnt[k] > 0]
        toks += [(b.dsem, b.dcnt) for b in self.dbufs if b.dcnt > 0]
        for e in self.engs:
            for t in toks:
                if t[0] is not self.esem[e]:
                    self._wait(e, t)

    def release(self, kinds=("sb", "ps")):
        keep = []
        for k, cm in reversed(self.live):
            if k in kinds:
                cm.__exit__(None, None, None)
            else:
                keep.append((k, cm))
        self.live = list(reversed(keep))
        if "sb" not in kinds:
            return
        for b in self.dbufs:
            self.free_sems.append((b.dsem, b.dcnt))
        self.dbufs = []

    def _wait(self, e, tok):
        sem, val = tok
        if self.seen[e].get(id(sem), 0) >= val:
            return
        self.engs[e].wait_ge(sem, val)
        self.seen[e][id(sem)] = val

    def _deps(self, e, reads, writes):
        toks = []
        for b in reads:
            if b.w is not None:
                toks.append(b.w)
        for b in writes:
            if b.w is not None:
                toks.append(b.w)
            toks.extend(b.r.values())
        for t in toks:
            if t[0] is self.esem[e] and e == "pe":
                continue
            self._wait(e, t)

    def _commit(self, tok, reads, writes):
        for b in writes:
            b.w = tok; b.r = {}
        for b in reads:
            k = id(tok[0])
            if k not in b.r or b.r[k][1] < tok[1]:
                b.r[k] = tok

    def op(self, e, fn, reads=(), writes=(), track=True):
        reads = [x.b if isinstance(x, T) else x for x in reads]
        writes = [x.b if isinstance(x, T) else x for x in writes]
        self._deps(e, reads, writes)
        ins = fn(self.engs[e])
        pr, pw = self.pend[e]
        if not track:
            pr.extend(reads); pw.extend(writes)
            return ins
        self.ecnt[e] += 1
        ins.then_inc(self.esem[e], 1)
        tok = (self.esem[e], self.ecnt[e])
        self._commit(tok, reads + pr, writes + pw)
        self.pend[e] = ([], [])
        return ins

    def dma(self, q, out, in_, reads=(), writes=(), sem_buf=None, is_output=False, **kw):
        reads = [x.b if isinstance(x, T) else x for x in reads]
        writes = [x.b if isinstance(x, T) else x for x in writes]
        self._deps(q, reads, writes)
        sb = sem_buf.b if isinstance(sem_buf, T) else sem_buf
        if sb.dsem is None:
            if self.free_sems:
                sb.dsem, sb.dcnt = self.free_sems.pop()
            else:
                sb.dsem = self.nc.semaphore("ds_%d" % self.nsem).__enter__(); self.nsem += 1
            self.dbufs.append(sb)
        sb.dcnt += 16
        ins = self.engs[q].dma_start(out=out, in_=in_, **kw)
        ins.then_inc(sb.dsem, 16)
        tok = (sb.dsem, sb.dcnt)
        self._commit(tok, reads, writes)
        if is_output:
            self.out_toks.append(tok)
        return ins

    def finish(self):
        if getattr(self, "_fin", False):
            return
        self._fin = True
        last = {}
        for sem, val in self.out_toks:
            if id(sem) not in last or last[id(sem)][1] < val:
                last[id(sem)] = (sem, val)
        for tok in last.values():
            self._wait("sp", tok)


def run(prog, in_maps):
    prog.finish()
    res = run_bass_kernel_spmd(prog.nc, in_maps, core_ids=list(range(NCORES)))
    return res.results


def build_mod(ncol):
    P = Prog()
    KT = D // 128; MT = ncol // 128
    cv = P.dram("cv", [3, D], F32, "ExternalInput")
    w = P.dram("w", [D, ncol], F32, "ExternalInput")
    bb = P.dram("b", [ncol], F32, "ExternalInput")
    out = P.dram("out", [128, MT, 3], F32, "ExternalOutput")
    cT = P.sb("cT", [128, KT, 3], F32); cS = P.sb("cS", [128, KT, 3], BF16)
    bt = P.sb("bt", [128, MT], F32); ot = P.sb("ot", [128, MT, 3], F32)
    wt = [P.sb("wt%d" % i, [128, KT, 128], BF16) for i in range(3)]
    pp = [P.ps("pp%d" % i, [128, 512]) for i in range(2)]
    for j in range(3):
        P.dma("sp", cT[:, :, j], cv[j].rearrange("(kt p) -> p kt", p=128), writes=[cT], sem_buf=cT,
              allow_slow_non_contiguous=True)
    P.dma("sp", bt[:], bb.t.rearrange("(m p) -> p m", p=128), writes=[bt], sem_buf=bt, allow_slow_non_contiguous=True)
    P.op("act", lambda e: e.activation(out=cS[:], in_=cT[:], func=AF.Silu), reads=[cT], writes=[cS])
    for m in range(MT):
        wb = wt[m % 3]; pt = pp[m % 2]
        P.dma("pool", wb[:], w[:, m * 128:(m + 1) * 128].rearrange("(kt p) m -> p kt m", p=128), writes=[wb], sem_buf=wb)
        for kt in range(KT):
            P.op("pe", lambda e: e.matmul(pt[:, 0:3], lhsT=wb[:, kt, :], rhs=cS[:, kt, :], start=(kt == 0), stop=(kt == KT - 1)),
                 reads=[wb, cS], writes=[pt], track=(kt == KT - 1))
        P.op("dve", lambda e: e.tensor_scalar(out=ot[:, m, :], in0=pt[:, 0:3], scalar1=bt[:, m:m + 1], scalar2=None, op0=ALU.add),
             reads=[pt, bt], writes=[ot])
    P.dma("sp", out[:], ot[:], reads=[ot], writes=[out], sem_buf=ot, is_output=True)
    return P


def run_mod(c, c_ctx, w_mod, b_mod):
    depth = w_mod.shape[0]
    ncol = depth * 6 * D // NCORES
    cv = np.ascontiguousarray(np.concatenate([c, c_ctx[None]], 0))
    wall = w_mod.transpose(1, 0, 2).reshape(D, depth * 6 * D)
    ball = b_mod.reshape(-1)
    P = build_mod(ncol)
    ims = [{"cv": cv, "w": np.ascontiguousarray(wall[:, i * ncol:(i + 1) * ncol]),
            "b": np.ascontiguousarray(ball[i * ncol:(i + 1) * ncol])} for i in range(NCORES)]
    res = run(P, ims)
    o = np.concatenate([r["out"].transpose(1, 0, 2).reshape(ncol, 3) for r in res], 0)
    return o.reshape(depth, 6, D, 3)


def load_cols(P, dst, src_vec, q="sp"):
    P.dma(q, dst[:], src_vec.rearrange("(kt p) -> p kt", p=128), writes=[dst], sem_buf=dst, allow_slow_non_contiguous=True)


def rstd_from_ssq(P, ssq_ps, rstd, tmp, n, fdim):
    P.op("act", lambda e: e.activation(out=tmp[:, :n], in_=ssq_ps[:, :n], func=AF.Sqrt, scale=1.0 / fdim, bias=EPS),
         reads=[ssq_ps], writes=[tmp])
    P.op("dve", lambda e: e.reciprocal(out=rstd[:, :n], in_=tmp[:, :n]), reads=[tmp], writes=[rstd])


def build_pre(nlat, nctx, TT=512, P=None):
    P = P or Prog()
    NT = nlat + nctx; KT = D // 128; MT = INC // 128
    xT = P.dram("xT", [D, NT], F32, "ExternalInput")
    w = P.dram("w", [D, INC], F32, "ExternalInput")
    modv = P.dram("modv", [2, 2, D], F32, "ExternalInput")
    gvec = P.dram("g", [D], F32, "ExternalInput")
    cosd = P.dram("cosT", [128, nlat], F32, "ExternalInput")
    sind = P.dram("sinT", [128, nlat], F32, "ExternalInput")
    rmat = P.dram("rmat", [128, 128], F32, "ExternalInput")
    qr = P.dram("qr", [128, NQH, nlat], BF16, "ExternalOutput")
    qp = P.dram("qp", [128, NQH, NT], BF16, "ExternalOutput")
    kk = P.dram("kk", [128, NKV, NT], BF16, "ExternalOutput")
    vv = P.dram("vv", [128, NKV, NT], BF16, "ExternalOutput")
    uu = P.dram("uu", [128, SW // 128, NT], F32, "ExternalOutput")

    ones = P.sb("ones", [128, 128], BF16)
    P.op("dve", lambda e: e.memset(ones[:], 1.0), writes=[ones])
    R = P.sb("R", [128, 128], BF16)
    P.dma("pool", R[:], rmat[:, :], writes=[R], sem_buf=R)
    cosT = P.sb("cos", [128, nlat], F32); sinT = P.sb("sin", [128, nlat], F32)
    P.dma("sp", cosT[:], cosd[:, :], writes=[cosT], sem_buf=cosT)
    P.dma("sp", sinT[:], sind[:, :], writes=[sinT], sem_buf=sinT)
    gt = P.sb("gt", [128, KT], F32); load_cols(P, gt, gvec.t)
    gs = []; sh = []
    for j in range(2):
        s_ = P.sb("sh%d" % j, [128, KT], F32); load_cols(P, s_, modv[j, 0])
        c_ = P.sb("sc%d" % j, [128, KT], F32); load_cols(P, c_, modv[j, 1])
        g_ = P.sb("gs%d" % j, [128, KT], F32)
        P.op("dve", lambda e: e.scalar_tensor_tensor(out=g_[:], in0=c_[:], scalar=1.0, in1=gt[:], op0=ALU.add, op1=ALU.mult),
             reads=[c_, gt], writes=[g_])
        gs.append(g_); sh.append(s_)

    xall = P.sb("xall", [128, KT, TT], F32)
    xk = [Buf("xk%d" % i) for i in range(KT)]
    sq = [P.sb("sq%d" % i, [128, TT], BF16) for i in range(2)]
    hT = P.sb("hT", [128, KT, TT], BF16)
    hk = [Buf("hk%d" % i) for i in range(KT)]
    tmpn = [P.sb("tmpn%d" % i, [128, TT], F32) for i in range(2)]
    rstd = P.sb("rstd", [128, TT], F32); rtmp = P.sb("rtmp", [128, TT], F32)
    wt = [P.sb("wt%d" % i, [128, KT, 128], BF16) for i in range(3)]
    ssq = P.ps("ssq", [128, 512])
    acc = [P.ps("acc%d" % i, [128, 512]) for i in range(2)]
    rot = [P.ps("rot%d" % i, [128, 512]) for i in range(2)]
    ob = [P.sb("ob%d" % i, [128, TT], BF16) for i in range(3)]
    of = [P.sb("of%d" % i, [128, TT], F32) for i in range(2)]
    t1 = [P.sb("t1_%d" % i, [128, TT], F32) for i in range(2)]
    t2 = [P.sb("t2_%d" % i, [128, TT], F32) for i in range(2)]
    orr = [P.sb("orr%d" % i, [128, TT], BF16) for i in range(2)]

    tiles = [(c0, min(TT, nlat - c0), 0) for c0 in range(0, nlat, TT)]
    if nctx:
        tiles += [(nlat + c0, min(TT, nctx - c0), 1) for c0 in range(0, nctx, TT)]
    it = 0
    for (c0, n, isctx) in tiles:
        for kt in range(KT):
            P.dma("sp", xall[:, kt, :n], xT[kt * 128:(kt + 1) * 128, c0:c0 + n], reads=[], writes=[xk[kt]], sem_buf=xk[kt])
            s = sq[kt % 2]
            P.op("act", lambda e: e.activation(out=s[:, :n], in_=xall[:, kt, :n], func=AF.Square), reads=[xk[kt]], writes=[s])
            P.op("pe", lambda e: e.matmul(ssq[:, :n], lhsT=ones[:], rhs=s[:, :n], start=(kt == 0), stop=(kt == KT - 1)),
                 reads=[s, ones], writes=[ssq], track=True)
        rstd_from_ssq(P, ssq, rstd, rtmp, n, D)
        for kt in range(KT):
            tn = tmpn[kt % 2]
            P.op("dve", lambda e: e.tensor_tensor(out=tn[:, :n], in0=xall[:, kt, :n], in1=rstd[:, :n], op=ALU.mult),
                 reads=[xk[kt], rstd], writes=[tn])
            P.op("act", lambda e: e.activation(out=hT[:, kt, :n], in_=tn[:, :n], func=AF.Identity,
                                               scale=gs[isctx][:, kt:kt + 1], bias=sh[isctx][:, kt:kt + 1]),
                 reads=[tn, gs[isctx], sh[isctx]], writes=[hk[kt]])
        for m in range(MT):
            wb = wt[it % 3]; a = acc[it % 2]; it += 1
            P.dma("pool", wb[:], w[:, m * 128:(m + 1) * 128].rearrange("(kt p) m -> p kt m", p=128), writes=[wb], sem_buf=wb)
            for kt in range(KT):
                P.op("pe", lambda e: e.matmul(a[:, :n], lhsT=wb[:, kt, :], rhs=hT[:, kt, :n], start=(kt == 0), stop=(kt == KT - 1)),
                     reads=[wb, hk[kt]], writes=[a], track=(kt == KT - 1))
            if m < 32:
                o = ob[m % 3]
                P.op("act", lambda e: e.copy(out=o[:, :n], in_=a[:, :n]), reads=[a], writes=[o])
                isq = m < NQH
                if isq:
                    P.dma("sp", qp[:, m, c0:c0 + n], o[:, :n], reads=[o], writes=[], sem_buf=o, is_output=True)
                if isctx:
                    if not isq:
                        P.dma("sp", kk[:, m - NQH, c0:c0 + n], o[:, :n], reads=[o], writes=[], sem_buf=o, is_output=True)
                else:
                    r = rot[m % 2]; a1 = t1[m % 2]; a2 = t2[m % 2]; oo = orr[m % 2]
                    P.op("pe", lambda e: e.matmul(r[:, :n], lhsT=R[:], rhs=o[:, :n], start=True, stop=True), reads=[R, o], writes=[r])
                    P.op("pool", lambda e: e.tensor_tensor(out=a1[:, :n], in0=o[:, :n], in1=cosT[:, c0:c0 + n], op=ALU.mult),
                         reads=[o, cosT], writes=[a1])
                    P.op("dve", lambda e: e.tensor_tensor(out=a2[:, :n], in0=r[:, :n], in1=sinT[:, c0:c0 + n], op=ALU.mult),
                         reads=[r, sinT], writes=[a2])
                    P.op("dve", lambda e: e.tensor_tensor(out=oo[:, :n], in0=a2[:, :n], in1=a1[:, :n], op=ALU.add),
                         reads=[a1, a2], writes=[oo])
                    dst = qr[:, m, c0:c0 + n] if isq else kk[:, m - NQH, c0:c0 + n]
                    P.dma("sp", dst, oo[:, :n], reads=[oo], writes=[], sem_buf=oo, is_output=True)
            elif m < 40:
                o = ob[m % 3]
                P.op("act", lambda e: e.copy(out=o[:, :n], in_=a[:, :n]), reads=[a], writes=[o])
                P.dma("sp", vv[:, m - 32, c0:c0 + n], o[:, :n], reads=[o], writes=[], sem_buf=o, is_output=True)
            else:
                o = of[m % 2]
                P.op("dve", lambda e: e.tensor_copy(out=o[:, :n], in_=a[:, :n]), reads=[a], writes=[o])
                P.dma("sp", uu[:, m - 40, c0:c0 + n], o[:, :n], reads=[o], writes=[], sem_buf=o, is_output=True)
    return P


def rope_consts(tok0, n):
    t = np.arange(tok0, tok0 + n)
    row = (t // 64).astype(np.float32); col = (t % 64).astype(np.float32)
    inv = (10000.0 ** (-np.arange(0, 64, 2, dtype=np.float32) / 64)).astype(np.float32)
    ar = row[:, None] * inv; ac = col[:, None] * inv
    ang = np.concatenate([ar, ar, ac, ac], -1)
    cosT = np.ascontiguousarray(np.cos(ang).T.astype(np.float32)); sinT = np.ascontiguousarray(np.sin(ang).T.astype(np.float32))
    Rm = np.zeros((128, 128), np.float32)
    for h0 in (0, 64):
        for d in range(32):
            Rm[h0 + d + 32, h0 + d] = -1.0
            Rm[h0 + d, h0 + d + 32] = 1.0
    return cosT, sinT, Rm


def build_attn(NB, NCQ, P=None):
    P = P or Prog()
    NL = NB * 128; NQ = NL + NCQ; NKB = NB + 2; NCB = CTX // 128
    qr = P.dram("qr", [128, NQH, NL], BF16, "ExternalInput")
    qp = P.dram("qp", [128, NQH, NQ], BF16, "ExternalInput")
    kr = P.dram("kr", [128, NKV, NKB * 128], BF16, "ExternalInput")
    vv = P.dram("v", [128, NKB, KVW], BF16, "ExternalInput")
    kc = P.dram("kc", [128, NKV, CTX], BF16, "ExternalInput")
    vc = P.dram("vc", [128, NCB, KVW], BF16, "ExternalInput")
    mk = P.dram("mask", [128, 4, 128], BF16, "ExternalInput")
    snk = P.dram("sink", [NQH], F32, "ExternalInput")
    oT = P.dram("oT", [128, NQH, NQ], F32, "ExternalOutput")

    ones = P.sb("ones", [128, 128], BF16)
    P.op("dve", lambda e: e.memset(ones[:], 1.0), writes=[ones])
    KR = P.sb("KR", [128, NKV, NKB * 128], BF16); P.dma("sp", KR[:], kr[:], writes=[KR], sem_buf=KR)
    VV = P.sb("VV", [128, NKB, KVW], BF16); P.dma("sp", VV[:], vv[:], writes=[VV], sem_buf=VV)
    KC = P.sb("KC", [128, NKV, CTX], BF16); P.dma("sp", KC[:], kc[:], writes=[KC], sem_buf=KC)
    VC = P.sb("VC", [128, NCB, KVW], BF16); P.dma("sp", VC[:], vc[:], writes=[VC], sem_buf=VC)
    MK = P.sb("MK", [128, 4, 128], BF16); P.dma("sp", MK[:], mk[:], writes=[MK], sem_buf=MK)
    es0 = P.sb("es0", [128, NQH], F32); es = P.sb("es", [128, NQH], F32)
    P.dma("sp", es0[:], snk.t.partition_broadcast(128), writes=[es0], sem_buf=es0, allow_slow_non_contiguous=True)
    P.op("act", lambda e: e.activation(out=es[:], in_=es0[:], func=AF.Exp), reads=[es0], writes=[es])

    QR = [P.sb("QR%d" % i, [128, 3, 128], BF16) for i in range(2)]
    QP = [P.sb("QP%d" % i, [128, 3, 128], BF16) for i in range(2)]
    sps = [P.ps("sps%d" % i, [128, 512]) for i in range(3)]
    ops_ = [P.ps("ops%d" % i, [128, 512]) for i in range(2)]
    dps = [P.ps("dps%d" % i, [128, 512]) for i in range(2)]
    pt = [P.sb("pt%d" % i, [128, 3, 128], BF16) for i in range(4)]
    dn = [P.sb("dn%d" % i, [128, 3, 128], F32) for i in range(2)]
    oo = [P.sb("oo%d" % i, [128, 3, 128], F32) for i in range(2)]
    it = 0; ip = 0
    blocks = [(n, 128, False) for n in range(NB)] + ([(NB, NCQ, True)] if NCQ else [])
    for (n, nq, isctx) in blocks:
        c0 = n * 128; N3 = 3 * nq
        for h in range(NKV):
            Qp_ = QP[it % 2]; Qr_ = QR[it % 2]
            P.dma("sp", Qp_[:, :, :nq], qp[:, 3 * h:3 * h + 3, c0:c0 + nq], writes=[Qp_], sem_buf=Qp_)
            if not isctx:
                P.dma("sp", Qr_[:, :, :nq], qr[:, 3 * h:3 * h + 3, c0:c0 + nq], writes=[Qr_], sem_buf=Qr_)
            kbs = [] if isctx else [("w", n + j, (None, (2 if n == 0 else 0), None, None)[1] if j == 0 else None) for j in range(3)]
            if not isctx:
                kbs = [("w", n, 2 if n == 0 else 0), ("w", n + 1, None), ("w", n + 2, 3 if n == NB - 1 else 1)]
            kbs += [("c", c, None) for c in range(NCB)]
            O = ops_[it % 2]; Dn = dps[it % 2]
            for bi, (kind, kb, mi) in enumerate(kbs):
                S = sps[ip % 3]; Pt = pt[ip % 4]; ip += 1
                if kind == "w":
                    P.op("pe", lambda e: e.matmul(S[:, :N3], lhsT=KR[:, h, kb * 128:(kb + 1) * 128], rhs=Qr_[:, :, :nq], start=True, stop=True),
                         reads=[KR, Qr_], writes=[S])
                else:
                    P.op("pe", lambda e: e.matmul(S[:, :N3], lhsT=KC[:, h, kb * 128:(kb + 1) * 128], rhs=Qp_[:, :, :nq], start=True, stop=True),
                         reads=[KC, Qp_], writes=[S])
                P.op("act", lambda e: e.activation(out=Pt[:, :, :nq], in_=S[:, :N3].rearrange("p (a b) -> p a b", a=3), func=AF.Exp, scale=SCALE),
                     reads=[S], writes=[Pt])
                if mi is not None:
                    P.op("pool", lambda e: e.tensor_tensor(out=Pt[:, :, :nq], in0=Pt[:, :, :nq],
                                                           in1=MK[:, mi:mi + 1, :nq].broadcast_to([128, 3, nq]), op=ALU.mult),
                         reads=[Pt, MK], writes=[Pt])
                vsrc = VV[:, kb, h * 128:(h + 1) * 128] if kind == "w" else VC[:, kb, h * 128:(h + 1) * 128]
                last = bi == len(kbs) - 1
                P.op("pe", lambda e: e.matmul(O[:, :N3], lhsT=vsrc, rhs=Pt[:, :, :nq], start=(bi == 0), stop=last),
                     reads=[VV, VC, Pt], writes=[O], track=last)
                P.op("pe", lambda e: e.matmul(Dn[:, :N3], lhsT=ones[:], rhs=Pt[:, :, :nq], start=(bi == 0), stop=last),
                     reads=[ones, Pt], writes=[Dn], track=True)
            d_ = dn[it % 2]; o_ = oo[it % 2]
            P.op("dve", lambda e: e.tensor_tensor(out=d_[:, :, :nq], in0=Dn[:, :N3].rearrange("p (a b) -> p a b", a=3),
                                                  in1=es[:, 3 * h:3 * h + 3].unsqueeze(2).broadcast_to([128, 3, nq]), op=ALU.add),
                 reads=[Dn, es], writes=[d_])
            P.op("dve", lambda e: e.reciprocal(out=d_[:, :, :nq], in_=d_[:, :, :nq]), reads=[d_], writes=[d_])
            P.op("dve", lambda e: e.tensor_tensor(out=o_[:, :, :nq], in0=O[:, :N3].rearrange("p (a b) -> p a b", a=3), in1=d_[:, :, :nq], op=ALU.mult),
                 reads=[O, d_], writes=[o_])
            P.dma("sp", oT[:, 3 * h:3 * h + 3, c0:c0 + nq], o_[:, :, :nq], reads=[o_], sem_buf=o_, is_output=True)
            it += 1
    return P


def attn_masks(first, last):
    m = np.arange(128)[:, None]; i = np.arange(128)[None, :]
    L = (i <= m).astype(np.float32); R = (m <= i).astype(np.float32)
    Z = np.zeros_like(L)
    return np.stack([L, R, Z if first else L, Z if last else R], 1).astype(NPBF)


def build_s5(NG=8, with_ctx=True, P=None):
    P = P or Prog()
    NGD = 2 * NG; NCC = CTX // TCH; NXC = SEQ // TCH; NJ = NCH; NU = NCH + NCC; NS = NG * B
    TWO_PI = 2.0 * math.pi
    U_d = P.dram("U", [128, 4, NG, B, NU], F32, "ExternalInput")
    are_d = P.dram("a_re", [2, NG, 64], F32, "ExternalInput"); aim_d = P.dram("a_im", [2, NG, 64], F32, "ExternalInput")
    ldt_d = P.dram("log_dt", [2 * NG], F32, "ExternalInput")
    bre_d = P.dram("b_re", [2, NG, 64, 16], F32, "ExternalInput"); bim_d = P.dram("b_im", [2, NG, 64, 16], F32, "ExternalInput")
    cre_d = P.dram("c_re", [2, NG, 16, 64], F32, "ExternalInput"); cim_d = P.dram("c_im", [2, NG, 16, 64], F32, "ExternalInput")
    msk_d = P.dram("msk", [128, 2, 4, 512], BF16, "ExternalInput")
    idn_d = P.dram("idn", [64, 64], F32, "ExternalInput")
    y_d = P.dram("y", [128, 4, NG, B, NXC + NCC], F32, "ExternalOutput")
    NP = 64
    cnt = [0]

    def tl(shape, dt=F32, name="t"):
        cnt[0] += 1
        return P.sb("%s%d" % (name, cnt[0]), [NP] + list(shape), dt)

    def tt(o, a, b, op, e="dve"):
        P.op(e, lambda en: en.tensor_tensor(out=o[:], in0=a[:], in1=b[:], op=op), reads=[a, b], writes=[o]); return o

    def ts(o, a, s1, op0, s2=None, op1=None, e="dve"):
        if op1 is None:
            P.op(e, lambda en: en.tensor_scalar(out=o[:], in0=a[:], scalar1=s1, scalar2=None, op0=op0), reads=[a], writes=[o])
        else:
            P.op(e, lambda en: en.tensor_scalar(out=o[:], in0=a[:], scalar1=s1, scalar2=s2, op0=op0, op1=op1), reads=[a], writes=[o])
        return o

    def act(o, a, func, **kw):
        P.op("act", lambda en: en.activation(out=o[:], in_=a[:], func=func, **kw), reads=[a], writes=[o]); return o

    are = tl([NGD]); aim = tl([NGD]); ldt = tl([NGD])
    P.dma("sp", are[:], are_d.t.rearrange("d g p -> p (d g)"), writes=[are], sem_buf=are, allow_slow_non_contiguous=True)
    P.dma("sp", aim[:], aim_d.t.rearrange("d g p -> p (d g)"), writes=[aim], sem_buf=aim, allow_slow_non_contiguous=True)
    P.dma("sp", ldt[:], ldt_d.t.partition_broadcast(NP), writes=[ldt], sem_buf=ldt, allow_slow_non_contiguous=True)
    bre = tl([NGD, 16]); bim = tl([NGD, 16]); cre = tl([NGD, 16]); cim = tl([NGD, 16])
    P.dma("sp", bre[:], bre_d.t.rearrange("d g p c -> p (d g) c"), writes=[bre], sem_buf=bre, allow_slow_non_contiguous=True)
    P.dma("sp", bim[:], bim_d.t.rearrange("d g p c -> p (d g) c"), writes=[bim], sem_buf=bim, allow_slow_non_contiguous=True)
    P.dma("sp", cre[:], cre_d.t.rearrange("d g c p -> p (d g) c"), writes=[cre], sem_buf=cre, allow_slow_non_contiguous=True)
    P.dma("sp", cim[:], cim_d.t.rearrange("d g c p -> p (d g) c"), writes=[cim], sem_buf=cim, allow_slow_non_contiguous=True)
    idn = P.sb("idn", [NP, 64], BF16); P.dma("pool", idn[:], idn_d[:, :], writes=[idn], sem_buf=idn)
    msk = P.sb("msk", [128, 2, 4, 512], BF16); P.dma("sp", msk[:], msk_d[:], writes=[msk], sem_buf=msk)

    dt_ = act(tl([NGD]), ldt, AF.Exp)
    xr = tt(tl([NGD]), are, dt_, ALU.mult); th = tt(tl([NGD]), aim, dt_, ALU.mult)
    mag = act(tl([NGD]), xr, AF.Exp)

    def sin_of(ang):
        q = ts(tl([NGD]), ang, 1.0 / TWO_PI, ALU.mult)
        qi = tl([NGD], I32)
        P.op("dve", lambda en: en.tensor_copy(out=qi[:], in_=q[:]), reads=[q], writes=[qi])
        qf = tl([NGD])
        P.op("dve", lambda en: en.tensor_copy(out=qf[:], in_=qi[:]), reads=[qi], writes=[qf])
        r = tl([NGD])
        P.op("dve", lambda en: en.scalar_tensor_tensor(out=r[:], in0=qf[:], scalar=-TWO_PI, in1=ang[:], op0=ALU.mult, op1=ALU.add),
             reads=[qf, ang], writes=[r])
        hi = ts(tl([NGD]), r, math.pi, ALU.is_gt, -TWO_PI, ALU.mult)
        lo = ts(tl([NGD]), r, -math.pi, ALU.is_lt, TWO_PI, ALU.mult)
        r2 = tt(tl([NGD]), r, hi, ALU.add); r3 = tt(tl([NGD]), r2, lo, ALU.add)
        r4 = ts(tl([NGD]), r3, math.pi, ALU.min, -math.pi, ALU.max)
        return act(tl([NGD]), r4, AF.Sin)

    sn = sin_of(th)
    thc = ts(tl([NGD]), th, math.pi / 2, ALU.add)
    cs = sin_of(thc)
    abr = tt(tl([NGD]), mag, cs, ALU.mult); abi = tt(tl([NGD]), mag, sn, ALU.mult)
    nr = ts(tl([NGD]), abr, -1.0, ALU.add)
    den = tt(tl([NGD]), tt(tl([NGD]), are, are, ALU.mult), tt(tl([NGD]), aim, aim, ALU.mult), ALU.add)
    rden = tl([NGD]); P.op("dve", lambda en: en.reciprocal(out=rden[:], in_=den[:]), reads=[den], writes=[rden])
    cfr = tt(tl([NGD]), tt(tl([NGD]), tt(tl([NGD]), nr, are, ALU.mult), tt(tl([NGD]), abi, aim, ALU.mult), ALU.add), rden, ALU.mult)
    cfi = tt(tl([NGD]), tt(tl([NGD]), tt(tl([NGD]), abi, are, ALU.mult), tt(tl([NGD]), nr, aim, ALU.mult), ALU.subtract), rden, ALU.mult)

    def cmul_b(va_r, va_i, vb_r, vb_i, rd, tmp, re, im, neg_im=False):
        m1, m2 = tmp
        P.op("dve", lambda en: en.tensor_tensor(out=m1[:], in0=va_r, in1=vb_r, op=ALU.mult), reads=rd, writes=[m1])
        P.op("dve", lambda en: en.tensor_tensor(out=m2[:], in0=va_i, in1=vb_i, op=ALU.mult), reads=rd, writes=[m2])
        P.op("dve", lambda en: en.tensor_tensor(out=re[:], in0=m1[:], in1=m2[:], op=ALU.subtract), reads=[m1, m2], writes=[re])
        P.op("dve", lambda en: en.tensor_tensor(out=m1[:], in0=va_r, in1=vb_i, op=ALU.mult), reads=rd, writes=[m1])
        P.op("dve", lambda en: en.tensor_tensor(out=m2[:], in0=va_i, in1=vb_r, op=ALU.mult), reads=rd, writes=[m2])
        P.op("dve", lambda en: en.tensor_tensor(out=im[:], in0=m1[:], in1=m2[:], op=ALU.add), reads=[m1, m2], writes=[im])
        if neg_im:
            P.op("dve", lambda en: en.tensor_scalar(out=im[:], in0=im[:], scalar1=-1.0, scalar2=None, op0=ALU.mult), reads=[im], writes=[im])

    Bbr = tl([NGD, 16]); Bbi = tl([NGD, 16]); tb1 = tl([NGD, 16]); tb2 = tl([NGD, 16])
    cmul_b(cfr[:].unsqueeze(2).broadcast_to([NP, NGD, 16]), cfi[:].unsqueeze(2).broadcast_to([NP, NGD, 16]), bre[:], bim[:],
           [cfr, cfi, bre, bim], (tb1, tb2), Bbr, Bbi)
    pwr = tl([NGD, TCH + 1]); pwi = tl([NGD, TCH + 1])
    P.op("dve", lambda en: en.memset(pwr[:], 1.0), writes=[pwr]); P.op("dve", lambda en: en.memset(pwi[:], 0.0), writes=[pwi])
    m1 = tl([NGD]); m2 = tl([NGD]); m3 = tl([NGD]); m4 = tl([NGD])
    for k in range(1, TCH + 1):
        P.op("dve", lambda en: en.tensor_tensor(out=m1[:], in0=pwr[:, :, k - 1], in1=abr[:], op=ALU.mult), reads=[pwr, abr], writes=[m1])
        P.op("dve", lambda en: en.tensor_tensor(out=m2[:], in0=pwi[:, :, k - 1], in1=abi[:], op=ALU.mult), reads=[pwi, abi], writes=[m2])
        P.op("dve", lambda en: en.tensor_tensor(out=m3[:], in0=pwr[:, :, k - 1], in1=abi[:], op=ALU.mult), reads=[pwr, abi], writes=[m3])
        P.op("dve", lambda en: en.tensor_tensor(out=m4[:], in0=pwi[:, :, k - 1], in1=abr[:], op=ALU.mult), reads=[pwi, abr], writes=[m4])
        P.op("dve", lambda en: en.tensor_tensor(out=pwr[:, :, k], in0=m1[:], in1=m2[:], op=ALU.subtract), reads=[m1, m2], writes=[pwr])
        P.op("dve", lambda en: en.tensor_tensor(out=pwi[:, :, k], in0=m3[:], in1=m4[:], op=ALU.add), reads=[m3, m4], writes=[pwi])
    mm = tt(tl([NGD, TCH + 1]), tt(tl([NGD, TCH + 1]), pwr, pwr, ALU.mult), tt(tl([NGD, TCH + 1]), pwi, pwi, ALU.mult), ALU.add)
    rm = tl([NGD, TCH + 1]); P.op("dve", lambda en: en.reciprocal(out=rm[:], in_=mm[:]), reads=[mm], writes=[rm])
    ipr = tt(tl([NGD, TCH + 1]), pwr, rm, ALU.mult)
    ipi = tt(tl([NGD, TCH + 1]), pwi, rm, ALU.mult); ts(ipi, ipi, -1.0, ALU.mult)
    E1r = tl([NGD, TCH]); E1i = tl([NGD, TCH]); E2r = tl([NGD, TCH]); E2i = tl([NGD, TCH])
    for (dst, f_src, b_src) in ((E1r, ipr, pwr), (E1i, ipi, pwi), (E2r, pwr, ipr), (E2i, pwi, ipi)):
        P.op("dve", lambda en: en.tensor_copy(out=dst[:, 0:NG, :], in_=f_src[:, 0:NG, 0:TCH]), reads=[f_src], writes=[dst])
        P.op("dve", lambda en: en.tensor_copy(out=dst[:, NG:NGD, :], in_=b_src[:, NG:NGD, 0:TCH]), reads=[b_src], writes=[dst])
    aTr = tl([NG, B]); aTi = tl([NG, B]); aTrb = tl([NG, B]); aTib = tl([NG, B])
    for (dst, src, lo_) in ((aTr, pwr, 0), (aTi, pwi, 0), (aTrb, pwr, NG), (aTib, pwi, NG)):
        P.op("dve", lambda en: en.tensor_copy(out=dst[:], in_=src[:, lo_:lo_ + NG, TCH:TCH + 1].broadcast_to([NP, NG, B])), reads=[src], writes=[dst])

    shp = [2, TCH, 16]
    gtmp = (tl(shp), tl(shp)); gre = tl(shp); gim = tl(shp)
    X1g = [P.sb("X1g%d" % i, [NP, 2, 2, TCH * 16], BF16) for i in range(2)]
    X2g = [P.sb("X2g%d" % i, [NP, 2, 2, TCH * 16], BF16) for i in range(2)]

    def dv(t_, g):
        return t_[:].rearrange("p (d g) n -> p d g n", d=2)[:, :, g, :]

    def gen_group(g, slot, need2):
        x1 = X1g[slot]; x2 = X2g[slot]
        e_b = lambda t_: dv(t_, g).unsqueeze(3).broadcast_to([NP] + shp)
        c_b = lambda t_: dv(t_, g).unsqueeze(2).broadcast_to([NP] + shp)
        cmul_b(e_b(E1r), e_b(E1i), c_b(Bbr), c_b(Bbi), [E1r, E1i, Bbr, Bbi], gtmp, gre, gim)
        P.op("act", lambda en: en.copy(out=x1[:, 0], in_=gre[:].rearrange("p d s c -> p d (s c)")), reads=[gre], writes=[x1])
        P.op("act", lambda en: en.copy(out=x1[:, 1], in_=gim[:].rearrange("p d s c -> p d (s c)")), reads=[gim], writes=[x1])
        if need2:
            cmul_b(e_b(E2r), e_b(E2i), c_b(cre), c_b(cim), [E2r, E2i, cre, cim], gtmp, gre, gim, neg_im=True)
            P.op("act", lambda en: en.copy(out=x2[:, 0], in_=gre[:].rearrange("p d s c -> p d (s c)")), reads=[gre], writes=[x2])
            P.op("act", lambda en: en.copy(out=x2[:, 1], in_=gim[:].rearrange("p d s c -> p d (s c)")), reads=[gim], writes=[x2])
        return x1, x2

    gps = [P.ps("gps%d" % i, [128, 512]) for i in range(3)]
    tps = [P.ps("tps%d" % i, [128, 512]) for i in range(2)]
    X1T = [P.sb("X1T%d" % i, [128, 2, 4, 2, 64], BF16) for i in range(2)]
    Ub = [P.sb("Ub%d" % i, [128, 4, NU], BF16) for i in range(4)]
    GH = {}
    for d in range(2):
        for c in range(2):
            GH[(d, c)] = P.sb("GH%d%d" % (d, c), [NP, NS, NJ + 1], F32)
            zc = 0 if d == 0 else NJ
            P.op("dve", lambda e: e.memset(GH[(d, c)][:, :, zc:zc + 1], 0.0), writes=[GH[(d, c)]])
    i_ = 0; iu = 0
    for g in range(NG):
        x1, _ = gen_group(g, g % 2, False)
        xt = X1T[g % 2]
        for d in range(2):
            for kt in range(4):
                t_ = tps[i_ % 2]; i_ += 1
                ks = slice(kt * 128, (kt + 1) * 128)
                for c in range(2):
                    P.op("pe", lambda e: e.matmul(t_[:, c * 64:(c + 1) * 64], lhsT=x1[:, c, d, ks], rhs=idn[:], start=True, stop=True), reads=[x1, idn], writes=[t_])
                P.op("act", lambda e: e.copy(out=xt[:, d, kt, :, :], in_=t_[:, 0:128].rearrange("p (a b) -> p a b", a=2)), reads=[t_], writes=[xt])
        for b in range(B):
            s = g * B + b
            ub = Ub[iu % 4]; iu += 1
            P.dma("pool", ub[:], U_d[:, :, g, b, :], writes=[ub], sem_buf=ub)
            for d in range(2):
                j0 = 0 if d == 0 else NCC; o0 = 1 if d == 0 else 0
                for c in range(2):
                    g_ = gps[i_ % 3]; i_ += 1
                    for kt in range(4):
                        P.op("pe", lambda e: e.matmul(g_[0:64, :NJ], lhsT=xt[:, d, kt, c, :], rhs=ub[:, kt, j0:j0 + NJ], start=(kt == 0), stop=(kt == 3)),
                             reads=[xt, ub], writes=[g_], track=(kt == 3))
                    P.op("act", lambda e: e.copy(out=GH[(d, c)][:, s, o0:o0 + NJ], in_=g_[0:64, :NJ]), reads=[g_], writes=[GH[(d, c)]])

    def rec(eng, Hr, Hi, ar_, ai_, j_in, j_io, tmps):
        tr, ti, q1, q2 = tmps
        av = ar_[:].rearrange("p g b -> p (g b)"); aiv = ai_[:].rearrange("p g b -> p (g b)")
        P.op(eng, lambda e: e.tensor_tensor(out=tr[:], in0=Hr[:, :, j_in], in1=Hr[:, :, j_io], op=ALU.add), reads=[Hr], writes=[tr])
        P.op(eng, lambda e: e.tensor_tensor(out=ti[:], in0=Hi[:, :, j_in], in1=Hi[:, :, j_io], op=ALU.add), reads=[Hi], writes=[ti])
        P.op(eng, lambda e: e.tensor_tensor(out=q1[:], in0=tr[:], in1=av, op=ALU.mult), reads=[tr, ar_], writes=[q1])
        P.op(eng, lambda e: e.tensor_tensor(out=q2[:], in0=ti[:], in1=aiv, op=ALU.mult), reads=[ti, ai_], writes=[q2])
        P.op(eng, lambda e: e.tensor_tensor(out=Hr[:, :, j_io], in0=q1[:], in1=q2[:], op=ALU.subtract), reads=[q1, q2], writes=[Hr])
        P.op(eng, lambda e: e.tensor_tensor(out=q1[:], in0=tr[:], in1=aiv, op=ALU.mult), reads=[tr, ai_], writes=[q1])
        P.op(eng, lambda e: e.tensor_tensor(out=q2[:], in0=ti[:], in1=av, op=ALU.mult), reads=[ti, ar_], writes=[q2])
        P.op(eng, lambda e: e.tensor_tensor(out=Hi[:, :, j_io], in0=q1[:], in1=q2[:], op=ALU.add), reads=[q1, q2], writes=[Hi])
    tf = [tl([NS]) for _ in range(4)]; tb = [tl([NS]) for _ in range(4)]
    for j in range(NJ):
        rec("dve", GH[(0, 0)], GH[(0, 1)], aTr, aTi, j, j + 1, tf)
        jb = NJ - 1 - j
        rec("pool", GH[(1, 0)], GH[(1, 1)], aTrb, aTib, jb + 1, jb, tb)

    A0 = [P.sb("A0_%d" % i, [128, 2, 4, 512], BF16) for i in range(2)]
    Hh = [P.sb("Hh%d" % i, [NP, 2, 2, NJ + 1], BF16) for i in range(2)]
    yo = [P.sb("yo%d" % i, [128, 4, NXC + NCC], F32) for i in range(2)]
    for g in range(NG):
        x1, x2 = gen_group(g, g % 2, True)
        a0 = A0[g % 2]
        for d in range(2):
            for kt in range(4):
                g_ = gps[i_ % 3]; i_ += 1
                ks = slice(kt * 128, (kt + 1) * 128)
                P.op("pe", lambda e: e.matmul(g_[:, :], lhsT=x1[:, 0, d, ks], rhs=x2[:, 0, d, :], start=True, stop=False), reads=[x1, x2], writes=[g_], track=False)
                P.op("pe", lambda e: e.matmul(g_[:, :], lhsT=x1[:, 1, d, ks], rhs=x2[:, 1, d, :], start=False, stop=True), reads=[x1, x2], writes=[g_])
                P.op("dve", lambda e: e.tensor_tensor(out=a0[:, d, kt, :], in0=g_[:, :], in1=msk[:, d, kt, :], op=ALU.mult), reads=[g_, msk], writes=[a0])
        for b in range(B):
            s = g * B + b; yt = yo[s % 2]; hh = Hh[s % 2]
            ub = Ub[iu % 4]; iu += 1
            P.dma("pool", ub[:], U_d[:, :, g, b, :], writes=[ub], sem_buf=ub)
            for d in range(2):
                for c in range(2):
                    P.op("act", lambda e: e.copy(out=hh[:, d, c, :], in_=GH[(d, c)][:, s, :]), reads=[GH[(d, c)]], writes=[hh])
            for mt in range(4):
                ms = slice(mt * 128, (mt + 1) * 128)
                segs = [(0, NXC, NCC, NCC, 1)]
                if with_ctx:
                    segs.append((NXC, NCC, 0, 0, NXC + 1))
                for (o0, n, u0f, hf0, hb0) in segs:
                    u0b = u0f if o0 == 0 else NCC + NXC
                    a_ = gps[i_ % 3]; i_ += 1
                    mms = []
                    for kt in range(4):
                        mms.append((a0[:, 0, kt, ms], ub[:, kt, u0f:u0f + n], [a0, ub]))
                        mms.append((a0[:, 1, kt, ms], ub[:, kt, u0b:u0b + n], [a0, ub]))
                    for c in range(2):
                        mms.append((x2[:, c, 0, ms], hh[:, 0, c, hf0:hf0 + n], [x2, hh]))
                        mms.append((x2[:, c, 1, ms], hh[:, 1, c, hb0:hb0 + n], [x2, hh]))
                    for mi, (l_, r_, rd) in enumerate(mms):
                        P.op("pe", lambda e: e.matmul(a_[:, :n], lhsT=l_, rhs=r_, start=(mi == 0), stop=(mi == len(mms) - 1)),
                             reads=rd, writes=[a_], track=(mi == len(mms) - 1))
                    P.op("act", lambda e: e.copy(out=yt[:, mt, o0:o0 + n], in_=a_[:, :n]), reads=[a_], writes=[yt])
            if not with_ctx:
                P.op("dve", lambda e: e.memset(yt[:, :, NXC:], 0.0), writes=[yt])
            P.dma("sp", y_d[:, :, g, b, :], yt[:], reads=[yt], sem_buf=yt, is_output=True)
    return P


def s5_consts():
    k = np.arange(512); n = np.arange(512)
    s = (k // 16)[:, None]; t = (n // 16)[None, :]
    mf = (t >= s).astype(np.float32); mb = (s >= t).astype(np.float32)
    msk = np.stack([mf.reshape(4, 128, 512).transpose(1, 0, 2), mb.reshape(4, 128, 512).transpose(1, 0, 2)], 1)
    return np.ascontiguousarray(msk.astype(NPBF)), np.eye(64, dtype=np.float32)


def s5_pack_u(u_x, u_c):
    st = np.concatenate([u_c, u_x, u_c], 1)
    NU = st.shape[1] // TCH
    a = st.reshape(B, NU, TCH, SW // 16, 16)
    a = a.transpose(2, 4, 3, 0, 1).reshape(TCH * 16, SW // 16, B, NU)
    a = a.reshape(4, 128, SW // 16, B, NU).transpose(1, 0, 2, 3, 4)
    return [np.ascontiguousarray(a[:, :, i * 8:(i + 1) * 8]) for i in range(NCORES)]


def s5_unpack_y(ys):
    a = np.concatenate(ys, 2)
    a = a.transpose(1, 0, 2, 3, 4).reshape(TCH, 16, SW // 16, B, -1)
    a = a.transpose(3, 4, 0, 2, 1).reshape(B, -1, SW)
    return a[:, :SEQ], a[:, SEQ:]


def token_tiles(nlat, nctx, TT):
    tiles = [(c0, min(TT, nlat - c0), 0) for c0 in range(0, nlat, TT)]
    if nctx:
        tiles += [(nlat + c0, min(TT, nctx - c0), 1) for c0 in range(0, nctx, TT)]
    return tiles


def build_post(nlat, nctx, TT=512, P=None):
    P = P or Prog()
    NT = nlat + nctx; KA_ = AW // 128; KS = SW // 128; KT = D // 128
    oT = P.dram("oT", [128, KA_, NT], F32, "ExternalInput")
    yT = P.dram("yT", [128, KS, NT], F32, "ExternalInput")
    uT = P.dram("uT", [128, KS, NT], F32, "ExternalInput")
    xT = P.dram("xT", [D, NT], F32, "ExternalInput")
    dsk = P.dram("dsk", [SW], F32, "ExternalInput")
    wglu = P.dram("wglu", [SW, SW], F32, "ExternalInput")
    goa = P.dram("goa", [AW], F32, "ExternalInput"); gos = P.dram("gos", [SW], F32, "ExternalInput")
    wout = P.dram("wout", [D, D], F32, "ExternalInput")
    gatev = P.dram("gatev", [2, D], F32, "ExternalInput")
    x1T = P.dram("x1T", [D, NT], F32, "ExternalOutput")

    ones = P.sb("ones", [128, 128], BF16); P.op("dve", lambda e: e.memset(ones[:], 1.0), writes=[ones])
    dskt = P.sb("dskt", [128, KS], F32); load_cols(P, dskt, dsk.t)
    goat = P.sb("goat", [128, KA_], F32); load_cols(P, goat, goa.t)
    gost = P.sb("gost", [128, KS], F32); load_cols(P, gost, gos.t)
    gat = [P.sb("gat%d" % j, [128, KT], F32) for j in range(2)]
    for j in range(2):
        load_cols(P, gat[j], gatev[j])
    OT = P.sb("OT", [128, KA_, TT], F32); ok = [Buf("ok%d" % i) for i in range(KA_)]
    YG = P.sb("YG", [128, KS, TT], F32); ygk = [Buf("ygk%d" % i) for i in range(KS)]
    YB = P.sb("YB", [128, KS, TT], BF16); ybk = [Buf("ybk%d" % i) for i in range(KS)]
    OS = P.sb("OS", [128, KS, TT], F32); osk = [Buf("osk%d" % i) for i in range(KS)]
    NTt = P.sb("NTt", [128, KT, TT], BF16); nk = [Buf("nk%d" % i) for i in range(KT)]
    yin = [P.sb("yin%d" % i, [128, TT], F32) for i in range(2)]
    uin = [P.sb("uin%d" % i, [128, TT], F32) for i in range(2)]
    e1 = [P.sb("e1_%d" % i, [128, TT], F32) for i in range(2)]
    e2 = [P.sb("e2_%d" % i, [128, TT], F32) for i in range(2)]
    e3 = [P.sb("e3_%d" % i, [128, TT], F32) for i in range(2)]
    sq = [P.sb("sq%d" % i, [128, TT], BF16) for i in range(2)]
    rs_a = P.sb("rs_a", [128, TT], F32); rs_s = P.sb("rs_s", [128, TT], F32); rtmp = P.sb("rtmp", [128, TT], F32)
    wg = [P.sb("wg%d" % i, [128, KS, 128], BF16) for i in range(2)]
    wt = [P.sb("wt%d" % i, [128, KT, 128], BF16) for i in range(3)]
    xin = [P.sb("xin%d" % i, [128, TT], F32) for i in range(3)]
    ssq = [P.ps("ssq%d" % i, [128, 512]) for i in range(2)]
    acc = [P.ps("acc%d" % i, [128, 512]) for i in range(3)]
    GC = 2.0 * math.sqrt(2.0 / math.pi)
    it = 0
    for (c0, n, isctx) in token_tiles(nlat, nctx, TT):
        for kt in range(KS):
            yi = yin[kt % 2]; ui = uin[kt % 2]; a1 = e1[kt % 2]; a2 = e2[kt % 2]; a3 = e3[kt % 2]
            P.dma("sp", yi[:, :n], yT[:, kt, c0:c0 + n], writes=[yi], sem_buf=yi)
            P.dma("sp", ui[:, :n], uT[:, kt, c0:c0 + n], writes=[ui], sem_buf=ui)
            P.op("dve", lambda e: e.scalar_tensor_tensor(out=a1[:, :n], in0=ui[:, :n], scalar=dskt[:, kt:kt + 1], in1=yi[:, :n], op0=ALU.mult, op1=ALU.add),
                 reads=[ui, yi, dskt], writes=[a1])
            P.op("act", lambda e: e.activation(out=a2[:, :n], in_=a1[:, :n], func=AF.Square), reads=[a1], writes=[a2])
            P.op("pool", lambda e: e.tensor_scalar(out=a2[:, :n], in0=a2[:, :n], scalar1=0.044715, scalar2=1.0, op0=ALU.mult, op1=ALU.add), reads=[a2], writes=[a2])
            P.op("pool", lambda e: e.tensor_tensor(out=a3[:, :n], in0=a2[:, :n], in1=a1[:, :n], op=ALU.mult), reads=[a1, a2], writes=[a3])
            P.op("act", lambda e: e.activation(out=a3[:, :n], in_=a3[:, :n], func=AF.Sigmoid, scale=GC), reads=[a3], writes=[a3])
            P.op("dve", lambda e: e.tensor_tensor(out=YG[:, kt, :n], in0=a1[:, :n], in1=a3[:, :n], op=ALU.mult), reads=[a1, a3], writes=[ygk[kt]])
            P.op("pool", lambda e: e.tensor_copy(out=YB[:, kt, :n], in_=YG[:, kt, :n]), reads=[ygk[kt]], writes=[ybk[kt]])
        for m in range(KS):
            wb = wg[m % 2]; a = acc[it % 3]; it += 1
            P.dma("pool", wb[:], wglu[:, m * 128:(m + 1) * 128].rearrange("(kt p) m -> p kt m", p=128), writes=[wb], sem_buf=wb)
            for kt in range(KS):
                P.op("pe", lambda e: e.matmul(a[:, :n], lhsT=wb[:, kt, :], rhs=YB[:, kt, :n], start=(kt == 0), stop=(kt == KS - 1)),
                     reads=[wb, ybk[kt]], writes=[a], track=(kt == KS - 1))
            s_ = e1[m % 2]
            P.op("act", lambda e: e.activation(out=s_[:, :n], in_=a[:, :n], func=AF.Sigmoid), reads=[a], writes=[s_])
            P.op("dve", lambda e: e.tensor_tensor(out=OS[:, m, :n], in0=YG[:, m, :n], in1=s_[:, :n], op=ALU.mult), reads=[ygk[m], s_], writes=[osk[m]])
        for kt in range(KS):
            s = sq[kt % 2]
            P.op("act", lambda e: e.activation(out=s[:, :n], in_=OS[:, kt, :n], func=AF.Square), reads=[osk[kt]], writes=[s])
            P.op("pe", lambda e: e.matmul(ssq[0][:, :n], lhsT=ones[:], rhs=s[:, :n], start=(kt == 0), stop=(kt == KS - 1)), reads=[s, ones], writes=[ssq[0]])
        rstd_from_ssq(P, ssq[0], rs_s, rtmp, n, SW)
        for kt in range(KA_):
            s = sq[kt % 2]
            P.dma("sp", OT[:, kt, :n], oT[:, kt, c0:c0 + n], writes=[ok[kt]], sem_buf=ok[kt])
            P.op("act", lambda e: e.activation(out=s[:, :n], in_=OT[:, kt, :n], func=AF.Square), reads=[ok[kt]], writes=[s])
            P.op("pe", lambda e: e.matmul(ssq[1][:, :n], lhsT=ones[:], rhs=s[:, :n], start=(kt == 0), stop=(kt == KA_ - 1)), reads=[s, ones], writes=[ssq[1]])
        rstd_from_ssq(P, ssq[1], rs_a, rtmp, n, AW)
        for kt in range(KT):
            a1 = e2[kt % 2]
            if kt < KA_:
                src = OT[:, kt, :n]; sb_ = ok[kt]; rs = rs_a; gsc = goat[:, kt:kt + 1]; gb_ = goat
            else:
                src = OS[:, kt - KA_, :n]; sb_ = osk[kt - KA_]; rs = rs_s; gsc = gost[:, kt - KA_:kt - KA_ + 1]; gb_ = gost
            P.op("dve", lambda e: e.tensor_tensor(out=a1[:, :n], in0=src, in1=rs[:, :n], op=ALU.mult), reads=[sb_, rs], writes=[a1])
            P.op("act", lambda e: e.activation(out=NTt[:, kt, :n], in_=a1[:, :n], func=AF.Copy, scale=gsc), reads=[a1, gb_], writes=[nk[kt]])
        for m in range(KT):
            wb = wt[m % 3]; a = acc[it % 3]; it += 1; xi = xin[m % 3]
            P.dma("pool", wb[:], wout[:, m * 128:(m + 1) * 128].rearrange("(kt p) m -> p kt m", p=128), writes=[wb], sem_buf=wb)
            P.dma("sp", xi[:, :n], xT[m * 128:(m + 1) * 128, c0:c0 + n], writes=[xi], sem_buf=xi)
            for kt in range(KT):
                P.op("pe", lambda e: e.matmul(a[:, :n], lhsT=wb[:, kt, :], rhs=NTt[:, kt, :n], start=(kt == 0), stop=(kt == KT - 1)),
                     reads=[wb, nk[kt]], writes=[a], track=(kt == KT - 1))
            P.op("dve", lambda e: e.scalar_tensor_tensor(out=xi[:, :n], in0=a[:, :n], scalar=gat[isctx][:, m:m + 1], in1=xi[:, :n], op0=ALU.mult, op1=ALU.add),
                 reads=[a, xi, gat[isctx]], writes=[xi])
            P.dma("sp", x1T[m * 128:(m + 1) * 128, c0:c0 + n], xi[:, :n], reads=[xi], sem_buf=xi, is_output=True)
    return P


def build_ffn(nlat, nctx, moe, final, TT=512, P=None):
    P = P or Prog()
    NT = nlat + nctx; KT = D // 128
    xT = P.dram("x1T", [D, NT], F32, "ExternalInput")
    modv = P.dram("modv", [2, 3, D], F32, "ExternalInput")
    gvec = P.dram("g", [D], F32, "ExternalInput")
    if moe:
        chunks = [(e, DE // 128) for e in range(NE)]
        wg = P.dram("wg", [NE, D, DE], F32, "ExternalInput"); wu = P.dram("wu", [NE, D, DE], F32, "ExternalInput")
        wd = P.dram("wd", [NE, DE, D], F32, "ExternalInput")
        wr = P.dram("wr", [D, NE], F32, "ExternalInput")
        idn_d = P.dram("idn", [128, 128], F32, "ExternalInput")
    else:
        HM = DFF // 128
        chunks = [(0, HM // 2), (HM // 2, HM - HM // 2)]
        wg = P.dram("wg", [D, DFF], F32, "ExternalInput"); wu = P.dram("wu", [D, DFF], F32, "ExternalInput")
        wd = P.dram("wd", [DFF, D], F32, "ExternalInput")
    HC = max(c[1] for c in chunks)
    if final:
        gfin = P.dram("gfin", [D], F32, "ExternalInput")
        x2T = P.dram("x2s", [D, NT], F32, "Internal")
        outT = P.dram("outT", [D, NT], F32, "ExternalOutput")
    else:
        x2T = P.dram("x2T", [D, NT], F32, "ExternalOutput")
    x2b = [Buf("x2b%d" % m) for m in range(KT)]

    ones = P.sb("ones", [128, 128], BF16); P.op("dve", lambda e: e.memset(ones[:], 1.0), writes=[ones])
    gt = P.sb("gt", [128, KT], F32); load_cols(P, gt, gvec.t)
    gs = []; sh = []; gf = []
    for j in range(2):
        s_ = P.sb("sh%d" % j, [128, KT], F32); load_cols(P, s_, modv[j, 0])
        c_ = P.sb("sc%d" % j, [128, KT], F32); load_cols(P, c_, modv[j, 1])
        f_ = P.sb("gf%d" % j, [128, KT], F32); load_cols(P, f_, modv[j, 2])
        g_ = P.sb("gs%d" % j, [128, KT], F32)
        P.op("dve", lambda e: e.scalar_tensor_tensor(out=g_[:], in0=c_[:], scalar=1.0, in1=gt[:], op0=ALU.add, op1=ALU.mult), reads=[c_, gt], writes=[g_])
        gs.append(g_); sh.append(s_); gf.append(f_)
    if final:
        gfn = P.sb("gfn", [128, KT], F32); load_cols(P, gfn, gfin.t)
    if moe:
        wrt = P.sb("wrt", [128, KT, NE], F32)
        P.dma("sp", wrt[:], wr.t.rearrange("(kt p) e -> p kt e", p=128), writes=[wrt], sem_buf=wrt, allow_slow_non_contiguous=True)
        idn = P.sb("idn", [128, 128], F32); P.dma("sp", idn[:], idn_d[:, :], writes=[idn], sem_buf=idn)
        GT = P.sb("GT", [128, NE, TT], F32)
        h32 = [P.sb("h32_%d" % i, [128, TT], F32) for i in range(2)]
        rps = [P.ps("rps%d" % i, [128, 512]) for i in range(4)]
        lg = P.sb("lg", [128, NE], F32); l2 = P.sb("l2", [128, NE], F32); eq1 = P.sb("eq1", [128, NE], F32); eq2 = P.sb("eq2", [128, NE], F32)
        mx1 = P.sb("mx1", [128, 1], F32); mx2 = P.sb("mx2", [128, 1], F32); dd = P.sb("dd", [128, 1], F32)
        w1 = P.sb("w1", [128, 1], F32); w2 = P.sb("w2", [128, 1], F32); gg = P.sb("gg", [128, NE], F32)
        gbb = [P.sb("gbb%d" % i, [128, 128], F32) for i in range(2)]
    hT = P.sb("hT", [128, KT, TT], BF16); hk = [Buf("hk%d" % i) for i in range(KT)]
    HID = P.sb("HID", [128, HC, TT], BF16); hidk = [Buf("hid%d" % i) for i in range(HC)]
    xs = [P.sb("xs%d" % i, [128, TT], F32) for i in range(3)]
    sq = [P.sb("sq%d" % i, [128, TT], BF16) for i in range(2)]
    tn = [P.sb("tn%d" % i, [128, TT], F32) for i in range(2)]
    rstd = P.sb("rstd", [128, TT], F32); rtmp = P.sb("rtmp", [128, TT], F32)
    wgt = [P.sb("wgt%d" % i, [128, KT, 128], BF16) for i in range(2)]
    wut = [P.sb("wut%d" % i, [128, KT, 128], BF16) for i in range(2)]
    wdt = [P.sb("wdt%d" % i, [128, HC, 128], BF16) for i in range(2)]
    sg = [P.sb("sg%d" % i, [128, TT], F32) for i in range(2)]
    ug = [P.sb("ug%d" % i, [128, TT], F32) for i in range(2)]
    ssq = P.ps("ssq", [128, 512])
    acc = [P.ps("acc%d" % i, [128, 512]) for i in range(3)]
    it = 0

    def stats(src_dram, src_bufs, c0, n, fdim):
        for kt in range(KT):
            xi = xs[kt % 3]; s = sq[kt % 2]
            P.dma("sp", xi[:, :n], src_dram[kt * 128:(kt + 1) * 128, c0:c0 + n], reads=[src_bufs[kt]] if src_bufs else [], writes=[xi], sem_buf=xi)
            P.op("act", lambda e: e.activation(out=s[:, :n], in_=xi[:, :n], func=AF.Square), reads=[xi], writes=[s])
            P.op("pe", lambda e: e.matmul(ssq[:, :n], lhsT=ones[:], rhs=s[:, :n], start=(kt == 0), stop=(kt == KT - 1)), reads=[s, ones], writes=[ssq])
        rstd_from_ssq(P, ssq, rstd, rtmp, n, fdim)

    for (c0, n, isctx) in token_tiles(nlat, nctx, TT):
        nblk = (n + 127) // 128
        stats(xT, None, c0, n, D)
        for kt in range(KT):
            xi = xs[kt % 3]; t_ = tn[kt % 2]
            P.dma("sp", xi[:, :n], xT[kt * 128:(kt + 1) * 128, c0:c0 + n], writes=[xi], sem_buf=xi)
            P.op("dve", lambda e: e.tensor_tensor(out=t_[:, :n], in0=xi[:, :n], in1=rstd[:, :n], op=ALU.mult), reads=[xi, rstd], writes=[t_])
            if not moe:
                P.op("act", lambda e: e.activation(out=hT[:, kt, :n], in_=t_[:, :n], func=AF.Identity, scale=gs[isctx][:, kt:kt + 1], bias=sh[isctx][:, kt:kt + 1]),
                     reads=[t_, gs[isctx], sh[isctx]], writes=[hk[kt]])
            else:
                h_ = h32[kt % 2]
                P.op("act", lambda e: e.activation(out=h_[:, :n], in_=t_[:, :n], func=AF.Identity, scale=gs[isctx][:, kt:kt + 1], bias=sh[isctx][:, kt:kt + 1]),
                     reads=[t_, gs[isctx], sh[isctx]], writes=[h_])
                P.op("pool", lambda e: e.tensor_copy(out=hT[:, kt, :n], in_=h_[:, :n]), reads=[h_], writes=[hk[kt]])
                for bk in range(nblk):
                    nb_ = min(128, n - bk * 128)
                    P.op("pe", lambda e: e.matmul(rps[bk][:nb_, 0:NE], lhsT=h_[:, bk * 128:bk * 128 + nb_], rhs=wrt[:, kt, :], start=(kt == 0), stop=(kt == KT - 1)),
                         reads=[h_, wrt], writes=[rps[bk]])
        if moe:
            for bk in range(nblk):
                nb_ = min(128, n - bk * 128)
                P.op("dve", lambda e: e.tensor_copy(out=lg[:nb_], in_=rps[bk][:nb_, 0:NE]), reads=[rps[bk]], writes=[lg])
                P.op("dve", lambda e: e.reduce_max(out=mx1[:nb_], in_=lg[:nb_], axis=mybir.AxisListType.X), reads=[lg], writes=[mx1])
                P.op("dve", lambda e: e.tensor_scalar(out=eq1[:nb_], in0=lg[:nb_], scalar1=mx1[:nb_, 0:1], scalar2=None, op0=ALU.is_equal), reads=[lg, mx1], writes=[eq1])
                P.op("dve", lambda e: e.scalar_tensor_tensor(out=l2[:nb_], in0=eq1[:nb_], scalar=-1e30, in1=lg[:nb_], op0=ALU.mult, op1=ALU.add), reads=[eq1, lg], writes=[l2])
                P.op("dve", lambda e: e.reduce_max(out=mx2[:nb_], in_=l2[:nb_], axis=mybir.AxisListType.X), reads=[l2], writes=[mx2])
                P.op("dve", lambda e: e.tensor_scalar(out=eq2[:nb_], in0=l2[:nb_], scalar1=mx2[:nb_, 0:1], scalar2=None, op0=ALU.is_equal), reads=[l2, mx2], writes=[eq2])
                P.op("dve", lambda e: e.tensor_tensor(out=dd[:nb_], in0=mx1[:nb_], in1=mx2[:nb_], op=ALU.subtract), reads=[mx1, mx2], writes=[dd])
                P.op("act", lambda e: e.activation(out=w1[:nb_], in_=dd[:nb_], func=AF.Sigmoid), reads=[dd], writes=[w1])
                P.op("act", lambda e: e.activation(out=w2[:nb_], in_=dd[:nb_], func=AF.Sigmoid, scale=-1.0), reads=[dd], writes=[w2])
                P.op("dve", lambda e: e.tensor_scalar(out=gg[:nb_], in0=eq1[:nb_], scalar1=w1[:nb_, 0:1], scalar2=None, op0=ALU.mult), reads=[eq1, w1], writes=[gg])
                P.op("dve", lambda e: e.scalar_tensor_tensor(out=gg[:nb_], in0=eq2[:nb_], scalar=w2[:nb_, 0:1], in1=gg[:nb_], op0=ALU.mult, op1=ALU.add), reads=[eq2, w2, gg], writes=[gg])
                for ex in range(NE):
                    gb_ = gbb[ex % 2]; a = acc[it % 3]; it += 1
                    P.op("dve", lambda e: e.tensor_copy(out=gb_[:nb_, :], in_=gg[:nb_, ex:ex + 1].broadcast_to([nb_, 128])), reads=[gg], writes=[gb_])
                    P.op("pe", lambda e: e.matmul(a[:, :nb_], lhsT=gb_[:nb_, :], rhs=idn[:nb_, :nb_], start=True, stop=True), reads=[gb_, idn], writes=[a])
                    P.op("act", lambda e: e.copy(out=GT[:, ex, bk * 128:bk * 128 + nb_], in_=a[:, :nb_]), reads=[a], writes=[GT])
        for ci, (cb, cn) in enumerate(chunks):
            for m in range(cn):
                wgb = wgt[m % 2]; wub = wut[m % 2]
                if moe:
                    gsrc = wg[cb, :, m * 128:(m + 1) * 128]; usrc = wu[cb, :, m * 128:(m + 1) * 128]
                else:
                    gsrc = wg[:, (cb + m) * 128:(cb + m + 1) * 128]; usrc = wu[:, (cb + m) * 128:(cb + m + 1) * 128]
                P.dma("pool", wgb[:], gsrc.rearrange("(kt p) m -> p kt m", p=128), writes=[wgb], sem_buf=wgb)
                P.dma("pool", wub[:], usrc.rearrange("(kt p) m -> p kt m", p=128), writes=[wub], sem_buf=wub)
                ag = acc[it % 3]; it += 1; au = acc[it % 3]; it += 1
                for kt in range(KT):
                    P.op("pe", lambda e: e.matmul(ag[:, :n], lhsT=wgb[:, kt, :], rhs=hT[:, kt, :n], start=(kt == 0), stop=(kt == KT - 1)),
                         reads=[wgb, hk[kt]], writes=[ag], track=(kt == KT - 1))
                for kt in range(KT):
                    P.op("pe", lambda e: e.matmul(au[:, :n], lhsT=wub[:, kt, :], rhs=hT[:, kt, :n], start=(kt == 0), stop=(kt == KT - 1)),
                         reads=[wub, hk[kt]], writes=[au], track=(kt == KT - 1))
                s_ = sg[m % 2]; u_ = ug[m % 2]
                P.op("act", lambda e: e.activation(out=s_[:, :n], in_=ag[:, :n], func=AF.Silu), reads=[ag], writes=[s_])
                if moe:
                    P.op("dve", lambda e: e.tensor_tensor(out=u_[:, :n], in0=au[:, :n], in1=GT[:, cb, :n], op=ALU.mult), reads=[au, GT], writes=[u_])
                    P.op("pool", lambda e: e.tensor_tensor(out=HID[:, m, :n], in0=s_[:, :n], in1=u_[:, :n], op=ALU.mult), reads=[s_, u_], writes=[hidk[m]])
                else:
                    P.op("dve", lambda e: e.tensor_tensor(out=HID[:, m, :n], in0=au[:, :n], in1=s_[:, :n], op=ALU.mult), reads=[au, s_], writes=[hidk[m]])
            for mo in range(KT):
                wdb = wdt[mo % 2]; a = acc[it % 3]; it += 1; xi = xs[mo % 3]
                if moe:
                    dsrc = wd[cb, :, mo * 128:(mo + 1) * 128]
                else:
                    dsrc = wd[cb * 128:(cb + cn) * 128, mo * 128:(mo + 1) * 128]
                P.dma("pool", wdb[:, :cn, :], dsrc.rearrange("(kt p) m -> p kt m", p=128), writes=[wdb], sem_buf=wdb)
                if ci == 0:
                    P.dma("sp", xi[:, :n], xT[mo * 128:(mo + 1) * 128, c0:c0 + n], writes=[xi], sem_buf=xi)
                else:
                    P.dma("sp", xi[:, :n], x2T[mo * 128:(mo + 1) * 128, c0:c0 + n], reads=[x2b[mo]], writes=[xi], sem_buf=xi)
                for kt in range(cn):
                    P.op("pe", lambda e: e.matmul(a[:, :n], lhsT=wdb[:, kt, :], rhs=HID[:, kt, :n], start=(kt == 0), stop=(kt == cn - 1)),
                         reads=[wdb, hidk[kt]], writes=[a], track=(kt == cn - 1))
                P.op("dve", lambda e: e.scalar_tensor_tensor(out=xi[:, :n], in0=a[:, :n], scalar=gf[isctx][:, mo:mo + 1], in1=xi[:, :n], op0=ALU.mult, op1=ALU.add),
                     reads=[a, xi, gf[isctx]], writes=[xi])
                P.dma("sp", x2T[mo * 128:(mo + 1) * 128, c0:c0 + n], xi[:, :n], reads=[xi], writes=[x2b[mo]], sem_buf=xi, is_output=not final)
        if final:
            stats(x2T, x2b, c0, n, D)
            for kt in range(KT):
                xi = xs[kt % 3]; t_ = tn[kt % 2]
                P.dma("sp", xi[:, :n], x2T[kt * 128:(kt + 1) * 128, c0:c0 + n], reads=[x2b[kt]], writes=[xi], sem_buf=xi)
                P.op("dve", lambda e: e.tensor_tensor(out=t_[:, :n], in0=xi[:, :n], in1=rstd[:, :n], op=ALU.mult), reads=[xi, rstd], writes=[t_])
                P.op("act", lambda e: e.activation(out=t_[:, :n], in_=t_[:, :n], func=AF.Copy, scale=gfn[:, kt:kt + 1]), reads=[t_, gfn], writes=[t_])
                P.dma("sp", outT[kt * 128:(kt + 1) * 128, c0:c0 + n], t_[:, :n], reads=[t_], sem_buf=t_, is_output=True)
    return P


NLAT = SEQ // 4
NCTX = CTX // 4
_PROGS = {}


def _prog(key, fn):
    if key not in _PROGS:
        _PROGS[key] = fn()
        _PROGS[key].finish()
    return _PROGS[key]


def _launch(P, ims):
    res = run_bass_kernel_spmd(P.nc, ims, core_ids=list(range(NCORES)))
    return res.results


def _ca(a):
    return np.ascontiguousarray(a)


def build_attn_s5(NB, NCQ, with_ctx):
    P = Prog()
    build_attn(NB, NCQ, P=P)
    P.barrier(); P.release()
    build_s5(8, with_ctx, P=P)
    return P


def build_ffn_pre(nlat, nctx):
    P = Prog()
    build_ffn(nlat, nctx, False, False, P=P)
    P.barrier(); P.release()
    P.over = {"xT": P.handles["x2T"]}; P.prefix = "p_"
    build_pre(nlat, nctx, P=P)
    return P


def _pre_inputs(l, i, mods, W, pre=""):
    b, r = divmod(i, 4)
    cosT, sinT, Rm = rope_consts(r * NLAT, NLAT)
    modv = np.stack([np.stack([mods[l, 0, :, b], mods[l, 1, :, b]]), np.stack([mods[l, 0, :, 2], mods[l, 1, :, 2]])])
    return {pre + "w": W["w_in"][l], pre + "modv": _ca(modv), pre + "g": W["g_attn_norm"][l], pre + "cosT": cosT, pre + "sinT": sinT,
            pre + "rmat": Rm}


def _attn_s5(l, ra, W, ncq, pre=""):
    g_ = lambda i, k: ra[i][pre + k]
    with_ctx = ncq > 0
    P = _prog(("attn_s5", ncq), lambda: build_attn_s5(NLAT // 128, ncq, with_ctx))
    zk = np.zeros((128, NKV, 128), NPBF)

    def tokmaj(a):
        return a.transpose(2, 1, 0).reshape(a.shape[2], -1)
    u_x = np.stack([np.concatenate([tokmaj(g_(4 * b + j, "uu")[:, :, :NLAT]) for j in range(4)], 0) for b in range(B)])
    u_c = np.stack([np.concatenate([tokmaj(g_(4 * b + j, "uu")[:, :, NLAT:]) for j in range(4)], 0) for b in range(B)])
    Us = s5_pack_u(u_x, u_c)
    msk, idn = s5_consts()
    ims = []
    for i in range(NCORES):
        b, r = divmod(i, 4)
        kk = g_(i, "kk"); vv = g_(i, "vv")
        kl = g_(i - 1, "kk")[:, :, NLAT - 128:NLAT] if r > 0 else zk
        kr_ = g_(i + 1, "kk")[:, :, 0:128] if r < 3 else zk
        vl = g_(i - 1, "vv")[:, :, NLAT - 128:NLAT] if r > 0 else zk
        vr_ = g_(i + 1, "vv")[:, :, 0:128] if r < 3 else zk
        krh = np.concatenate([kl, kk[:, :, :NLAT], kr_], 2)
        vh = np.concatenate([vl, vv[:, :, :NLAT], vr_], 2)
        vt = vh.reshape(128, NKV, -1, 128).transpose(3, 2, 1, 0).reshape(128, -1, KVW)
        kc = np.concatenate([g_(4 * b + j, "kk")[:, :, NLAT:] for j in range(4)], 2)
        vcf = np.concatenate([g_(4 * b + j, "vv")[:, :, NLAT:] for j in range(4)], 2)
        vct = vcf.reshape(128, NKV, -1, 128).transpose(3, 2, 1, 0).reshape(128, -1, KVW)
        gsl = slice(i * 8, (i + 1) * 8)
        ims.append({"qr": g_(i, "qr"), "qp": _ca(g_(i, "qp")[:, :, :NLAT + ncq]), "kr": _ca(krh), "v": _ca(vt), "kc": _ca(kc), "vc": _ca(vct),
                    "mask": attn_masks(r == 0, r == 3), "sink": W["attn_sink"][l],
                    "U": Us[i], "a_re": _ca(W["ssm_a_re"][l][:, gsl]), "a_im": _ca(W["ssm_a_im"][l][:, gsl]),
                    "log_dt": _ca(W["ssm_log_dt"][l][:, gsl].reshape(-1)), "b_re": _ca(W["ssm_b_re"][l][:, gsl]), "b_im": _ca(W["ssm_b_im"][l][:, gsl]),
                    "c_re": _ca(W["ssm_c_re"][l][:, gsl]), "c_im": _ca(W["ssm_c_im"][l][:, gsl]), "msk": msk, "idn": idn})
    rb = _launch(P, ims)
    y_x, y_c = s5_unpack_y([r_["y"] for r_ in rb])
    return [r_["oT"] for r_ in rb], y_x, y_c


def _post_inputs(l, i, oT, y_x, y_c, uu, xT, mods, W, nctx_post):
    b, r = divmod(i, 4)
    ntp = NLAT + nctx_post
    yt = y_x[b, r * NLAT:(r + 1) * NLAT]
    if nctx_post:
        yt = np.concatenate([yt, y_c[b, r * NCTX:(r + 1) * NCTX]], 0)
    yT = yt.reshape(ntp, SW // 128, 128).transpose(2, 1, 0)
    gatev = np.stack([mods[l, 2, :, b], mods[l, 2, :, 2]])
    return {"oT": _ca(oT[:, :, :ntp]), "yT": _ca(yT), "uT": _ca(uu[:, :, :ntp]), "xT": _ca(xT[:, :ntp]),
            "dsk": W["ssm_d"][l], "wglu": W["w_glu"][l], "goa": W["g_out_attn"][l], "gos": W["g_out_ssm"][l], "wout": W["w_out"][l],
            "gatev": _ca(gatev)}


def _ffn_modv(l, i, mods):
    b, r = divmod(i, 4)
    return _ca(np.stack([np.stack([mods[l, 3 + j, :, b] for j in range(3)]), np.stack([mods[l, 3 + j, :, 2] for j in range(3)])]))


def kernel(x, c, ctx, c_ctx, w_mod, b_mod, g_attn_norm, w_in, attn_sink, ssm_a_re, ssm_a_im, ssm_log_dt, ssm_b_re, ssm_b_im,
           ssm_c_re, ssm_c_im, ssm_d, w_glu, g_out_attn, g_out_ssm, w_out, g_ffn_norm, w_ff_gate, w_ff_up, w_ff_down, w_router,
           w_exp_gate, w_exp_up, w_exp_down, g_final):
    W = {k: np.asarray(v) for k, v in dict(
        g_attn_norm=g_attn_norm, w_in=w_in, attn_sink=attn_sink, ssm_a_re=ssm_a_re, ssm_a_im=ssm_a_im, ssm_log_dt=ssm_log_dt,
        ssm_b_re=ssm_b_re, ssm_b_im=ssm_b_im, ssm_c_re=ssm_c_re, ssm_c_im=ssm_c_im, ssm_d=ssm_d, w_glu=w_glu, g_out_attn=g_out_attn,
        g_out_ssm=g_out_ssm, w_out=w_out, g_ffn_norm=g_ffn_norm).items()}
    x = np.asarray(x); ctx = np.asarray(ctx)
    mods = run_mod(np.asarray(c), np.asarray(c_ctx), np.asarray(w_mod), np.asarray(b_mod))
    xTs = []
    for i in range(NCORES):
        b, r = divmod(i, 4)
        xTs.append(_ca(np.concatenate([x[b, r * NLAT:(r + 1) * NLAT], ctx[b, r * NCTX:(r + 1) * NCTX]], 0).T))
    P = _prog(("pre",), lambda: build_pre(NLAT, NCTX))
    ims = []
    for i in range(NCORES):
        d = _pre_inputs(0, i, mods, W); d["xT"] = xTs[i]; ims.append(d)
    ra = _launch(P, ims)
    oT, y_x, y_c = _attn_s5(0, ra, W, NCTX)
    P = _prog(("post", NCTX), lambda: build_post(NLAT, NCTX))
    ims = [_post_inputs(0, i, oT[i], y_x, y_c, ra[i]["uu"], xTs[i], mods, W, NCTX) for i in range(NCORES)]
    x1 = [r_["x1T"] for r_ in _launch(P, ims)]
    del ra, oT, y_x, y_c, xTs
    P = _prog(("ffn_pre",), lambda: build_ffn_pre(NLAT, NCTX))
    ims = []
    for i in range(NCORES):
        d = {"x1T": x1[i], "modv": _ffn_modv(0, i, mods), "g": W["g_ffn_norm"][0], "wg": np.asarray(w_ff_gate)[0], "wu": np.asarray(w_ff_up)[0],
             "wd": np.asarray(w_ff_down)[0]}
        d.update(_pre_inputs(1, i, mods, W, "p_"))
        ims.append(d)
    r1 = _launch(P, ims)
    x2 = [r_["x2T"] for r_ in r1]
    oT, y_x, y_c = _attn_s5(1, r1, W, 0, "p_")
    P = _prog(("post", 0), lambda: build_post(NLAT, 0))
    ims = [_post_inputs(1, i, oT[i], y_x, y_c, r1[i]["p_uu"], x2[i], mods, W, 0) for i in range(NCORES)]
    x1 = [r_["x1T"] for r_ in _launch(P, ims)]
    del r1, oT, y_x, y_c, x2
    P = _prog(("ffn", 1), lambda: build_ffn(NLAT, 0, True, True))
    ims = []
    for i in range(NCORES):
        ims.append({"x1T": x1[i], "modv": _ffn_modv(1, i, mods), "g": W["g_ffn_norm"][1], "wg": np.asarray(w_exp_gate)[0], "wu": np.asarray(w_exp_up)[0],
                    "wd": np.asarray(w_exp_down)[0], "wr": np.asarray(w_router)[0], "idn": np.eye(128, dtype=np.float32), "gfin": np.asarray(g_final)})
    ro = _launch(P, ims)
    out = np.empty((B, SEQ, D), np.float32)
    for i in range(NCORES):
        b, r = divmod(i, 4)
        out[b, r * NLAT:(r + 1) * NLAT] = ro[i]["outT"].T
    return out
```

```python
import math
import numpy as np
import ml_dtypes
import concourse.bass as bass
import concourse.mybir as mybir
from concourse.bass_utils import run_bass_kernel_spmd

F32 = mybir.dt.float32
BF16 = mybir.dt.bfloat16
I32 = mybir.dt.int32
AF = mybir.ActivationFunctionType
ALU = mybir.AluOpType
NPBF = ml_dtypes.bfloat16
NCORES = 8

D = 4096; NQH = 24; NKV = 8; HD = 128; AW = 3072; KVW = 1024; SW = 1024
INC = 6144; DFF = 11008; NE = 8; DE = 4096; CTX = 256; SEQ = 8192; B = 2
EPS = 1e-6; SCALE = HD ** -0.5
TCH = 32
NCH = (SEQ + CTX) // TCH


class Buf:
    __slots__ = ("name", "w", "r", "dsem", "dcnt")

    def __init__(self, name):
        self.name = name; self.w = None; self.r = {}; self.dsem = None; self.dcnt = 0


class T:
    def __init__(self, t, b):
        self.t = t; self.b = b

    def __getitem__(self, k):
        return self.t[k]


class Prog:
    def __init__(self):
        self.nc = bass.Bass("TRN2", target_bir_lowering=False)
        nc = self.nc
        self.engs = {"pe": nc.tensor, "dve": nc.vector, "act": nc.scalar, "pool": nc.gpsimd, "sp": nc.sync}
        self.esem = {k: nc.semaphore("es_" + k).__enter__() for k in self.engs}
        self.ecnt = {k: 0 for k in self.engs}
        self.seen = {k: {} for k in self.engs}
        self.pend = {k: ([], []) for k in self.engs}
        self.nsem = len(self.engs)
        self.out_toks = []
        self.uid = 0
        self.live = []
        self.dbufs = []
        self.free_sems = []
        self.prefix = ""
        self.over = {}
        self.kind_over = {}
        self.handles = {}

    def sb(self, name, shape, dt):
        self.uid += 1
        cm = self.nc.sbuf_tensor(f"{name}_{self.uid}", list(shape), dt)
        t = cm.__enter__(); self.live.append(("sb", cm))
        return T(t, Buf(name))

    def ps(self, name, shape, dt=F32):
        self.uid += 1
        cm = self.nc.psum_tensor(f"{name}_{self.uid}", list(shape), dt)
        t = cm.__enter__(); self.live.append(("ps", cm))
        return T(t, Buf(name))

    def dram(self, name, shape, dt, kind="Internal"):
        if name in self.over:
            return self.over[name]
        kind = self.kind_over.get(name, kind)
        t = T(self.nc.dram_tensor(self.prefix + name, list(shape), dt, kind=kind).ap(), Buf(name))
        self.handles[name] = t
        return t

    def barrier(self):
        toks = [(self.esem[k], self.ecnt[k]) for k in self.engs if self.ecnt[k] > 0]
        toks += [(b.dsem, b.dcnt) for b in self.dbufs if b.dcnt > 0]
        for e in self.engs:
            for t in toks:
                if t[0] is not self.esem[e]:
                    self._wait(e, t)

    def release(self, kinds=("sb", "ps")):
        keep = []
        for k, cm in reversed(self.live):
            if k in kinds:
                cm.__exit__(None, None, None)
            else:
                keep.append((k, cm))
        self.live = list(reversed(keep))
        if "sb" not in kinds:
            return
        for b in self.dbufs:
            self.free_sems.append((b.dsem, b.dcnt))
        self.dbufs = []

    def _wait(self, e, tok):
        sem, val = tok
        if self.seen[e].get(id(sem), 0) >= val:
            return
        self.engs[e].wait_ge(sem, val)
        self.seen[e][id(sem)] = val

    def _deps(self, e, reads, writes):
        toks = []
        for b in reads:
            if b.w is not None:
                toks.append(b.w)
        for b in writes:
            if b.w is not None:
                toks.append(b.w)
            toks.extend(b.r.values())
        for t in toks:
            if t[0] is self.esem[e] and e == "pe":
                continue
            self._wait(e, t)

    def _commit(self, tok, reads, writes):
        for b in writes:
            b.w = tok; b.r = {}
        for b in reads:
            k = id(tok[0])
            if k not in b.r or b.r[k][1] < tok[1]:
                b.r[k] = tok

    def op(self, e, fn, reads=(), writes=(), track=True):
        reads = [x.b if isinstance(x, T) else x for x in reads]
        writes = [x.b if isinstance(x, T) else x for x in writes]
        self._deps(e, reads, writes)
        ins = fn(self.engs[e])
        pr, pw = self.pend[e]
        if not track:
            pr.extend(reads); pw.extend(writes)
            return ins
        self.ecnt[e] += 1
        ins.then_inc(self.esem[e], 1)
        tok = (self.esem[e], self.ecnt[e])
        self._commit(tok, reads + pr, writes + pw)
        self.pend[e] = ([], [])
        return ins

    def dma(self, q, out, in_, reads=(), writes=(), sem_buf=None, is_output=False, **kw):
        reads = [x.b if isinstance(x, T) else x for x in reads]
        writes = [x.b if isinstance(x, T) else x for x in writes]
        self._deps(q, reads, writes)
        sb = sem_buf.b if isinstance(sem_buf, T) else sem_buf
        if sb.dsem is None:
            if self.free_sems:
                sb.dsem, sb.dcnt = self.free_sems.pop()
            else:
                sb.dsem = self.nc.semaphore("ds_%d" % self.nsem).__enter__(); self.nsem += 1
            self.dbufs.append(sb)
        sb.dcnt += 16
        ins = self.engs[q].dma_start(out=out, in_=in_, **kw)
        ins.then_inc(sb.dsem, 16)
        tok = (sb.dsem, sb.dcnt)
        self._commit(tok, reads, writes)
        if is_output:
            self.out_toks.append(tok)
        return ins

    def finish(self):
        if getattr(self, "_fin", False):
            return
        self._fin = True
        last = {}
        for sem, val in self.out_toks:
            if id(sem) not in last or last[id(sem)][1] < val:
                last[id(sem)] = (sem, val)
        for tok in last.values():
            self._wait("sp", tok)


def run(prog, in_maps):
    prog.finish()
    res = run_bass_kernel_spmd(prog.nc, in_maps, core_ids=list(range(NCORES)))
    return res.results


def build_mod(ncol):
    P = Prog()
    KT = D // 128; MT = ncol // 128
    cv = P.dram("cv", [3, D], F32, "ExternalInput")
    w = P.dram("w", [D, ncol], F32, "ExternalInput")
    bb = P.dram("b", [ncol], F32, "ExternalInput")
    out = P.dram("out", [128, MT, 3], F32, "ExternalOutput")
    cT = P.sb("cT", [128, KT, 3], F32); cS = P.sb("cS", [128, KT, 3], BF16)
    bt = P.sb("bt", [128, MT], F32); ot = P.sb("ot", [128, MT, 3], F32)
    wt = [P.sb("wt%d" % i, [128, KT, 128], BF16) for i in range(3)]
    pp = [P.ps("pp%d" % i, [128, 512]) for i in range(2)]
    for j in range(3):
        P.dma("sp", cT[:, :, j], cv[j].rearrange("(kt p) -> p kt", p=128), writes=[cT], sem_buf=cT,
              allow_slow_non_contiguous=True)
    P.dma("sp", bt[:], bb.t.rearrange("(m p) -> p m", p=128), writes=[bt], sem_buf=bt, allow_slow_non_contiguous=True)
    P.op("act", lambda e: e.activation(out=cS[:], in_=cT[:], func=AF.Silu), reads=[cT], writes=[cS])
    for m in range(MT):
        wb = wt[m % 3]; pt = pp[m % 2]
        P.dma("pool", wb[:], w[:, m * 128:(m + 1) * 128].rearrange("(kt p) m -> p kt m", p=128), writes=[wb], sem_buf=wb)
        for kt in range(KT):
            P.op("pe", lambda e: e.matmul(pt[:, 0:3], lhsT=wb[:, kt, :], rhs=cS[:, kt, :], start=(kt == 0), stop=(kt == KT - 1)),
                 reads=[wb, cS], writes=[pt], track=(kt == KT - 1))
        P.op("dve", lambda e: e.tensor_scalar(out=ot[:, m, :], in0=pt[:, 0:3], scalar1=bt[:, m:m + 1], scalar2=None, op0=ALU.add),
             reads=[pt, bt], writes=[ot])
    P.dma("sp", out[:], ot[:], reads=[ot], writes=[out], sem_buf=ot, is_output=True)
    return P


def run_mod(c, c_ctx, w_mod, b_mod):
    depth = w_mod.shape[0]
    ncol = depth * 6 * D // NCORES
    cv = np.ascontiguousarray(np.concatenate([c, c_ctx[None]], 0))
    wall = w_mod.transpose(1, 0, 2).reshape(D, depth * 6 * D)
    ball = b_mod.reshape(-1)
    P = build_mod(ncol)
    ims = [{"cv": cv, "w": np.ascontiguousarray(wall[:, i * ncol:(i + 1) * ncol]),
            "b": np.ascontiguousarray(ball[i * ncol:(i + 1) * ncol])} for i in range(NCORES)]
    res = run(P, ims)
    o = np.concatenate([r["out"].transpose(1, 0, 2).reshape(ncol, 3) for r in res], 0)
    return o.reshape(depth, 6, D, 3)


def load_cols(P, dst, src_vec, q="sp"):
    P.dma(q, dst[:], src_vec.rearrange("(kt p) -> p kt", p=128), writes=[dst], sem_buf=dst, allow_slow_non_contiguous=True)


def rstd_from_ssq(P, ssq_ps, rstd, tmp, n, fdim):
    P.op("act", lambda e: e.activation(out=tmp[:, :n], in_=ssq_ps[:, :n], func=AF.Sqrt, scale=1.0 / fdim, bias=EPS),
         reads=[ssq_ps], writes=[tmp])
    P.op("dve", lambda e: e.reciprocal(out=rstd[:, :n], in_=tmp[:, :n]), reads=[tmp], writes=[rstd])


def build_pre(nlat, nctx, TT=512, P=None):
    P = P or Prog()
    NT = nlat + nctx; KT = D // 128; MT = INC // 128
    xT = P.dram("xT", [D, NT], F32, "ExternalInput")
    w = P.dram("w", [D, INC], F32, "ExternalInput")
    modv = P.dram("modv", [2, 2, D], F32, "ExternalInput")
    gvec = P.dram("g", [D], F32, "ExternalInput")
    cosd = P.dram("cosT", [128, nlat], F32, "ExternalInput")
    sind = P.dram("sinT", [128, nlat], F32, "ExternalInput")
    rmat = P.dram("rmat", [128, 128], F32, "ExternalInput")
    qr = P.dram("qr", [128, NQH, nlat], BF16, "ExternalOutput")
    qp = P.dram("qp", [128, NQH, NT], BF16, "ExternalOutput")
    kk = P.dram("kk", [128, NKV, NT], BF16, "ExternalOutput")
    vv = P.dram("vv", [128, NKV, NT], BF16, "ExternalOutput")
    uu = P.dram("uu", [128, SW // 128, NT], F32, "ExternalOutput")

    ones = P.sb("ones", [128, 128], BF16)
    P.op("dve", lambda e: e.memset(ones[:], 1.0), writes=[ones])
    R = P.sb("R", [128, 128], BF16)
    P.dma("pool", R[:], rmat[:, :], writes=[R], sem_buf=R)
    cosT = P.sb("cos", [128, nlat], F32); sinT = P.sb("sin", [128, nlat], F32)
    P.dma("sp", cosT[:], cosd[:, :], writes=[cosT], sem_buf=cosT)
    P.dma("sp", sinT[:], sind[:, :], writes=[sinT], sem_buf=sinT)
    gt = P.sb("gt", [128, KT], F32); load_cols(P, gt, gvec.t)
    gs = []; sh = []
    for j in range(2):
        s_ = P.sb("sh%d" % j, [128, KT], F32); load_cols(P, s_, modv[j, 0])
        c_ = P.sb("sc%d" % j, [128, KT], F32); load_cols(P, c_, modv[j, 1])
        g_ = P.sb("gs%d" % j, [128, KT], F32)
        P.op("dve", lambda e: e.scalar_tensor_tensor(out=g_[:], in0=c_[:], scalar=1.0, in1=gt[:], op0=ALU.add, op1=ALU.mult),
             reads=[c_, gt], writes=[g_])
        gs.append(g_); sh.append(s_)

    xall = P.sb("xall", [128, KT, TT], F32)
    xk = [Buf("xk%d" % i) for i in range(KT)]
    sq = [P.sb("sq%d" % i, [128, TT], BF16) for i in range(2)]
    hT = P.sb("hT", [128, KT, TT], BF16)
    hk = [Buf("hk%d" % i) for i in range(KT)]
    tmpn = [P.sb("tmpn%d" % i, [128, TT], F32) for i in range(2)]
    rstd = P.sb("rstd", [128, TT], F32); rtmp = P.sb("rtmp", [128, TT], F32)
    wt = [P.sb("wt%d" % i, [128, KT, 128], BF16) for i in range(3)]
    ssq = P.ps("ssq", [128, 512])
    acc = [P.ps("acc%d" % i, [128, 512]) for i in range(2)]
    rot = [P.ps("rot%d" % i, [128, 512]) for i in range(2)]
    ob = [P.sb("ob%d" % i, [128, TT], BF16) for i in range(3)]
    of = [P.sb("of%d" % i, [128, TT], F32) for i in range(2)]
    t1 = [P.sb("t1_%d" % i, [128, TT], F32) for i in range(2)]
    t2 = [P.sb("t2_%d" % i, [128, TT], F32) for i in range(2)]
    orr = [P.sb("orr%d" % i, [128, TT], BF16) for i in range(2)]

    tiles = [(c0, min(TT, nlat - c0), 0) for c0 in range(0, nlat, TT)]
    if nctx:
        tiles += [(nlat + c0, min(TT, nctx - c0), 1) for c0 in range(0, nctx, TT)]
    it = 0
    for (c0, n, isctx) in tiles:
        for kt in range(KT):
            P.dma("sp", xall[:, kt, :n], xT[kt * 128:(kt + 1) * 128, c0:c0 + n], reads=[], writes=[xk[kt]], sem_buf=xk[kt])
            s = sq[kt % 2]
            P.op("act", lambda e: e.activation(out=s[:, :n], in_=xall[:, kt, :n], func=AF.Square), reads=[xk[kt]], writes=[s])
            P.op("pe", lambda e: e.matmul(ssq[:, :n], lhsT=ones[:], rhs=s[:, :n], start=(kt == 0), stop=(kt == KT - 1)),
                 reads=[s, ones], writes=[ssq], track=True)
        rstd_from_ssq(P, ssq, rstd, rtmp, n, D)
        for kt in range(KT):
            tn = tmpn[kt % 2]
            P.op("dve", lambda e: e.tensor_tensor(out=tn[:, :n], in0=xall[:, kt, :n], in1=rstd[:, :n], op=ALU.mult),
                 reads=[xk[kt], rstd], writes=[tn])
            P.op("act", lambda e: e.activation(out=hT[:, kt, :n], in_=tn[:, :n], func=AF.Identity,
                                               scale=gs[isctx][:, kt:kt + 1], bias=sh[isctx][:, kt:kt + 1]),
                 reads=[tn, gs[isctx], sh[isctx]], writes=[hk[kt]])
        for m in range(MT):
            wb = wt[it % 3]; a = acc[it % 2]; it += 1
            P.dma("pool", wb[:], w[:, m * 128:(m + 1) * 128].rearrange("(kt p) m -> p kt m", p=128), writes=[wb], sem_buf=wb)
            for kt in range(KT):
                P.op("pe", lambda e: e.matmul(a[:, :n], lhsT=wb[:, kt, :], rhs=hT[:, kt, :n], start=(kt == 0), stop=(kt == KT - 1)),
                     reads=[wb, hk[kt]], writes=[a], track=(kt == KT - 1))
            if m < 32:
                o = ob[m % 3]
                P.op("act", lambda e: e.copy(out=o[:, :n], in_=a[:, :n]), reads=[a], writes=[o])
                isq = m < NQH
                if isq:
                    P.dma("sp", qp[:, m, c0:c0 + n], o[:, :n], reads=[o], writes=[], sem_buf=o, is_output=True)
                if isctx:
                    if not isq:
                        P.dma("sp", kk[:, m - NQH, c0:c0 + n], o[:, :n], reads=[o], writes=[], sem_buf=o, is_output=True)
                else:
                    r = rot[m % 2]; a1 = t1[m % 2]; a2 = t2[m % 2]; oo = orr[m % 2]
                    P.op("pe", lambda e: e.matmul(r[:, :n], lhsT=R[:], rhs=o[:, :n], start=True, stop=True), reads=[R, o], writes=[r])
                    P.op("dve", lambda e: e.tensor_tensor(out=a1[:, :n], in0=o[:, :n], in1=cosT[:, c0:c0 + n], op=ALU.mult),
                         reads=[o, cosT], writes=[a1])
                    P.op("dve", lambda e: e.tensor_tensor(out=a2[:, :n], in0=r[:, :n], in1=sinT[:, c0:c0 + n], op=ALU.mult),
                         reads=[r, sinT], writes=[a2])
                    P.op("dve", lambda e: e.tensor_tensor(out=oo[:, :n], in0=a2[:, :n], in1=a1[:, :n], op=ALU.add),
                         reads=[a1, a2], writes=[oo])
                    dst = qr[:, m, c0:c0 + n] if isq else kk[:, m - NQH, c0:c0 + n]
                    P.dma("sp", dst, oo[:, :n], reads=[oo], writes=[], sem_buf=oo, is_output=True)
            elif m < 40:
                o = ob[m % 3]
                P.op("act", lambda e: e.copy(out=o[:, :n], in_=a[:, :n]), reads=[a], writes=[o])
                P.dma("sp", vv[:, m - 32, c0:c0 + n], o[:, :n], reads=[o], writes=[], sem_buf=o, is_output=True)
            else:
                o = of[m % 2]
                P.op("dve", lambda e: e.tensor_copy(out=o[:, :n], in_=a[:, :n]), reads=[a], writes=[o])
                P.dma("sp", uu[:, m - 40, c0:c0 + n], o[:, :n], reads=[o], writes=[], sem_buf=o, is_output=True)
    return P


def rope_consts(tok0, n):
    t = np.arange(tok0, tok0 + n)
    row = (t // 64).astype(np.float32); col = (t % 64).astype(np.float32)
    inv = (10000.0 ** (-np.arange(0, 64, 2, dtype=np.float32) / 64)).astype(np.float32)
    ar = row[:, None] * inv; ac = col[:, None] * inv
    ang = np.concatenate([ar, ar, ac, ac], -1)
    cosT = np.ascontiguousarray(np.cos(ang).T.astype(np.float32)); sinT = np.ascontiguousarray(np.sin(ang).T.astype(np.float32))
    Rm = np.zeros((128, 128), np.float32)
    for h0 in (0, 64):
        for d in range(32):
            Rm[h0 + d + 32, h0 + d] = -1.0
            Rm[h0 + d, h0 + d + 32] = 1.0
    return cosT, sinT, Rm


def build_attn(NB, NCQ, P=None):
    P = P or Prog()
    NL = NB * 128; NQ = NL + NCQ; NKB = NB + 2; NCB = CTX // 128
    qr = P.dram("qr", [128, NQH, NL], BF16, "ExternalInput")
    qp = P.dram("qp", [128, NQH, NQ], BF16, "ExternalInput")
    kr = P.dram("kr", [128, NKV, NKB * 128], BF16, "ExternalInput")
    vv = P.dram("v", [128, NKB, KVW], BF16, "ExternalInput")
    kc = P.dram("kc", [128, NKV, CTX], BF16, "ExternalInput")
    vc = P.dram("vc", [128, NCB, KVW], BF16, "ExternalInput")
    mk = P.dram("mask", [128, 4, 128], BF16, "ExternalInput")
    snk = P.dram("sink", [NQH], F32, "ExternalInput")
    oT = P.dram("oT", [128, NQH, NQ], F32, "ExternalOutput")

    ones = P.sb("ones", [128, 128], BF16)
    P.op("dve", lambda e: e.memset(ones[:], 1.0), writes=[ones])
    KR = P.sb("KR", [128, NKV, NKB * 128], BF16); P.dma("sp", KR[:], kr[:], writes=[KR], sem_buf=KR)
    VV = P.sb("VV", [128, NKB, KVW], BF16); P.dma("sp", VV[:], vv[:], writes=[VV], sem_buf=VV)
    KC = P.sb("KC", [128, NKV, CTX], BF16); P.dma("sp", KC[:], kc[:], writes=[KC], sem_buf=KC)
    VC = P.sb("VC", [128, NCB, KVW], BF16); P.dma("sp", VC[:], vc[:], writes=[VC], sem_buf=VC)
    MK = P.sb("MK", [128, 4, 128], BF16); P.dma("sp", MK[:], mk[:], writes=[MK], sem_buf=MK)
    es0 = P.sb("es0", [128, NQH], F32); es = P.sb("es", [128, NQH], F32)
    P.dma("sp", es0[:], snk.t.partition_broadcast(128), writes=[es0], sem_buf=es0, allow_slow_non_contiguous=True)
    P.op("act", lambda e: e.activation(out=es[:], in_=es0[:], func=AF.Exp), reads=[es0], writes=[es])

    QR = [P.sb("QR%d" % i, [128, 3, 128], BF16) for i in range(2)]
    QP = [P.sb("QP%d" % i, [128, 3, 128], BF16) for i in range(2)]
    sps = [P.ps("sps%d" % i, [128, 512]) for i in range(3)]
    ops_ = [P.ps("ops%d" % i, [128, 512]) for i in range(2)]
    dps = [P.ps("dps%d" % i, [128, 512]) for i in range(2)]
    pt = [P.sb("pt%d" % i, [128, 3, 128], BF16) for i in range(4)]
    dn = [P.sb("dn%d" % i, [128, 3, 128], F32) for i in range(2)]
    oo = [P.sb("oo%d" % i, [128, 3, 128], F32) for i in range(2)]
    it = 0; ip = 0
    blocks = [(n, 128, False) for n in range(NB)] + ([(NB, NCQ, True)] if NCQ else [])
    for (n, nq, isctx) in blocks:
        c0 = n * 128; N3 = 3 * nq
        for h in range(NKV):
            Qp_ = QP[it % 2]; Qr_ = QR[it % 2]
            P.dma("sp", Qp_[:, :, :nq], qp[:, 3 * h:3 * h + 3, c0:c0 + nq], writes=[Qp_], sem_buf=Qp_)
            if not isctx:
                P.dma("sp", Qr_[:, :, :nq], qr[:, 3 * h:3 * h + 3, c0:c0 + nq], writes=[Qr_], sem_buf=Qr_)
            kbs = [] if isctx else [("w", n + j, (None, (2 if n == 0 else 0), None, None)[1] if j == 0 else None) for j in range(3)]
            if not isctx:
                kbs = [("w", n, 2 if n == 0 else 0), ("w", n + 1, None), ("w", n + 2, 3 if n == NB - 1 else 1)]
            kbs += [("c", c, None) for c in range(NCB)]
            O = ops_[it % 2]; Dn = dps[it % 2]
            for bi, (kind, kb, mi) in enumerate(kbs):
                S = sps[ip % 3]; Pt = pt[ip % 4]; ip += 1
                if kind == "w":
                    P.op("pe", lambda e: e.matmul(S[:, :N3], lhsT=KR[:, h, kb * 128:(kb + 1) * 128], rhs=Qr_[:, :, :nq], start=True, stop=True),
                         reads=[KR, Qr_], writes=[S])
                else:
                    P.op("pe", lambda e: e.matmul(S[:, :N3], lhsT=KC[:, h, kb * 128:(kb + 1) * 128], rhs=Qp_[:, :, :nq], start=True, stop=True),
                         reads=[KC, Qp_], writes=[S])
                P.op("act", lambda e: e.activation(out=Pt[:, :, :nq], in_=S[:, :N3].rearrange("p (a b) -> p a b", a=3), func=AF.Exp, scale=SCALE),
                     reads=[S], writes=[Pt])
                if mi is not None:
                    P.op("pool", lambda e: e.tensor_tensor(out=Pt[:, :, :nq], in0=Pt[:, :, :nq],
                                                           in1=MK[:, mi:mi + 1, :nq].broadcast_to([128, 3, nq]), op=ALU.mult),
                         reads=[Pt, MK], writes=[Pt])
                vsrc = VV[:, kb, h * 128:(h + 1) * 128] if kind == "w" else VC[:, kb, h * 128:(h + 1) * 128]
                last = bi == len(kbs) - 1
                P.op("pe", lambda e: e.matmul(O[:, :N3], lhsT=vsrc, rhs=Pt[:, :, :nq], start=(bi == 0), stop=last),
                     reads=[VV, VC, Pt], writes=[O], track=last)
                P.op("pe", lambda e: e.matmul(Dn[:, :N3], lhsT=ones[:], rhs=Pt[:, :, :nq], start=(bi == 0), stop=last),
                     reads=[ones, Pt], writes=[Dn], track=True)
            d_ = dn[it % 2]; o_ = oo[it % 2]
            P.op("dve", lambda e: e.tensor_tensor(out=d_[:, :, :nq], in0=Dn[:, :N3].rearrange("p (a b) -> p a b", a=3),
                                                  in1=es[:, 3 * h:3 * h + 3].unsqueeze(2).broadcast_to([128, 3, nq]), op=ALU.add),
                 reads=[Dn, es], writes=[d_])
            P.op("dve", lambda e: e.reciprocal(out=d_[:, :, :nq], in_=d_[:, :, :nq]), reads=[d_], writes=[d_])
            P.op("dve", lambda e: e.tensor_tensor(out=o_[:, :, :nq], in0=O[:, :N3].rearrange("p (a b) -> p a b", a=3), in1=d_[:, :, :nq], op=ALU.mult),
                 reads=[O, d_], writes=[o_])
            P.dma("sp", oT[:, 3 * h:3 * h + 3, c0:c0 + nq], o_[:, :, :nq], reads=[o_], sem_buf=o_, is_output=True)
            it += 1
    return P


def attn_masks(first, last):
    m = np.arange(128)[:, None]; i = np.arange(128)[None, :]
    L = (i <= m).astype(np.float32); R = (m <= i).astype(np.float32)
    Z = np.zeros_like(L)
    return np.stack([L, R, Z if first else L, Z if last else R], 1).astype(NPBF)


def build_s5(NG=8, with_ctx=True, P=None):
    P = P or Prog()
    NGD = 2 * NG; NCC = CTX // TCH; NXC = SEQ // TCH; NJ = NCH; NU = NCH + NCC; NS = NG * B
    TWO_PI = 2.0 * math.pi
    U_d = P.dram("U", [128, 4, NG, B, NU], F32, "ExternalInput")
    are_d = P.dram("a_re", [2, NG, 64], F32, "ExternalInput"); aim_d = P.dram("a_im", [2, NG, 64], F32, "ExternalInput")
    ldt_d = P.dram("log_dt", [2 * NG], F32, "ExternalInput")
    bre_d = P.dram("b_re", [2, NG, 64, 16], F32, "ExternalInput"); bim_d = P.dram("b_im", [2, NG, 64, 16], F32, "ExternalInput")
    cre_d = P.dram("c_re", [2, NG, 16, 64], F32, "ExternalInput"); cim_d = P.dram("c_im", [2, NG, 16, 64], F32, "ExternalInput")
    msk_d = P.dram("msk", [128, 2, 4, 512], BF16, "ExternalInput")
    idn_d = P.dram("idn", [64, 64], F32, "ExternalInput")
    y_d = P.dram("y", [128, 4, NG, B, NXC + NCC], F32, "ExternalOutput")
    NP = 64
    cnt = [0]

    def tl(shape, dt=F32, name="t"):
        cnt[0] += 1
        return P.sb("%s%d" % (name, cnt[0]), [NP] + list(shape), dt)

    def tt(o, a, b, op, e="dve"):
        P.op(e, lambda en: en.tensor_tensor(out=o[:], in0=a[:], in1=b[:], op=op), reads=[a, b], writes=[o]); return o

    def ts(o, a, s1, op0, s2=None, op1=None, e="dve"):
        if op1 is None:
            P.op(e, lambda en: en.tensor_scalar(out=o[:], in0=a[:], scalar1=s1, scalar2=None, op0=op0), reads=[a], writes=[o])
        else:
            P.op(e, lambda en: en.tensor_scalar(out=o[:], in0=a[:], scalar1=s1, scalar2=s2, op0=op0, op1=op1), reads=[a], writes=[o])
        return o

    def act(o, a, func, **kw):
        P.op("act", lambda en: en.activation(out=o[:], in_=a[:], func=func, **kw), reads=[a], writes=[o]); return o

    are = tl([NGD]); aim = tl([NGD]); ldt = tl([NGD])
    P.dma("sp", are[:], are_d.t.rearrange("d g p -> p (d g)"), writes=[are], sem_buf=are, allow_slow_non_contiguous=True)
    P.dma("sp", aim[:], aim_d.t.rearrange("d g p -> p (d g)"), writes=[aim], sem_buf=aim, allow_slow_non_contiguous=True)
    P.dma("sp", ldt[:], ldt_d.t.partition_broadcast(NP), writes=[ldt], sem_buf=ldt, allow_slow_non_contiguous=True)
    bre = tl([NGD, 16]); bim = tl([NGD, 16]); cre = tl([NGD, 16]); cim = tl([NGD, 16])
    P.dma("sp", bre[:], bre_d.t.rearrange("d g p c -> p (d g) c"), writes=[bre], sem_buf=bre, allow_slow_non_contiguous=True)
    P.dma("sp", bim[:], bim_d.t.rearrange("d g p c -> p (d g) c"), writes=[bim], sem_buf=bim, allow_slow_non_contiguous=True)
    P.dma("sp", cre[:], cre_d.t.rearrange("d g c p -> p (d g) c"), writes=[cre], sem_buf=cre, allow_slow_non_contiguous=True)
    P.dma("sp", cim[:], cim_d.t.rearrange("d g c p -> p (d g) c"), writes=[cim], sem_buf=cim, allow_slow_non_contiguous=True)
    idn = P.sb("idn", [NP, 64], BF16); P.dma("pool", idn[:], idn_d[:, :], writes=[idn], sem_buf=idn)
    msk = P.sb("msk", [128, 2, 4, 512], BF16); P.dma("sp", msk[:], msk_d[:], writes=[msk], sem_buf=msk)

    dt_ = act(tl([NGD]), ldt, AF.Exp)
    xr = tt(tl([NGD]), are, dt_, ALU.mult); th = tt(tl([NGD]), aim, dt_, ALU.mult)
    mag = act(tl([NGD]), xr, AF.Exp)

    def sin_of(ang):
        q = ts(tl([NGD]), ang, 1.0 / TWO_PI, ALU.mult)
        qi = tl([NGD], I32)
        P.op("dve", lambda en: en.tensor_copy(out=qi[:], in_=q[:]), reads=[q], writes=[qi])
        qf = tl([NGD])
        P.op("dve", lambda en: en.tensor_copy(out=qf[:], in_=qi[:]), reads=[qi], writes=[qf])
        r = tl([NGD])
        P.op("dve", lambda en: en.scalar_tensor_tensor(out=r[:], in0=qf[:], scalar=-TWO_PI, in1=ang[:], op0=ALU.mult, op1=ALU.add),
             reads=[qf, ang], writes=[r])
        hi = ts(tl([NGD]), r, math.pi, ALU.is_gt, -TWO_PI, ALU.mult)
        lo = ts(tl([NGD]), r, -math.pi, ALU.is_lt, TWO_PI, ALU.mult)
        r2 = tt(tl([NGD]), r, hi, ALU.add); r3 = tt(tl([NGD]), r2, lo, ALU.add)
        r4 = ts(tl([NGD]), r3, math.pi, ALU.min, -math.pi, ALU.max)
        return act(tl([NGD]), r4, AF.Sin)

    sn = sin_of(th)
    thc = ts(tl([NGD]), th, math.pi / 2, ALU.add)
    cs = sin_of(thc)
    abr = tt(tl([NGD]), mag, cs, ALU.mult); abi = tt(tl([NGD]), mag, sn, ALU.mult)
    nr = ts(tl([NGD]), abr, -1.0, ALU.add)
    den = tt(tl([NGD]), tt(tl([NGD]), are, are, ALU.mult), tt(tl([NGD]), aim, aim, ALU.mult), ALU.add)
    rden = tl([NGD]); P.op("dve", lambda en: en.reciprocal(out=rden[:], in_=den[:]), reads=[den], writes=[rden])
    cfr = tt(tl([NGD]), tt(tl([NGD]), tt(tl([NGD]), nr, are, ALU.mult), tt(tl([NGD]), abi, aim, ALU.mult), ALU.add), rden, ALU.mult)
    cfi = tt(tl([NGD]), tt(tl([NGD]), tt(tl([NGD]), abi, are, ALU.mult), tt(tl([NGD]), nr, aim, ALU.mult), ALU.subtract), rden, ALU.mult)

    def cmul_b(va_r, va_i, vb_r, vb_i, rd, tmp, re, im, neg_im=False):
        m1, m2 = tmp
        P.op("dve", lambda en: en.tensor_tensor(out=m1[:], in0=va_r, in1=vb_r, op=ALU.mult), reads=rd, writes=[m1])
        P.op("dve", lambda en: en.tensor_tensor(out=m2[:], in0=va_i, in1=vb_i, op=ALU.mult), reads=rd, writes=[m2])
        P.op("dve", lambda en: en.tensor_tensor(out=re[:], in0=m1[:], in1=m2[:], op=ALU.subtract), reads=[m1, m2], writes=[re])
        P.op("dve", lambda en: en.tensor_tensor(out=m1[:], in0=va_r, in1=vb_i, op=ALU.mult), reads=rd, writes=[m1])
        P.op("dve", lambda en: en.tensor_tensor(out=m2[:], in0=va_i, in1=vb_r, op=ALU.mult), reads=rd, writes=[m2])
        P.op("dve", lambda en: en.tensor_tensor(out=im[:], in0=m1[:], in1=m2[:], op=ALU.add), reads=[m1, m2], writes=[im])
        if neg_im:
            P.op("dve", lambda en: en.tensor_scalar(out=im[:], in0=im[:], scalar1=-1.0, scalar2=None, op0=ALU.mult), reads=[im], writes=[im])

    Bbr = tl([NGD, 16]); Bbi = tl([NGD, 16]); tb1 = tl([NGD, 16]); tb2 = tl([NGD, 16])
    cmul_b(cfr[:].unsqueeze(2).broadcast_to([NP, NGD, 16]), cfi[:].unsqueeze(2).broadcast_to([NP, NGD, 16]), bre[:], bim[:],
           [cfr, cfi, bre, bim], (tb1, tb2), Bbr, Bbi)
    pwr = tl([NGD, TCH + 1]); pwi = tl([NGD, TCH + 1])
    P.op("dve", lambda en: en.memset(pwr[:], 1.0), writes=[pwr]); P.op("dve", lambda en: en.memset(pwi[:], 0.0), writes=[pwi])
    m1 = tl([NGD]); m2 = tl([NGD]); m3 = tl([NGD]); m4 = tl([NGD])
    for k in range(1, TCH + 1):
        P.op("dve", lambda en: en.tensor_tensor(out=m1[:], in0=pwr[:, :, k - 1], in1=abr[:], op=ALU.mult), reads=[pwr, abr], writes=[m1])
        P.op("dve", lambda en: en.tensor_tensor(out=m2[:], in0=pwi[:, :, k - 1], in1=abi[:], op=ALU.mult), reads=[pwi, abi], writes=[m2])
        P.op("dve", lambda en: en.tensor_tensor(out=m3[:], in0=pwr[:, :, k - 1], in1=abi[:], op=ALU.mult), reads=[pwr, abi], writes=[m3])
        P.op("dve", lambda en: en.tensor_tensor(out=m4[:], in0=pwi[:, :, k - 1], in1=abr[:], op=ALU.mult), reads=[pwi, abr], writes=[m4])
        P.op("dve", lambda en: en.tensor_tensor(out=pwr[:, :, k], in0=m1[:], in1=m2[:], op=ALU.subtract), reads=[m1, m2], writes=[pwr])
        P.op("dve", lambda en: en.tensor_tensor(out=pwi[:, :, k], in0=m3[:], in1=m4[:], op=ALU.add), reads=[m3, m4], writes=[pwi])
    mm = tt(tl([NGD, TCH + 1]), tt(tl([NGD, TCH + 1]), pwr, pwr, ALU.mult), tt(tl([NGD, TCH + 1]), pwi, pwi, ALU.mult), ALU.add)
    rm = tl([NGD, TCH + 1]); P.op("dve", lambda en: en.reciprocal(out=rm[:], in_=mm[:]), reads=[mm], writes=[rm])
    ipr = tt(tl([NGD, TCH + 1]), pwr, rm, ALU.mult)
    ipi = tt(tl([NGD, TCH + 1]), pwi, rm, ALU.mult); ts(ipi, ipi, -1.0, ALU.mult)
    E1r = tl([NGD, TCH]); E1i = tl([NGD, TCH]); E2r = tl([NGD, TCH]); E2i = tl([NGD, TCH])
    for (dst, f_src, b_src) in ((E1r, ipr, pwr), (E1i, ipi, pwi), (E2r, pwr, ipr), (E2i, pwi, ipi)):
        P.op("dve", lambda en: en.tensor_copy(out=dst[:, 0:NG, :], in_=f_src[:, 0:NG, 0:TCH]), reads=[f_src], writes=[dst])
        P.op("dve", lambda en: en.tensor_copy(out=dst[:, NG:NGD, :], in_=b_src[:, NG:NGD, 0:TCH]), reads=[b_src], writes=[dst])
    aTr = tl([NG, B]); aTi = tl([NG, B]); aTrb = tl([NG, B]); aTib = tl([NG, B])
    for (dst, src, lo_) in ((aTr, pwr, 0), (aTi, pwi, 0), (aTrb, pwr, NG), (aTib, pwi, NG)):
        P.op("dve", lambda en: en.tensor_copy(out=dst[:], in_=src[:, lo_:lo_ + NG, TCH:TCH + 1].broadcast_to([NP, NG, B])), reads=[src], writes=[dst])

    shp = [2, TCH, 16]
    gtmp = (tl(shp), tl(shp)); gre = tl(shp); gim = tl(shp)
    X1g = [P.sb("X1g%d" % i, [NP, 2, 2, TCH * 16], BF16) for i in range(2)]
    X2g = [P.sb("X2g%d" % i, [NP, 2, 2, TCH * 16], BF16) for i in range(2)]

    def dv(t_, g):
        return t_[:].rearrange("p (d g) n -> p d g n", d=2)[:, :, g, :]

    def gen_group(g, slot, need2):
        x1 = X1g[slot]; x2 = X2g[slot]
        e_b = lambda t_: dv(t_, g).unsqueeze(3).broadcast_to([NP] + shp)
        c_b = lambda t_: dv(t_, g).unsqueeze(2).broadcast_to([NP] + shp)
        cmul_b(e_b(E1r), e_b(E1i), c_b(Bbr), c_b(Bbi), [E1r, E1i, Bbr, Bbi], gtmp, gre, gim)
        P.op("act", lambda en: en.copy(out=x1[:, 0], in_=gre[:].rearrange("p d s c -> p d (s c)")), reads=[gre], writes=[x1])
        P.op("act", lambda en: en.copy(out=x1[:, 1], in_=gim[:].rearrange("p d s c -> p d (s c)")), reads=[gim], writes=[x1])
        if need2:
            cmul_b(e_b(E2r), e_b(E2i), c_b(cre), c_b(cim), [E2r, E2i, cre, cim], gtmp, gre, gim, neg_im=True)
            P.op("act", lambda en: en.copy(out=x2[:, 0], in_=gre[:].rearrange("p d s c -> p d (s c)")), reads=[gre], writes=[x2])
            P.op("act", lambda en: en.copy(out=x2[:, 1], in_=gim[:].rearrange("p d s c -> p d (s c)")), reads=[gim], writes=[x2])
        return x1, x2

    gps = [P.ps("gps%d" % i, [128, 512]) for i in range(3)]
    tps = [P.ps("tps%d" % i, [128, 512]) for i in range(2)]
    X1T = [P.sb("X1T%d" % i, [128, 2, 4, 2, 64], BF16) for i in range(2)]
    Ub = [P.sb("Ub%d" % i, [128, 4, NU], BF16) for i in range(4)]
    GH = {}
    for d in range(2):
        for c in range(2):
            GH[(d, c)] = P.sb("GH%d%d" % (d, c), [NP, NS, NJ + 1], F32)
            zc = 0 if d == 0 else NJ
            P.op("dve", lambda e: e.memset(GH[(d, c)][:, :, zc:zc + 1], 0.0), writes=[GH[(d, c)]])
    i_ = 0; iu = 0
    for g in range(NG):
        x1, _ = gen_group(g, g % 2, False)
        xt = X1T[g % 2]
        for d in range(2):
            for kt in range(4):
                t_ = tps[i_ % 2]; i_ += 1
                ks = slice(kt * 128, (kt + 1) * 128)
                for c in range(2):
                    P.op("pe", lambda e: e.matmul(t_[:, c * 64:(c + 1) * 64], lhsT=x1[:, c, d, ks], rhs=idn[:], start=True, stop=True), reads=[x1, idn], writes=[t_])
                P.op("act", lambda e: e.copy(out=xt[:, d, kt, :, :], in_=t_[:, 0:128].rearrange("p (a b) -> p a b", a=2)), reads=[t_], writes=[xt])
        for b in range(B):
            s = g * B + b
            ub = Ub[iu % 4]; iu += 1
            P.dma("pool", ub[:], U_d[:, :, g, b, :], writes=[ub], sem_buf=ub)
            for d in range(2):
                j0 = 0 if d == 0 else NCC; o0 = 1 if d == 0 else 0
                for c in range(2):
                    g_ = gps[i_ % 3]; i_ += 1
                    for kt in range(4):
                        P.op("pe", lambda e: e.matmul(g_[0:64, :NJ], lhsT=xt[:, d, kt, c, :], rhs=ub[:, kt, j0:j0 + NJ], start=(kt == 0), stop=(kt == 3)),
                             reads=[xt, ub], writes=[g_], track=(kt == 3))
                    P.op("act", lambda e: e.copy(out=GH[(d, c)][:, s, o0:o0 + NJ], in_=g_[0:64, :NJ]), reads=[g_], writes=[GH[(d, c)]])

    def rec(eng, Hr, Hi, ar_, ai_, j_in, j_io, tmps):
        tr, ti, q1, q2 = tmps
        av = ar_[:].rearrange("p g b -> p (g b)"); aiv = ai_[:].rearrange("p g b -> p (g b)")
        P.op(eng, lambda e: e.tensor_tensor(out=tr[:], in0=Hr[:, :, j_in], in1=Hr[:, :, j_io], op=ALU.add), reads=[Hr], writes=[tr])
        P.op(eng, lambda e: e.tensor_tensor(out=ti[:], in0=Hi[:, :, j_in], in1=Hi[:, :, j_io], op=ALU.add), reads=[Hi], writes=[ti])
        P.op(eng, lambda e: e.tensor_tensor(out=q1[:], in0=tr[:], in1=av, op=ALU.mult), reads=[tr, ar_], writes=[q1])
        P.op(eng, lambda e: e.tensor_tensor(out=q2[:], in0=ti[:], in1=aiv, op=ALU.mult), reads=[ti, ai_], writes=[q2])
        P.op(eng, lambda e: e.tensor_tensor(out=Hr[:, :, j_io], in0=q1[:], in1=q2[:], op=ALU.subtract), reads=[q1, q2], writes=[Hr])
        P.op(eng, lambda e: e.tensor_tensor(out=q1[:], in0=tr[:], in1=aiv, op=ALU.mult), reads=[tr, ai_], writes=[q1])
        P.op(eng, lambda e: e.tensor_tensor(out=q2[:], in0=ti[:], in1=av, op=ALU.mult), reads=[ti, ar_], writes=[q2])
        P.op(eng, lambda e: e.tensor_tensor(out=Hi[:, :, j_io], in0=q1[:], in1=q2[:], op=ALU.add), reads=[q1, q2], writes=[Hi])
    tf = [tl([NS]) for _ in range(4)]; tb = [tl([NS]) for _ in range(4)]
    for j in range(NJ):
        rec("dve", GH[(0, 0)], GH[(0, 1)], aTr, aTi, j, j + 1, tf)
        jb = NJ - 1 - j
        rec("pool", GH[(1, 0)], GH[(1, 1)], aTrb, aTib, jb + 1, jb, tb)

    A0 = [P.sb("A0_%d" % i, [128, 2, 4, 512], BF16) for i in range(2)]
    Hh = [P.sb("Hh%d" % i, [NP, 2, 2, NJ + 1], BF16) for i in range(2)]
    yo = [P.sb("yo%d" % i, [128, 4, NXC + NCC], F32) for i in range(2)]
    for g in range(NG):
        x1, x2 = gen_group(g, g % 2, True)
        a0 = A0[g % 2]
        for d in range(2):
            for kt in range(4):
                g_ = gps[i_ % 3]; i_ += 1
                ks = slice(kt * 128, (kt + 1) * 128)
                P.op("pe", lambda e: e.matmul(g_[:, :], lhsT=x1[:, 0, d, ks], rhs=x2[:, 0, d, :], start=True, stop=False), reads=[x1, x2], writes=[g_], track=False)
                P.op("pe", lambda e: e.matmul(g_[:, :], lhsT=x1[:, 1, d, ks], rhs=x2[:, 1, d, :], start=False, stop=True), reads=[x1, x2], writes=[g_])
                P.op("dve", lambda e: e.tensor_tensor(out=a0[:, d, kt, :], in0=g_[:, :], in1=msk[:, d, kt, :], op=ALU.mult), reads=[g_, msk], writes=[a0])
        for b in range(B):
            s = g * B + b; yt = yo[s % 2]; hh = Hh[s % 2]
            ub = Ub[iu % 4]; iu += 1
            P.dma("pool", ub[:], U_d[:, :, g, b, :], writes=[ub], sem_buf=ub)
            for d in range(2):
                for c in range(2):
                    P.op("act", lambda e: e.copy(out=hh[:, d, c, :], in_=GH[(d, c)][:, s, :]), reads=[GH[(d, c)]], writes=[hh])
            for mt in range(4):
                ms = slice(mt * 128, (mt + 1) * 128)
                segs = [(0, NXC, NCC, NCC, 1)]
                if with_ctx:
                    segs.append((NXC, NCC, 0, 0, NXC + 1))
                for (o0, n, u0f, hf0, hb0) in segs:
                    u0b = u0f if o0 == 0 else NCC + NXC
                    a_ = gps[i_ % 3]; i_ += 1
                    mms = []
                    for kt in range(4):
                        mms.append((a0[:, 0, kt, ms], ub[:, kt, u0f:u0f + n], [a0, ub]))
                        mms.append((a0[:, 1, kt, ms], ub[:, kt, u0b:u0b + n], [a0, ub]))
                    for c in range(2):
                        mms.append((x2[:, c, 0, ms], hh[:, 0, c, hf0:hf0 + n], [x2, hh]))
                        mms.append((x2[:, c, 1, ms], hh[:, 1, c, hb0:hb0 + n], [x2, hh]))
                    for mi, (l_, r_, rd) in enumerate(mms):
                        P.op("pe", lambda e: e.matmul(a_[:, :n], lhsT=l_, rhs=r_, start=(mi == 0), stop=(mi == len(mms) - 1)),
                             reads=rd, writes=[a_], track=(mi == len(mms) - 1))
                    P.op("act", lambda e: e.copy(out=yt[:, mt, o0:o0 + n], in_=a_[:, :n]), reads=[a_], writes=[yt])
            if not with_ctx:
                P.op("dve", lambda e: e.memset(yt[:, :, NXC:], 0.0), writes=[yt])
            P.dma("sp", y_d[:, :, g, b, :], yt[:], reads=[yt], sem_buf=yt, is_output=True)
    return P


def s5_consts():
    k = np.arange(512); n = np.arange(512)
    s = (k // 16)[:, None]; t = (n // 16)[None, :]
    mf = (t >= s).astype(np.float32); mb = (s >= t).astype(np.float32)
    msk = np.stack([mf.reshape(4, 128, 512).transpose(1, 0, 2), mb.reshape(4, 128, 512).transpose(1, 0, 2)], 1)
    return np.ascontiguousarray(msk.astype(NPBF)), np.eye(64, dtype=np.float32)


def s5_pack_u(u_x, u_c):
    st = np.concatenate([u_c, u_x, u_c], 1)
    NU = st.shape[1] // TCH
    a = st.reshape(B, NU, TCH, SW // 16, 16)
    a = a.transpose(2, 4, 3, 0, 1).reshape(TCH * 16, SW // 16, B, NU)
    a = a.reshape(4, 128, SW // 16, B, NU).transpose(1, 0, 2, 3, 4)
    return [np.ascontiguousarray(a[:, :, i * 8:(i + 1) * 8]) for i in range(NCORES)]


def s5_unpack_y(ys):
    a = np.concatenate(ys, 2)
    a = a.transpose(1, 0, 2, 3, 4).reshape(TCH, 16, SW // 16, B, -1)
    a = a.transpose(3, 4, 0, 2, 1).reshape(B, -1, SW)
    return a[:, :SEQ], a[:, SEQ:]


def token_tiles(nlat, nctx, TT):
    tiles = [(c0, min(TT, nlat - c0), 0) for c0 in range(0, nlat, TT)]
    if nctx:
        tiles += [(nlat + c0, min(TT, nctx - c0), 1) for c0 in range(0, nctx, TT)]
    return tiles


def build_post(nlat, nctx, TT=512, P=None):
    P = P or Prog()
    NT = nlat + nctx; KA_ = AW // 128; KS = SW // 128; KT = D // 128
    oT = P.dram("oT", [128, KA_, NT], F32, "ExternalInput")
    yT = P.dram("yT", [128, KS, NT], F32, "ExternalInput")
    uT = P.dram("uT", [128, KS, NT], F32, "ExternalInput")
    xT = P.dram("xT", [D, NT], F32, "ExternalInput")
    dsk = P.dram("dsk", [SW], F32, "ExternalInput")
    wglu = P.dram("wglu", [SW, SW], F32, "ExternalInput")
    goa = P.dram("goa", [AW], F32, "ExternalInput"); gos = P.dram("gos", [SW], F32, "ExternalInput")
    wout = P.dram("wout", [D, D], F32, "ExternalInput")
    gatev = P.dram("gatev", [2, D], F32, "ExternalInput")
    x1T = P.dram("x1T", [D, NT], F32, "ExternalOutput")

    ones = P.sb("ones", [128, 128], BF16); P.op("dve", lambda e: e.memset(ones[:], 1.0), writes=[ones])
    dskt = P.sb("dskt", [128, KS], F32); load_cols(P, dskt, dsk.t)
    goat = P.sb("goat", [128, KA_], F32); load_cols(P, goat, goa.t)
    gost = P.sb("gost", [128, KS], F32); load_cols(P, gost, gos.t)
    gat = [P.sb("gat%d" % j, [128, KT], F32) for j in range(2)]
    for j in range(2):
        load_cols(P, gat[j], gatev[j])
    OT = P.sb("OT", [128, KA_, TT], F32); ok = [Buf("ok%d" % i) for i in range(KA_)]
    YG = P.sb("YG", [128, KS, TT], F32); ygk = [Buf("ygk%d" % i) for i in range(KS)]
    YB = P.sb("YB", [128, KS, TT], BF16); ybk = [Buf("ybk%d" % i) for i in range(KS)]
    OS = P.sb("OS", [128, KS, TT], F32); osk = [Buf("osk%d" % i) for i in range(KS)]
    NTt = P.sb("NTt", [128, KT, TT], BF16); nk = [Buf("nk%d" % i) for i in range(KT)]
    yin = [P.sb("yin%d" % i, [128, TT], F32) for i in range(2)]
    uin = [P.sb("uin%d" % i, [128, TT], F32) for i in range(2)]
    e1 = [P.sb("e1_%d" % i, [128, TT], F32) for i in range(2)]
    e2 = [P.sb("e2_%d" % i, [128, TT], F32) for i in range(2)]
    e3 = [P.sb("e3_%d" % i, [128, TT], F32) for i in range(2)]
    sq = [P.sb("sq%d" % i, [128, TT], BF16) for i in range(2)]
    rs_a = P.sb("rs_a", [128, TT], F32); rs_s = P.sb("rs_s", [128, TT], F32); rtmp = P.sb("rtmp", [128, TT], F32)
    wg = [P.sb("wg%d" % i, [128, KS, 128], BF16) for i in range(2)]
    wt = [P.sb("wt%d" % i, [128, KT, 128], BF16) for i in range(3)]
    xin = [P.sb("xin%d" % i, [128, TT], F32) for i in range(3)]
    ssq = [P.ps("ssq%d" % i, [128, 512]) for i in range(2)]
    acc = [P.ps("acc%d" % i, [128, 512]) for i in range(3)]
    GC = 2.0 * math.sqrt(2.0 / math.pi)
    it = 0
    for (c0, n, isctx) in token_tiles(nlat, nctx, TT):
        for kt in range(KS):
            yi = yin[kt % 2]; ui = uin[kt % 2]; a1 = e1[kt % 2]; a2 = e2[kt % 2]; a3 = e3[kt % 2]
            P.dma("sp", yi[:, :n], yT[:, kt, c0:c0 + n], writes=[yi], sem_buf=yi)
            P.dma("sp", ui[:, :n], uT[:, kt, c0:c0 + n], writes=[ui], sem_buf=ui)
            P.op("dve", lambda e: e.scalar_tensor_tensor(out=a1[:, :n], in0=ui[:, :n], scalar=dskt[:, kt:kt + 1], in1=yi[:, :n], op0=ALU.mult, op1=ALU.add),
                 reads=[ui, yi, dskt], writes=[a1])
            P.op("act", lambda e: e.activation(out=a2[:, :n], in_=a1[:, :n], func=AF.Square), reads=[a1], writes=[a2])
            P.op("pool", lambda e: e.tensor_scalar(out=a2[:, :n], in0=a2[:, :n], scalar1=0.044715, scalar2=1.0, op0=ALU.mult, op1=ALU.add), reads=[a2], writes=[a2])
            P.op("pool", lambda e: e.tensor_tensor(out=a3[:, :n], in0=a2[:, :n], in1=a1[:, :n], op=ALU.mult), reads=[a1, a2], writes=[a3])
            P.op("act", lambda e: e.activation(out=a3[:, :n], in_=a3[:, :n], func=AF.Sigmoid, scale=GC), reads=[a3], writes=[a3])
            P.op("dve", lambda e: e.tensor_tensor(out=YG[:, kt, :n], in0=a1[:, :n], in1=a3[:, :n], op=ALU.mult), reads=[a1, a3], writes=[ygk[kt]])
            P.op("pool", lambda e: e.tensor_copy(out=YB[:, kt, :n], in_=YG[:, kt, :n]), reads=[ygk[kt]], writes=[ybk[kt]])
        for m in range(KS):
            wb = wg[m % 2]; a = acc[it % 3]; it += 1
            P.dma("pool", wb[:], wglu[:, m * 128:(m + 1) * 128].rearrange("(kt p) m -> p kt m", p=128), writes=[wb], sem_buf=wb)
            for kt in range(KS):
                P.op("pe", lambda e: e.matmul(a[:, :n], lhsT=wb[:, kt, :], rhs=YB[:, kt, :n], start=(kt == 0), stop=(kt == KS - 1)),
                     reads=[wb, ybk[kt]], writes=[a], track=(kt == KS - 1))
            s_ = e1[m % 2]
            P.op("act", lambda e: e.activation(out=s_[:, :n], in_=a[:, :n], func=AF.Sigmoid), reads=[a], writes=[s_])
            P.op("dve", lambda e: e.tensor_tensor(out=OS[:, m, :n], in0=YG[:, m, :n], in1=s_[:, :n], op=ALU.mult), reads=[ygk[m], s_], writes=[osk[m]])
        for kt in range(KS):
            s = sq[kt % 2]
            P.op("act", lambda e: e.activation(out=s[:, :n], in_=OS[:, kt, :n], func=AF.Square), reads=[osk[kt]], writes=[s])
            P.op("pe", lambda e: e.matmul(ssq[0][:, :n], lhsT=ones[:], rhs=s[:, :n], start=(kt == 0), stop=(kt == KS - 1)), reads=[s, ones], writes=[ssq[0]])
        rstd_from_ssq(P, ssq[0], rs_s, rtmp, n, SW)
        for kt in range(KA_):
            s = sq[kt % 2]
            P.dma("sp", OT[:, kt, :n], oT[:, kt, c0:c0 + n], writes=[ok[kt]], sem_buf=ok[kt])
            P.op("act", lambda e: e.activation(out=s[:, :n], in_=OT[:, kt, :n], func=AF.Square), reads=[ok[kt]], writes=[s])
            P.op("pe", lambda e: e.matmul(ssq[1][:, :n], lhsT=ones[:], rhs=s[:, :n], start=(kt == 0), stop=(kt == KA_ - 1)), reads=[s, ones], writes=[ssq[1]])
        rstd_from_ssq(P, ssq[1], rs_a, rtmp, n, AW)
        for kt in range(KT):
            a1 = e2[kt % 2]
            if kt < KA_:
                src = OT[:, kt, :n]; sb_ = ok[kt]; rs = rs_a; gsc = goat[:, kt:kt + 1]; gb_ = goat
            else:
                src = OS[:, kt - KA_, :n]; sb_ = osk[kt - KA_]; rs = rs_s; gsc = gost[:, kt - KA_:kt - KA_ + 1]; gb_ = gost
            P.op("dve", lambda e: e.tensor_tensor(out=a1[:, :n], in0=src, in1=rs[:, :n], op=ALU.mult), reads=[sb_, rs], writes=[a1])
            P.op("act", lambda e: e.activation(out=NTt[:, kt, :n], in_=a1[:, :n], func=AF.Copy, scale=gsc), reads=[a1, gb_], writes=[nk[kt]])
        for m in range(KT):
            wb = wt[m % 3]; a = acc[it % 3]; it += 1; xi = xin[m % 3]
            P.dma("pool", wb[:], wout[:, m * 128:(m + 1) * 128].rearrange("(kt p) m -> p kt m", p=128), writes=[wb], sem_buf=wb)
            P.dma("sp", xi[:, :n], xT[m * 128:(m + 1) * 128, c0:c0 + n], writes=[xi], sem_buf=xi)
            for kt in range(KT):
                P.op("pe", lambda e: e.matmul(a[:, :n], lhsT=wb[:, kt, :], rhs=NTt[:, kt, :n], start=(kt == 0), stop=(kt == KT - 1)),
                     reads=[wb, nk[kt]], writes=[a], track=(kt == KT - 1))
            P.op("dve", lambda e: e.scalar_tensor_tensor(out=xi[:, :n], in0=a[:, :n], scalar=gat[isctx][:, m:m + 1], in1=xi[:, :n], op0=ALU.mult, op1=ALU.add),
                 reads=[a, xi, gat[isctx]], writes=[xi])
            P.dma("sp", x1T[m * 128:(m + 1) * 128, c0:c0 + n], xi[:, :n], reads=[xi], sem_buf=xi, is_output=True)
    return P


def build_ffn(nlat, nctx, moe, final, TT=512, P=None):
    P = P or Prog()
    NT = nlat + nctx; KT = D // 128
    xT = P.dram("x1T", [D, NT], F32, "ExternalInput")
    modv = P.dram("modv", [2, 3, D], F32, "ExternalInput")
    gvec = P.dram("g", [D], F32, "ExternalInput")
    if moe:
        chunks = [(e, DE // 128) for e in range(NE)]
        wg = P.dram("wg", [NE, D, DE], F32, "ExternalInput"); wu = P.dram("wu", [NE, D, DE], F32, "ExternalInput")
        wd = P.dram("wd", [NE, DE, D], F32, "ExternalInput")
        wr = P.dram("wr", [D, NE], F32, "ExternalInput")
        idn_d = P.dram("idn", [128, 128], F32, "ExternalInput")
    else:
        HM = DFF // 128
        chunks = [(0, HM // 2), (HM // 2, HM - HM // 2)]
        wg = P.dram("wg", [D, DFF], F32, "ExternalInput"); wu = P.dram("wu", [D, DFF], F32, "ExternalInput")
        wd = P.dram("wd", [DFF, D], F32, "ExternalInput")
    HC = max(c[1] for c in chunks)
    if final:
        gfin = P.dram("gfin", [D], F32, "ExternalInput")
        x2T = P.dram("x2s", [D, NT], F32, "Internal")
        outT = P.dram("outT", [D, NT], F32, "ExternalOutput")
    else:
        x2T = P.dram("x2T", [D, NT], F32, "ExternalOutput")
    x2b = [Buf("x2b%d" % m) for m in range(KT)]

    ones = P.sb("ones", [128, 128], BF16); P.op("dve", lambda e: e.memset(ones[:], 1.0), writes=[ones])
    gt = P.sb("gt", [128, KT], F32); load_cols(P, gt, gvec.t)
    gs = []; sh = []; gf = []
    for j in range(2):
        s_ = P.sb("sh%d" % j, [128, KT], F32); load_cols(P, s_, modv[j, 0])
        c_ = P.sb("sc%d" % j, [128, KT], F32); load_cols(P, c_, modv[j, 1])
        f_ = P.sb("gf%d" % j, [128, KT], F32); load_cols(P, f_, modv[j, 2])
        g_ = P.sb("gs%d" % j, [128, KT], F32)
        P.op("dve", lambda e: e.scalar_tensor_tensor(out=g_[:], in0=c_[:], scalar=1.0, in1=gt[:], op0=ALU.add, op1=ALU.mult), reads=[c_, gt], writes=[g_])
        gs.append(g_); sh.append(s_); gf.append(f_)
    if final:
        gfn = P.sb("gfn", [128, KT], F32); load_cols(P, gfn, gfin.t)
    if moe:
        wrt = P.sb("wrt", [128, KT, NE], F32)
        P.dma("sp", wrt[:], wr.t.rearrange("(kt p) e -> p kt e", p=128), writes=[wrt], sem_buf=wrt, allow_slow_non_contiguous=True)
        idn = P.sb("idn", [128, 128], F32); P.dma("sp", idn[:], idn_d[:, :], writes=[idn], sem_buf=idn)
        GT = P.sb("GT", [128, NE, TT], F32)
        h32 = [P.sb("h32_%d" % i, [128, TT], F32) for i in range(2)]
        rps = [P.ps("rps%d" % i, [128, 512]) for i in range(4)]
        lg = P.sb("lg", [128, NE], F32); l2 = P.sb("l2", [128, NE], F32); eq1 = P.sb("eq1", [128, NE], F32); eq2 = P.sb("eq2", [128, NE], F32)
        mx1 = P.sb("mx1", [128, 1], F32); mx2 = P.sb("mx2", [128, 1], F32); dd = P.sb("dd", [128, 1], F32)
        w1 = P.sb("w1", [128, 1], F32); w2 = P.sb("w2", [128, 1], F32); gg = P.sb("gg", [128, NE], F32)
        gbb = [P.sb("gbb%d" % i, [128, 128], F32) for i in range(2)]
    hT = P.sb("hT", [128, KT, TT], BF16); hk = [Buf("hk%d" % i) for i in range(KT)]
    HID = P.sb("HID", [128, HC, TT], BF16); hidk = [Buf("hid%d" % i) for i in range(HC)]
    xs = [P.sb("xs%d" % i, [128, TT], F32) for i in range(3)]
    sq = [P.sb("sq%d" % i, [128, TT], BF16) for i in range(2)]
    tn = [P.sb("tn%d" % i, [128, TT], F32) for i in range(2)]
    rstd = P.sb("rstd", [128, TT], F32); rtmp = P.sb("rtmp", [128, TT], F32)
    wgt = [P.sb("wgt%d" % i, [128, KT, 128], BF16) for i in range(2)]
    wut = [P.sb("wut%d" % i, [128, KT, 128], BF16) for i in range(2)]
    wdt = [P.sb("wdt%d" % i, [128, HC, 128], BF16) for i in range(2)]
    sg = [P.sb("sg%d" % i, [128, TT], F32) for i in range(2)]
    ug = [P.sb("ug%d" % i, [128, TT], F32) for i in range(2)]
    ssq = P.ps("ssq", [128, 512])
    acc = [P.ps("acc%d" % i, [128, 512]) for i in range(3)]
    it = 0

    def stats(src_dram, src_bufs, c0, n, fdim):
        for kt in range(KT):
            xi = xs[kt % 3]; s = sq[kt % 2]
            P.dma("sp", xi[:, :n], src_dram[kt * 128:(kt + 1) * 128, c0:c0 + n], reads=[src_bufs[kt]] if src_bufs else [], writes=[xi], sem_buf=xi)
            P.op("act", lambda e: e.activation(out=s[:, :n], in_=xi[:, :n], func=AF.Square), reads=[xi], writes=[s])
            P.op("pe", lambda e: e.matmul(ssq[:, :n], lhsT=ones[:], rhs=s[:, :n], start=(kt == 0), stop=(kt == KT - 1)), reads=[s, ones], writes=[ssq])
        rstd_from_ssq(P, ssq, rstd, rtmp, n, fdim)

    for (c0, n, isctx) in token_tiles(nlat, nctx, TT):
        nblk = (n + 127) // 128
        stats(xT, None, c0, n, D)
        for kt in range(KT):
            xi = xs[kt % 3]; t_ = tn[kt % 2]
            P.dma("sp", xi[:, :n], xT[kt * 128:(kt + 1) * 128, c0:c0 + n], writes=[xi], sem_buf=xi)
            P.op("dve", lambda e: e.tensor_tensor(out=t_[:, :n], in0=xi[:, :n], in1=rstd[:, :n], op=ALU.mult), reads=[xi, rstd], writes=[t_])
            if not moe:
                P.op("act", lambda e: e.activation(out=hT[:, kt, :n], in_=t_[:, :n], func=AF.Identity, scale=gs[isctx][:, kt:kt + 1], bias=sh[isctx][:, kt:kt + 1]),
                     reads=[t_, gs[isctx], sh[isctx]], writes=[hk[kt]])
            else:
                h_ = h32[kt % 2]
                P.op("act", lambda e: e.activation(out=h_[:, :n], in_=t_[:, :n], func=AF.Identity, scale=gs[isctx][:, kt:kt + 1], bias=sh[isctx][:, kt:kt + 1]),
                     reads=[t_, gs[isctx], sh[isctx]], writes=[h_])
                P.op("dve", lambda e: e.tensor_copy(out=hT[:, kt, :n], in_=h_[:, :n]), reads=[h_], writes=[hk[kt]])
                for bk in range(nblk):
                    nb_ = min(128, n - bk * 128)
                    P.op("pe", lambda e: e.matmul(rps[bk][:nb_, 0:NE], lhsT=h_[:, bk * 128:bk * 128 + nb_], rhs=wrt[:, kt, :], start=(kt == 0), stop=(kt == KT - 1)),
                         reads=[h_, wrt], writes=[rps[bk]])
        if moe:
            for bk in range(nblk):
                nb_ = min(128, n - bk * 128)
                P.op("dve", lambda e: e.tensor_copy(out=lg[:nb_], in_=rps[bk][:nb_, 0:NE]), reads=[rps[bk]], writes=[lg])
                P.op("dve", lambda e: e.reduce_max(out=mx1[:nb_], in_=lg[:nb_], axis=mybir.AxisListType.X), reads=[lg], writes=[mx1])
                P.op("dve", lambda e: e.tensor_scalar(out=eq1[:nb_], in0=lg[:nb_], scalar1=mx1[:nb_, 0:1], scalar2=None, op0=ALU.is_equal), reads=[lg, mx1], writes=[eq1])
                P.op("dve", lambda e: e.scalar_tensor_tensor(out=l2[:nb_], in0=eq1[:nb_], scalar=-1e30, in1=lg[:nb_], op0=ALU.mult, op1=ALU.add), reads=[eq1, lg], writes=[l2])
                P.op("dve", lambda e: e.reduce_max(out=mx2[:nb_], in_=l2[:nb_], axis=mybir.AxisListType.X), reads=[l2], writes=[mx2])
                P.op("dve", lambda e: e.tensor_scalar(out=eq2[:nb_], in0=l2[:nb_], scalar1=mx2[:nb_, 0:1], scalar2=None, op0=ALU.is_equal), reads=[l2, mx2], writes=[eq2])
                P.op("dve", lambda e: e.tensor_tensor(out=dd[:nb_], in0=mx1[:nb_], in1=mx2[:nb_], op=ALU.subtract), reads=[mx1, mx2], writes=[dd])
                P.op("act", lambda e: e.activation(out=w1[:nb_], in_=dd[:nb_], func=AF.Sigmoid), reads=[dd], writes=[w1])
                P.op("act", lambda e: e.activation(out=w2[:nb_], in_=dd[:nb_], func=AF.Sigmoid, scale=-1.0), reads=[dd], writes=[w2])
                P.op("dve", lambda e: e.tensor_scalar(out=gg[:nb_], in0=eq1[:nb_], scalar1=w1[:nb_, 0:1], scalar2=None, op0=ALU.mult), reads=[eq1, w1], writes=[gg])
                P.op("dve", lambda e: e.scalar_tensor_tensor(out=gg[:nb_], in0=eq2[:nb_], scalar=w2[:nb_, 0:1], in1=gg[:nb_], op0=ALU.mult, op1=ALU.add), reads=[eq2, w2, gg], writes=[gg])
                for ex in range(NE):
                    gb_ = gbb[ex % 2]; a = acc[it % 3]; it += 1
                    P.op("dve", lambda e: e.tensor_copy(out=gb_[:nb_, :], in_=gg[:nb_, ex:ex + 1].broadcast_to([nb_, 128])), reads=[gg], writes=[gb_])
                    P.op("pe", lambda e: e.matmul(a[:, :nb_], lhsT=gb_[:nb_, :], rhs=idn[:nb_, :nb_], start=True, stop=True), reads=[gb_, idn], writes=[a])
                    P.op("act", lambda e: e.copy(out=GT[:, ex, bk * 128:bk * 128 + nb_], in_=a[:, :nb_]), reads=[a], writes=[GT])
        for ci, (cb, cn) in enumerate(chunks):
            for m in range(cn):
                wgb = wgt[m % 2]; wub = wut[m % 2]
                if moe:
                    gsrc = wg[cb, :, m * 128:(m + 1) * 128]; usrc = wu[cb, :, m * 128:(m + 1) * 128]
                else:
                    gsrc = wg[:, (cb + m) * 128:(cb + m + 1) * 128]; usrc = wu[:, (cb + m) * 128:(cb + m + 1) * 128]
                P.dma("pool", wgb[:], gsrc.rearrange("(kt p) m -> p kt m", p=128), writes=[wgb], sem_buf=wgb)
                P.dma("pool", wub[:], usrc.rearrange("(kt p) m -> p kt m", p=128), writes=[wub], sem_buf=wub)
                ag = acc[it % 3]; it += 1; au = acc[it % 3]; it += 1
                for kt in range(KT):
                    P.op("pe", lambda e: e.matmul(ag[:, :n], lhsT=wgb[:, kt, :], rhs=hT[:, kt, :n], start=(kt == 0), stop=(kt == KT - 1)),
                         reads=[wgb, hk[kt]], writes=[ag], track=(kt == KT - 1))
                for kt in range(KT):
                    P.op("pe", lambda e: e.matmul(au[:, :n], lhsT=wub[:, kt, :], rhs=hT[:, kt, :n], start=(kt == 0), stop=(kt == KT - 1)),
                         reads=[wub, hk[kt]], writes=[au], track=(kt == KT - 1))
                s_ = sg[m % 2]; u_ = ug[m % 2]
                P.op("act", lambda e: e.activation(out=s_[:, :n], in_=ag[:, :n], func=AF.Silu), reads=[ag], writes=[s_])
                if moe:
                    P.op("dve", lambda e: e.tensor_tensor(out=u_[:, :n], in0=au[:, :n], in1=GT[:, cb, :n], op=ALU.mult), reads=[au, GT], writes=[u_])
                    P.op("dve", lambda e: e.tensor_tensor(out=HID[:, m, :n], in0=s_[:, :n], in1=u_[:, :n], op=ALU.mult), reads=[s_, u_], writes=[hidk[m]])
                else:
                    P.op("dve", lambda e: e.tensor_tensor(out=HID[:, m, :n], in0=au[:, :n], in1=s_[:, :n], op=ALU.mult), reads=[au, s_], writes=[hidk[m]])
            for mo in range(KT):
                wdb = wdt[mo % 2]; a = acc[it % 3]; it += 1; xi = xs[mo % 3]
                if moe:
                    dsrc = wd[cb, :, mo * 128:(mo + 1) * 128]
                else:
                    dsrc = wd[cb * 128:(cb + cn) * 128, mo * 128:(mo + 1) * 128]
                P.dma("pool", wdb[:, :cn, :], dsrc.rearrange("(kt p) m -> p kt m", p=128), writes=[wdb], sem_buf=wdb)
                if ci == 0:
                    P.dma("sp", xi[:, :n], xT[mo * 128:(mo + 1) * 128, c0:c0 + n], writes=[xi], sem_buf=xi)
                else:
                    P.dma("sp", xi[:, :n], x2T[mo * 128:(mo + 1) * 128, c0:c0 + n], reads=[x2b[mo]], writes=[xi], sem_buf=xi)
                for kt in range(cn):
                    P.op("pe", lambda e: e.matmul(a[:, :n], lhsT=wdb[:, kt, :], rhs=HID[:, kt, :n], start=(kt == 0), stop=(kt == cn - 1)),
                         reads=[wdb, hidk[kt]], writes=[a], track=(kt == cn - 1))
                P.op("dve", lambda e: e.scalar_tensor_tensor(out=xi[:, :n], in0=a[:, :n], scalar=gf[isctx][:, mo:mo + 1], in1=xi[:, :n], op0=ALU.mult, op1=ALU.add),
                     reads=[a, xi, gf[isctx]], writes=[xi])
                P.dma("sp", x2T[mo * 128:(mo + 1) * 128, c0:c0 + n], xi[:, :n], reads=[xi], writes=[x2b[mo]], sem_buf=xi, is_output=not final)
        if final:
            stats(x2T, x2b, c0, n, D)
            for kt in range(KT):
                xi = xs[kt % 3]; t_ = tn[kt % 2]
                P.dma("sp", xi[:, :n], x2T[kt * 128:(kt + 1) * 128, c0:c0 + n], reads=[x2b[kt]], writes=[xi], sem_buf=xi)
                P.op("dve", lambda e: e.tensor_tensor(out=t_[:, :n], in0=xi[:, :n], in1=rstd[:, :n], op=ALU.mult), reads=[xi, rstd], writes=[t_])
                P.op("act", lambda e: e.activation(out=t_[:, :n], in_=t_[:, :n], func=AF.Copy, scale=gfn[:, kt:kt + 1]), reads=[t_, gfn], writes=[t_])
                P.dma("sp", outT[kt * 128:(kt + 1) * 128, c0:c0 + n], t_[:, :n], reads=[t_], sem_buf=t_, is_output=True)
    return P


NLAT = SEQ // 4
NCTX = CTX // 4
_PROGS = {}


def _prog(key, fn):
    if key not in _PROGS:
        _PROGS[key] = fn()
        _PROGS[key].finish()
    return _PROGS[key]


def _launch(P, ims):
    res = run_bass_kernel_spmd(P.nc, ims, core_ids=list(range(NCORES)))
    return res.results


def _ca(a):
    return np.ascontiguousarray(a)


def build_attn_s5(NB, NCQ, with_ctx):
    P = Prog()
    build_attn(NB, NCQ, P=P)
    P.barrier(); P.release()
    build_s5(8, with_ctx, P=P)
    return P


def build_ffn_pre(nlat, nctx):
    P = Prog()
    build_ffn(nlat, nctx, False, False, P=P)
    P.barrier(); P.release()
    P.over = {"xT": P.handles["x2T"]}; P.prefix = "p_"
    build_pre(nlat, nctx, P=P)
    return P


def _pre_inputs(l, i, mods, W, pre=""):
    b, r = divmod(i, 4)
    cosT, sinT, Rm = rope_consts(r * NLAT, NLAT)
    modv = np.stack([np.stack([mods[l, 0, :, b], mods[l, 1, :, b]]), np.stack([mods[l, 0, :, 2], mods[l, 1, :, 2]])])
    return {pre + "w": W["w_in"][l], pre + "modv": _ca(modv), pre + "g": W["g_attn_norm"][l], pre + "cosT": cosT, pre + "sinT": sinT,
            pre + "rmat": Rm}


def _attn_s5(l, ra, W, ncq, pre=""):
    g_ = lambda i, k: ra[i][pre + k]
    with_ctx = ncq > 0
    P = _prog(("attn_s5", ncq), lambda: build_attn_s5(NLAT // 128, ncq, with_ctx))
    zk = np.zeros((128, NKV, 128), NPBF)

    def tokmaj(a):
        return a.transpose(2, 1, 0).reshape(a.shape[2], -1)
    u_x = np.stack([np.concatenate([tokmaj(g_(4 * b + j, "uu")[:, :, :NLAT]) for j in range(4)], 0) for b in range(B)])
    u_c = np.stack([np.concatenate([tokmaj(g_(4 * b + j, "uu")[:, :, NLAT:]) for j in range(4)], 0) for b in range(B)])
    Us = s5_pack_u(u_x, u_c)
    msk, idn = s5_consts()
    ims = []
    for i in range(NCORES):
        b, r = divmod(i, 4)
        kk = g_(i, "kk"); vv = g_(i, "vv")
        kl = g_(i - 1, "kk")[:, :, NLAT - 128:NLAT] if r > 0 else zk
        kr_ = g_(i + 1, "kk")[:, :, 0:128] if r < 3 else zk
        vl = g_(i - 1, "vv")[:, :, NLAT - 128:NLAT] if r > 0 else zk
        vr_ = g_(i + 1, "vv")[:, :, 0:128] if r < 3 else zk
        krh = np.concatenate([kl, kk[:, :, :NLAT], kr_], 2)
        vh = np.concatenate([vl, vv[:, :, :NLAT], vr_], 2)
        vt = vh.reshape(128, NKV, -1, 128).transpose(3, 2, 1, 0).reshape(128, -1, KVW)
        kc = np.concatenate([g_(4 * b + j, "kk")[:, :, NLAT:] for j in range(4)], 2)
        vcf = np.concatenate([g_(4 * b + j, "vv")[:, :, NLAT:] for j in range(4)], 2)
        vct = vcf.reshape(128, NKV, -1, 128).transpose(3, 2, 1, 0).reshape(128, -1, KVW)
        gsl = slice(i * 8, (i + 1) * 8)
        ims.append({"qr": g_(i, "qr"), "qp": _ca(g_(i, "qp")[:, :, :NLAT + ncq]), "kr": _ca(krh), "v": _ca(vt), "kc": _ca(kc), "vc": _ca(vct),
                    "mask": attn_masks(r == 0, r == 3), "sink": W["attn_sink"][l],
                    "U": Us[i], "a_re": _ca(W["ssm_a_re"][l][:, gsl]), "a_im": _ca(W["ssm_a_im"][l][:, gsl]),
                    "log_dt": _ca(W["ssm_log_dt"][l][:, gsl].reshape(-1)), "b_re": _ca(W["ssm_b_re"][l][:, gsl]), "b_im": _ca(W["ssm_b_im"][l][:, gsl]),
                    "c_re": _ca(W["ssm_c_re"][l][:, gsl]), "c_im": _ca(W["ssm_c_im"][l][:, gsl]), "msk": msk, "idn": idn})
    rb = _launch(P, ims)
    y_x, y_c = s5_unpack_y([r_["y"] for r_ in rb])
    return [r_["oT"] for r_ in rb], y_x, y_c


def _post_inputs(l, i, oT, y_x, y_c, uu, xT, mods, W, nctx_post):
    b, r = divmod(i, 4)
    ntp = NLAT + nctx_post
    yt = y_x[b, r * NLAT:(r + 1) * NLAT]
    if nctx_post:
        yt = np.concatenate([yt, y_c[b, r * NCTX:(r + 1) * NCTX]], 0)
    yT = yt.reshape(ntp, SW // 128, 128).transpose(2, 1, 0)
    gatev = np.stack([mods[l, 2, :, b], mods[l, 2, :, 2]])
    return {"oT": _ca(oT[:, :, :ntp]), "yT": _ca(yT), "uT": _ca(uu[:, :, :ntp]), "xT": _ca(xT[:, :ntp]),
            "dsk": W["ssm_d"][l], "wglu": W["w_glu"][l], "goa": W["g_out_attn"][l], "gos": W["g_out_ssm"][l], "wout": W["w_out"][l],
            "gatev": _ca(gatev)}


def _ffn_modv(l, i, mods):
    b, r = divmod(i, 4)
    return _ca(np.stack([np.stack([mods[l, 3 + j, :, b] for j in range(3)]), np.stack([mods[l, 3 + j, :, 2] for j in range(3)])]))


def kernel(x, c, ctx, c_ctx, w_mod, b_mod, g_attn_norm, w_in, attn_sink, ssm_a_re, ssm_a_im, ssm_log_dt, ssm_b_re, ssm_b_im,
           ssm_c_re, ssm_c_im, ssm_d, w_glu, g_out_attn, g_out_ssm, w_out, g_ffn_norm, w_ff_gate, w_ff_up, w_ff_down, w_router,
           w_exp_gate, w_exp_up, w_exp_down, g_final):
    W = {k: np.asarray(v) for k, v in dict(
        g_attn_norm=g_attn_norm, w_in=w_in, attn_sink=attn_sink, ssm_a_re=ssm_a_re, ssm_a_im=ssm_a_im, ssm_log_dt=ssm_log_dt,
        ssm_b_re=ssm_b_re, ssm_b_im=ssm_b_im, ssm_c_re=ssm_c_re, ssm_c_im=ssm_c_im, ssm_d=ssm_d, w_glu=w_glu, g_out_attn=g_out_attn,
        g_out_ssm=g_out_ssm, w_out=w_out, g_ffn_norm=g_ffn_norm).items()}
    x = np.asarray(x); ctx = np.asarray(ctx)
    mods = run_mod(np.asarray(c), np.asarray(c_ctx), np.asarray(w_mod), np.asarray(b_mod))
    xTs = []
    for i in range(NCORES):
        b, r = divmod(i, 4)
        xTs.append(_ca(np.concatenate([x[b, r * NLAT:(r + 1) * NLAT], ctx[b, r * NCTX:(r + 1) * NCTX]], 0).T))
    P = _prog(("pre",), lambda: build_pre(NLAT, NCTX))
    ims = []
    for i in range(NCORES):
        d = _pre_inputs(0, i, mods, W); d["xT"] = xTs[i]; ims.append(d)
    ra = _launch(P, ims)
    oT, y_x, y_c = _attn_s5(0, ra, W, NCTX)
    P = _prog(("post", NCTX), lambda: build_post(NLAT, NCTX))
    ims = [_post_inputs(0, i, oT[i], y_x, y_c, ra[i]["uu"], xTs[i], mods, W, NCTX) for i in range(NCORES)]
    x1 = [r_["x1T"] for r_ in _launch(P, ims)]
    del ra, oT, y_x, y_c, xTs
    P = _prog(("ffn_pre",), lambda: build_ffn_pre(NLAT, NCTX))
    ims = []
    for i in range(NCORES):
        d = {"x1T": x1[i], "modv": _ffn_modv(0, i, mods), "g": W["g_ffn_norm"][0], "wg": np.asarray(w_ff_gate)[0], "wu": np.asarray(w_ff_up)[0],
             "wd": np.asarray(w_ff_down)[0]}
        d.update(_pre_inputs(1, i, mods, W, "p_"))
        ims.append(d)
    r1 = _launch(P, ims)
    x2 = [r_["x2T"] for r_ in r1]
    oT, y_x, y_c = _attn_s5(1, r1, W, 0, "p_")
    P = _prog(("post", 0), lambda: build_post(NLAT, 0))
    ims = [_post_inputs(1, i, oT[i], y_x, y_c, r1[i]["p_uu"], x2[i], mods, W, 0) for i in range(NCORES)]
    x1 = [r_["x1T"] for r_ in _launch(P, ims)]
    del r1, oT, y_x, y_c, x2
    P = _prog(("ffn", 1), lambda: build_ffn(NLAT, 0, True, True))
    ims = []
    for i in range(NCORES):
        ims.append({"x1T": x1[i], "modv": _ffn_modv(1, i, mods), "g": W["g_ffn_norm"][1], "wg": np.asarray(w_exp_gate)[0], "wu": np.asarray(w_exp_up)[0],
                    "wd": np.asarray(w_exp_down)[0], "wr": np.asarray(w_router)[0], "idn": np.eye(128, dtype=np.float32), "gfin": np.asarray(g_final)})
    ro = _launch(P, ims)
    out = np.empty((B, SEQ, D), np.float32)
    for i in range(NCORES):
        b, r = divmod(i, 4)
        out[b, r * NLAT:(r + 1) * NLAT] = ro[i]["outT"].T
    return out
```
